# Optimizing a Trainium2 kernel written in Bass

```python
import jax, jax.numpy as jnp
from jax import lax
import numpy as np

D_MODEL = 2048
BATCH = 2
SEQ = 4096
DEPTH = 1
DEC_BATCH = 2
DEC_SEQ = 16384
PAST_LEN = 128

POOL_GROUPS = 4
POOL_WINDOWS = (2, 4, 8, 16)
POOL_WIDTH = D_MODEL // 2
POOL_GC = POOL_WIDTH // POOL_GROUPS
HEAD_DIM = 128
N_HEADS = D_MODEL // HEAD_DIM
N_KV_HEADS = N_HEADS // 4
Q_PER_KV = N_HEADS // N_KV_HEADS
ATTN_WIDTH = N_HEADS * HEAD_DIM
KV_WIDTH = N_KV_HEADS * HEAD_DIM
WINDOW = 128
BLOCK = 128
ROT_DIM = HEAD_DIM // 4
ROPE_THETA = 500000.0
N_BRANCHES = 2
IN_WIDTH = POOL_WIDTH + ATTN_WIDTH + 2 * KV_WIDTH + N_BRANCHES * D_MODEL
N_GROUPS = 4
EXPERTS_PER_GROUP = 8
N_EXPERTS = N_GROUPS * EXPERTS_PER_GROUP
TOP_K = 2
D_FF = D_MODEL // 2
MOE_BLOCK = 128
EPS = 1e-6

kernel_name = "hybrid_pool_bandattn_hmoe_encoder"


def rms_norm(x, g):
    xf = x.astype(jnp.float32)
    y = xf * lax.rsqrt(jnp.mean(xf * xf, axis=-1, keepdims=True) + EPS)
    return (y * g.astype(jnp.float32)).astype(x.dtype)


def multiscale_pool(xp, w_grp, scale):
    B, S, _ = xp.shape
    xg = xp.reshape(B, S, POOL_GROUPS, POOL_GC).astype(jnp.float32)
    cs = jnp.concatenate([jnp.zeros((B, 1, POOL_GROUPS, POOL_GC), jnp.float32),
                          jnp.cumsum(xg, axis=1)], axis=1)
    half = jnp.array([w // 2 for w in POOL_WINDOWS], jnp.int32)
    t = jnp.arange(S, dtype=jnp.int32)[:, None]
    lo = jnp.clip(t - half[None, :], 0, S)
    hi = jnp.clip(t + half[None, :], 0, S)
    g = jnp.arange(POOL_GROUPS, dtype=jnp.int32)[None, :]
    win_sum = cs[:, hi, g, :] - cs[:, lo, g, :]
    cnt = (hi - lo).astype(jnp.float32)[None, :, :, None]
    mixed = (win_sum / cnt - xg).astype(xp.dtype)
    y = jnp.einsum('bsgc,gcd->bsgd', mixed, w_grp)
    return y.reshape(B, S, POOL_WIDTH) * scale


def partial_rotary(x, pos):
    half = ROT_DIM // 2
    inv = ROPE_THETA ** (-jnp.arange(half, dtype=jnp.float32) / half)
    ang = pos.astype(jnp.float32)[:, None] * inv[None, :]
    cos = jnp.cos(ang)[None, :, None, :]
    sin = jnp.sin(ang)[None, :, None, :]
    xf = x.astype(jnp.float32)
    x1 = xf[..., :half]
    x2 = xf[..., half:ROT_DIM]
    out = jnp.concatenate([x1 * cos - x2 * sin, x2 * cos + x1 * sin, xf[..., ROT_DIM:]], axis=-1)
    return out.astype(x.dtype)


def banded_gqa_attention(q, k, v, q_g, k_g, sink):
    B, S = q.shape[0], q.shape[1]
    nb = S // BLOCK
    pos = jnp.arange(S, dtype=jnp.int32)
    q = partial_rotary(rms_norm(q, q_g), pos)
    k = partial_rotary(rms_norm(k, k_g), pos)
    qb = q.reshape(B, nb, BLOCK, N_KV_HEADS, Q_PER_KV, HEAD_DIM)

    def neighbours(t):
        tp = jnp.pad(t, ((0, 0), (BLOCK, BLOCK), (0, 0), (0, 0)))
        tp = tp.reshape(B, nb + 2, BLOCK, N_KV_HEADS, HEAD_DIM)
        return jnp.concatenate([tp[:, :-2], tp[:, 1:-1], tp[:, 2:]], axis=2)

    kb = neighbours(k)
    vb = neighbours(v)
    s = jnp.einsum('bnqkgd,bnjkd->bnkgqj', qb, kb,
                   preferred_element_type=jnp.float32) * (HEAD_DIM ** -0.5)
    blk = jnp.arange(nb, dtype=jnp.int32)[:, None, None]
    qpos = blk * BLOCK + jnp.arange(BLOCK, dtype=jnp.int32)[None, :, None]
    kpos = (blk - 1) * BLOCK + jnp.arange(3 * BLOCK, dtype=jnp.int32)[None, None, :]
    valid = (jnp.abs(qpos - kpos) <= WINDOW) & (kpos >= 0) & (kpos < S)
    s = jnp.where(valid[None, :, None, None], s, -1e30)
    sink_b = sink.astype(jnp.float32).reshape(1, 1, N_KV_HEADS, Q_PER_KV, 1, 1)
    m = jnp.maximum(jnp.max(s, axis=-1, keepdims=True), sink_b)
    p = jnp.exp(s - m)
    p = p / (jnp.sum(p, axis=-1, keepdims=True) + jnp.exp(sink_b - m))
    o = jnp.einsum('bnkgqj,bnjkd->bnqkgd', p.astype(v.dtype), vb)
    return o.reshape(B, S, ATTN_WIDTH)


def gated_mixer(xn, w_in, pool_w, pool_scale, pool_proj, q_g, k_g, sink, attn_proj, w_out):
    B, S, _ = xn.shape
    u = xn @ w_in
    c0 = POOL_WIDTH
    c1 = c0 + ATTN_WIDTH
    c2 = c1 + KV_WIDTH
    c3 = c2 + KV_WIDTH
    xp, q, k, v, gl = jnp.split(u, [c0, c1, c2, c3], axis=-1)
    pool_out = multiscale_pool(xp, pool_w, pool_scale) @ pool_proj
    attn_out = banded_gqa_attention(q.reshape(B, S, N_HEADS, HEAD_DIM),
                                    k.reshape(B, S, N_KV_HEADS, HEAD_DIM),
                                    v.reshape(B, S, N_KV_HEADS, HEAD_DIM),
                                    q_g, k_g, sink) @ attn_proj
    g_pool, g_attn = jnp.split(jax.nn.sigmoid(gl), N_BRANCHES, axis=-1)
    return (g_pool * pool_out + g_attn * attn_out) @ w_out


def hierarchical_route(x, wg, bg, we, be):
    T = x.shape[0]
    lg = jnp.einsum('td,dg->tg', x, wg, preferred_element_type=jnp.float32) + bg.astype(jnp.float32)
    pg = jax.nn.softmax(lg, axis=-1)
    grp = jnp.argmax(lg, axis=-1).astype(jnp.int32)
    p_grp = jnp.take_along_axis(pg, grp[:, None], axis=1)
    le = jnp.einsum('td,de->te', x, we, preferred_element_type=jnp.float32) + be.astype(jnp.float32)
    le = le.reshape(T, N_GROUPS, EXPERTS_PER_GROUP)
    le_g = jnp.take_along_axis(le, grp[:, None, None], axis=1)[:, 0]
    top_l, top_i = lax.top_k(le_g, TOP_K)
    p_e = jax.nn.softmax(top_l, axis=-1)
    expert = grp[:, None] * EXPERTS_PER_GROUP + top_i.astype(jnp.int32)
    return expert, p_grp * p_e


def grouped_experts(x, expert, weight, w_gate, w_up, w_down):
    T, D = x.shape
    M = T * TOP_K
    flat_e = expert.reshape(M)
    flat_tok = jnp.arange(M, dtype=jnp.int32) // TOP_K
    flat_w = weight.reshape(M)
    order = jnp.argsort(flat_e)
    se = flat_e[order]
    counts = jnp.bincount(flat_e, length=N_EXPERTS).astype(jnp.int32)
    padded = (counts + MOE_BLOCK - 1) // MOE_BLOCK * MOE_BLOCK
    pad_end = jnp.cumsum(padded)
    pad_start = pad_end - padded
    start = jnp.cumsum(counts) - counts
    dest = pad_start[se] + jnp.arange(M, dtype=jnp.int32) - start[se]
    nblk = -(-M // MOE_BLOCK) + N_EXPERTS
    P = nblk * MOE_BLOCK
    slot_tok = jnp.full((P,), T, jnp.int32).at[dest].set(flat_tok[order])
    slot_w = jnp.zeros((P,), flat_w.dtype).at[dest].set(flat_w[order])
    blk_start = jnp.arange(nblk, dtype=jnp.int32) * MOE_BLOCK
    blk_exp = jnp.minimum(jnp.searchsorted(pad_end, blk_start, side='right'), N_EXPERTS - 1).astype(jnp.int32)
    xpad = jnp.concatenate([x, jnp.zeros((1, D), x.dtype)], axis=0)

    def block_ffn(args):
        tok, e = args
        xb = xpad[tok]
        h = jax.nn.silu(xb @ w_gate[e]) * (xb @ w_up[e])
        return h @ w_down[e]

    yb = lax.map(block_ffn, (slot_tok.reshape(nblk, MOE_BLOCK), blk_exp))
    y = yb.reshape(P, D) * slot_w[:, None].astype(x.dtype)
    return jnp.zeros((T + 1, D), x.dtype).at[slot_tok].add(y)[:T]


def setup_inputs(seed: int = 0) -> dict:
    key = jax.random.key(seed)
    ks = jax.random.split(key, 24)
    f32 = jnp.float32
    nrm = lambda k, shape, s: jax.random.normal(k, shape, f32) * s
    return {
        "x_prompt": nrm(ks[0], (BATCH, SEQ, D_MODEL), 1.0),
        "x_sample": nrm(ks[1], (DEC_BATCH, DEC_SEQ, D_MODEL), 1.0),
        "norm1_g": 1.0 + nrm(ks[2], (DEPTH, D_MODEL), 0.02),
        "w_in": nrm(ks[3], (DEPTH, D_MODEL, IN_WIDTH), D_MODEL ** -0.5),
        "pool_w": nrm(ks[4], (DEPTH, POOL_GROUPS, POOL_GC, POOL_GC), POOL_GC ** -0.5),
        "pool_scale": 1.0 + nrm(ks[5], (DEPTH, POOL_WIDTH), 0.02),
        "pool_proj": nrm(ks[6], (DEPTH, POOL_WIDTH, D_MODEL), POOL_WIDTH ** -0.5),
        "q_norm_g": 1.0 + nrm(ks[7], (DEPTH, HEAD_DIM), 0.02),
        "k_norm_g": 1.0 + nrm(ks[8], (DEPTH, HEAD_DIM), 0.02),
        "sink": nrm(ks[9], (DEPTH, N_HEADS), 0.5),
        "attn_proj": nrm(ks[10], (DEPTH, ATTN_WIDTH, D_MODEL), ATTN_WIDTH ** -0.5),
        "w_out": nrm(ks[11], (DEPTH, D_MODEL, D_MODEL), D_MODEL ** -0.5),
        "norm2_g": 1.0 + nrm(ks[12], (DEPTH, D_MODEL), 0.02),
        "router_group_w": nrm(ks[13], (DEPTH, D_MODEL, N_GROUPS), D_MODEL ** -0.5),
        "router_group_b": nrm(ks[14], (DEPTH, N_GROUPS), 0.01),
        "router_expert_w": nrm(ks[15], (DEPTH, D_MODEL, N_EXPERTS), D_MODEL ** -0.5),
        "router_expert_b": nrm(ks[16], (DEPTH, N_EXPERTS), 0.01),
        "w_gate": nrm(ks[17], (DEPTH, N_EXPERTS, D_MODEL, D_FF), D_MODEL ** -0.5),
        "w_up": nrm(ks[18], (DEPTH, N_EXPERTS, D_MODEL, D_FF), D_MODEL ** -0.5),
        "w_down": nrm(ks[19], (DEPTH, N_EXPERTS, D_FF, D_MODEL), D_FF ** -0.5),
    }


def reference(x_prompt, x_sample, norm1_g, w_in, pool_w, pool_scale, pool_proj, q_norm_g,
              k_norm_g, sink, attn_proj, w_out, norm2_g, router_group_w, router_group_b,
              router_expert_w, router_expert_b, w_gate, w_up, w_down):
    def trunk(x):
        B, S, D = x.shape
        for l in range(DEPTH):
            xn = rms_norm(x, norm1_g[l])
            x = x + gated_mixer(xn, w_in[l], pool_w[l], pool_scale[l], pool_proj[l],
                                q_norm_g[l], k_norm_g[l], sink[l], attn_proj[l], w_out[l])
            hn = rms_norm(x, norm2_g[l]).reshape(B * S, D)
            expert, weight = hierarchical_route(hn, router_group_w[l], router_group_b[l],
                                                router_expert_w[l], router_expert_b[l])
            x = x + grouped_experts(hn, expert, weight, w_gate[l], w_up[l], w_down[l]).reshape(B, S, D)
        return x

    y_prompt = trunk(x_prompt)
    y_sample = trunk(x_sample)
    return (y_prompt, y_sample)
```

```python
import contextlib
import numpy as np
import concourse.bass as bass
import concourse.mybir as mybir
from concourse.bass_utils import run_bass_kernel_spmd

F32 = mybir.dt.float32
BF16 = mybir.dt.bfloat16
I32 = mybir.dt.int32
AF = mybir.ActivationFunctionType
ALU = mybir.AluOpType
AX = mybir.AxisListType

ENGS = ("sync", "tensor", "scalar", "vector", "gpsimd")

D = 2048
NCORE = 8
NB = 4
NBH = NB + 2
T = NB * 128
TH = NBH * 128
NST = 10
NTOK = NST * T
NE = 32
CAP = 512
NSLOT = NE * CAP
DUMP = NSLOT
DFF = 1024
EPS = 1e-6
NEG = -30000.0
import os
PHASES = int(os.environ.get('MK_PHASES', '3'))


class Sem:
    def __init__(self, handle, name):
        self.h = handle
        self.name = name
        self.count = 0


class U:
    def __init__(self, name):
        self.name = name
        self.lw = []
        self.rd = []
        self.sem = None


class Prog:
    def __init__(self, nc, stack):
        self.nc = nc
        self.stack = stack
        self.q = {e: [] for e in ENGS}
        self.sems = []
        self.esem = {}
        for e in ("tensor", "scalar", "vector", "gpsimd"):
            self.esem[e] = self.new_sem("e_" + e)
        self.waited = {e: {} for e in ENGS}

    def new_sem(self, name):
        h = self.stack.enter_context(self.nc.semaphore(name))
        s = Sem(h, name)
        self.sems.append(s)
        return s

    def _waits(self, eng, r, w, extra=()):
        need = {}

        def add(t):
            s, v = t
            if need.get(s, 0) < v:
                need[s] = v
        for u in r:
            for t in u.lw:
                add(t)
        for u in w:
            for t in u.lw:
                add(t)
            for t in u.rd:
                add(t)
        for t in extra:
            add(t)
        out = []
        wd = self.waited[eng]
        for s, v in need.items():
            if wd.get(s, 0) >= v:
                continue
            wd[s] = v
            out.append((s, v))
        return out

    def _record(self, ticket, r, w, wacc):
        for u in r:
            u.rd.append(ticket)
            if len(u.rd) > 64:
                u.rd = _compress(u.rd)
        for u in w:
            u.lw = [ticket]
            u.rd = []
        for u in wacc:
            u.lw.append(ticket)
            if len(u.lw) > 64:
                u.lw = _compress(u.lw)

    def op(self, eng, fn, r=(), w=(), wacc=(), extra=()):
        waits = self._waits(eng, list(r) + list(wacc), w, extra)
        s = self.esem[eng]
        s.count += 1
        ticket = (s, s.count)
        self._record(ticket, r, w, wacc)

        def run(e, waits=waits, fn=fn, s=s):
            for (ws, v) in waits:
                e.wait_ge(ws.h, v)
            fn(e).then_inc(s.h, 1)
        self.q[eng].append(run)
        return ticket

    def group(self, eng, fns, r=(), w=(), extra=()):
        waits = self._waits(eng, r, w, extra)
        s = self.esem[eng]
        s.count += 1
        ticket = (s, s.count)
        self._record(ticket, r, w, ())

        def run(e, waits=waits, fns=fns, s=s):
            for (ws, v) in waits:
                e.wait_ge(ws.h, v)
            for f in fns[:-1]:
                f(e)
            fns[-1](e).then_inc(s.h, 1)
        self.q[eng].append(run)
        return ticket

    def dma(self, eng, fn, su, r=(), w=(), wacc=(), extra=()):
        if su.sem is None:
            su.sem = self.new_sem("d_" + su.name)
        waits = self._waits(eng, list(r) + list(wacc), w, extra)
        s = su.sem
        s.count += 16
        ticket = (s, s.count)
        self._record(ticket, r, w, wacc)

        def run(e, waits=waits, fn=fn, s=s):
            for (ws, v) in waits:
                e.wait_ge(ws.h, v)
            fn(e).then_inc(s.h, 16)
        self.q[eng].append(run)
        return ticket

    def barrier(self, engs=ENGS):
        for eng in engs:
            lst = []
            wd = self.waited[eng]
            for s in self.sems:
                if s.count > 0 and wd.get(s, 0) < s.count:
                    wd[s] = s.count
                    lst.append((s, s.count))

            def run(e, lst=lst):
                for (ws, v) in lst:
                    e.wait_ge(ws.h, v)
            self.q[eng].append(run)

    def emit(self, block):
        q = self.q

        @block.sync
        def _(e):
            for f in q["sync"]:
                f(e)

        @block.tensor
        def _(e):
            for f in q["tensor"]:
                f(e)

        @block.scalar
        def _(e):
            for f in q["scalar"]:
                f(e)

        @block.vector
        def _(e):
            for f in q["vector"]:
                f(e)

        @block.gpsimd
        def _(e):
            for f in q["gpsimd"]:
                f(e)


def _compress(tickets):
    need = {}
    for s, v in tickets:
        if need.get(s, 0) < v:
            need[s] = v
    return list(need.items())


def handoff(olds, news):
    ts = []
    for ou in olds:
        ts.extend(ou.lw)
        ts.extend(ou.rd)
    ts = _compress(ts)
    for nu in news:
        nu.rd.extend(ts)


def build_program():
    nc = bass.Bass("TRN2", target_bir_lowering=False)
    dt = nc.dram_tensor

    def din(name, shape, d=F32):
        return dt(name, list(shape), d, kind="ExternalInput").ap()

    x_in = din("x_in", [NST, TH, D])
    rope = din("rope", [NST, TH, 64])
    kbias_d = din("kbias", [128, NST * NBH])
    invcnt_d = din("invcnt", [NST, 128, 4 * T])
    w_in = din("w_in", [D, 8192])
    pool_w = din("pool_w", [4, 256, 256])
    pool_proj = din("pool_proj", [1024, D])
    attn_proj = din("attn_proj", [D, D])
    w_out = din("w_out", [D, D])
    w_gate = din("w_gate", [NE, D, DFF])
    w_up = din("w_up", [NE, D, DFF])
    w_down = din("w_down", [NE, DFF, D])
    g1T_d = din("g1T", [128, 16])
    g2_d = din("g2bc", [128, D])
    pscT_d = din("pscT", [128, 8])
    qg_d = din("qgbc", [128, 128])
    kg_d = din("kgbc", [128, 128])
    sink_d = din("sinkbc", [128, 16])
    wr_d = din("wr", [D, 36])
    br_d = din("brbc", [128, 36])
    ident_d = din("ident", [128, 128])
    masks_d = din("masks", [128, 2 * 512])
    lstrict_d = din("lstrict", [128, 128])
    ones_d = din("ones", [128, 128])
    ecap_d = din("ecap", [128, NE])
    pidx_d = din("pidx", [128, 1])

    y_out = dt("y", [NTOK, D], F32, kind="ExternalOutput").ap()
    xg = dt("xg", [NSLOT + 128, D], BF16, kind="Internal").ap()
    ye = dt("ye", [NSLOT + 128, D], F32, kind="Internal").ap()

    with contextlib.ExitStack() as st:
        P = Prog(nc, st)
        block = None

        def sb(name, shape, d, stack=st):
            return stack.enter_context(nc.sbuf_tensor("s_" + name, list(shape), d))

        banks = [st.enter_context(nc.psum_tensor("bank%d" % i, [128, 512], F32)) for i in range(8)]
        banks_bf = [b.bitcast(BF16) for b in banks]
        BK = [U("bk%d" % i) for i in range(8)]
        bstate = {"i": 0}

        def nb():
            i = bstate["i"]
            bstate["i"] = (i + 1) % 8
            return i

        UC = U("const")
        g1T = sb("g1T", [128, 16], F32)
        g2bc = sb("g2bc", [128, D], F32)
        pscT = sb("pscT", [128, 8], F32)
        qgbc = sb("qgbc", [128, 128], F32)
        kgbc = sb("kgbc", [128, 128], F32)
        esink = sb("esink", [128, 16], F32)
        wr = sb("wr", [128, 16, 36], F32)
        brbc = sb("brbc", [128, 36], F32)
        ident_f = sb("ident_f", [128, 128], F32)
        ident_b = sb("ident_b", [128, 128], BF16)
        masks = sb("masks", [128, 2, 512], BF16)
        lstrict = sb("lstrict", [128, 128], BF16)
        ones_b = sb("ones_b", [128, 128], BF16)
        ecap = sb("ecap", [128, NE], F32)
        pidx = sb("pidx", [128, 1], F32)
        kbias = sb("kbias", [128, NST * NBH], F32)
        poolw = sb("poolw", [128, 8, 256], BF16)
        tslot = sb("tslot", [128, NST * NB, 2], I32)
        twt = sb("twt", [128, NST * NB, 2], F32)
        basebc = sb("basebc", [128, NE], F32)
        UTS = [U("ts%d" % i) for i in range(NST * NB)]
        UBASE = U("base")

        def cload(eng, out, in_):
            P.dma(eng, lambda e: e.dma_start(out=out, in_=in_), UC, wacc=[UC])

        cload("sync", g1T[:], g1T_d)
        cload("sync", g2bc[:], g2_d)
        cload("sync", pscT[:], pscT_d)
        cload("sync", qgbc[:], qg_d)
        cload("sync", kgbc[:], kg_d)
        cload("sync", esink[:], sink_d)
        cload("sync", wr[:], wr_d.rearrange("(k p) n -> p k n", p=128))
        cload("sync", brbc[:], br_d)
        cload("sync", ident_f[:], ident_d)
        cload("sync", ecap[:], ecap_d)
        cload("sync", pidx[:], pidx_d)
        cload("sync", kbias[:], kbias_d)
        cload("gpsimd", ident_b[:], ident_d)
        cload("gpsimd", masks[:], masks_d.rearrange("p (a n) -> p a n", a=2))
        cload("gpsimd", lstrict[:], lstrict_d)
        cload("gpsimd", ones_b[:], ones_d)
        cload("gpsimd", poolw[:], pool_w.rearrange("g (cc p) d -> p (g cc) d", p=128))
        P.op("scalar", lambda e: e.activation(out=esink[:], in_=esink[:], func=AF.Exp), r=[UC], w=[UC])
        P.op("vector", lambda e: e.memset(basebc[:], 0.0), w=[UBASE])

        UXGZ = U("xgz")
        if PHASES >= 2:
            p0 = contextlib.ExitStack()
            zt0 = sb("zt0", [128, 8, D], BF16, p0)
            UZ0 = U("zt0")
            P.op("vector", lambda e: e.memset(zt0[:], 0.0), w=[UZ0])
            nfull = (NSLOT + 128) // 1024
            for kz in range(nfull):
                P.dma("sync", lambda e, kz=kz: e.dma_start(
                    out=xg[kz * 1024:(kz + 1) * 1024, :].rearrange("(j p) d -> p j d", p=128), in_=zt0[:]),
                    UZ0, r=[UZ0], wacc=[UXGZ])
            rem0 = nfull * 1024
            P.dma("sync", lambda e: e.dma_start(out=xg[rem0:rem0 + 128, :], in_=zt0[:, 0, :]), UZ0, r=[UZ0], wacc=[UXGZ])
            P.barrier()
            p0.close()

        p1 = contextlib.ExitStack()
        sb1 = lambda name, shape, d: sb(name, shape, d, p1)
        xnT = sb1("xnT", [128, 16, TH], BF16)
        wbuf = [sb1("wbuf%d" % i, [128, 16, 512], BF16) for i in range(2)]
        hbuf = [sb1("hbuf%d" % i, [128, D], F32) for i in range(4)]
        xs = sb1("xs", [128, D], BF16)
        yT = sb1("yT", [128, 8, T], BF16)
        qT = sb1("qT", [128, 16, T], BF16)
        mT = qT
        kT = sb1("kT", [128, 4, TH], BF16)
        v_sb = sb1("v_sb", [128, NBH, 512], BF16)
        oT = sb1("oT", [128, 16, T], BF16)
        mixT = oT[:, 0:8, :]
        R1 = sb1("R1", [128, 11264], BF16)
        R2x = sb1("R2x", [128, 3072], BF16)
        stat = sb1("stat", [128, 64], F32)
        ropet = sb1("ropet", [128, NBH, 64], F32)
        rtmp = sb1("rtmp", [128, 512], F32)
        rtb = sb1("rtb", [128, 64], BF16)
        rti = sb1("rti", [128, 8], I32)

        def view(ap2d, d, pattern=None, **kw):
            v = ap2d.bitcast(d) if d != BF16 else ap2d
            if pattern:
                v = v.rearrange(pattern, **kw)
            return v

        invc = view(R1[:, 0:4096], F32, "p (g t) -> p g t", g=4)
        xpc = [view(R1[:, 4096 + i * 1536: 4096 + (i + 1) * 1536], F32) for i in range(2)]
        sA = view(R1[:, 7168:8704], F32)
        sB = view(R1[:, 8704:10240], F32)
        tmpw = view(R1[:, 10240:11264], F32)
        sg = view(R1[:, 0:4096], F32, "p (c t) -> p c t", c=4)
        tacc = view(R1[:, 4096:8192], F32, "p (c t) -> p c t", c=4)
        hnT = view(R1[:, 0:4096], F32, "p (k t) -> p k t", k=16)
        h2b = hbuf[2][:].bitcast(BF16)
        h3b = hbuf[3][:].bitcast(BF16)
        pT = [view(h2b[:, i * 1536:(i + 1) * 1536], BF16, "p (j n) -> p j n", j=3) for i in range(2)]
        sqj = h3b[:, 0:2048]
        qr = [view(h3b[:, 2048 + i * 1024: 2048 + (i + 1) * 1024], F32) for i in range(2)]
        ddt = view(R2x[:, 0:1024], F32)
        q_bf = [R2x[:, 1024 + i * 512: 1024 + (i + 1) * 512] for i in range(2)]
        rotA = view(R2x[:, 2048:2304], F32, "p (h d) -> p h d", h=4)
        rotB = view(R2x[:, 2304:2560], F32, "p (h d) -> p h d", h=4)

        UXB = [U("xb%d" % i) for i in range(4)]
        UXS = U("xs")
        USTAT = U("stat")
        UXN = [U("xn%d" % b) for b in range(NBH)]
        UW = [U("w%d" % i) for i in range(2)]
        UXPC = [U("xpc%d" % i) for i in range(2)]
        USA, USB, UTMPW, UINVC = U("sA"), U("sB"), U("tmpw"), U("invc")
        UMIX = [U("mix%d" % c) for c in range(8)]
        UYT = [U("yT%d" % c) for c in range(8)]
        UQR = [U("qr%d" % i) for i in range(2)]
        UQBF = [U("qbf%d" % i) for i in range(2)]
        UROT = U("rot")
        UQT = [[U("qT%d_%d" % (b, g)) for g in range(4)] for b in range(NB)]
        UKT = [U("kT%d" % b) for b in range(NBH)]
        UV = [U("v%d" % b) for b in range(NBH)]
        UPT = [[U("pT%d_%d" % (i, j)) for j in range(3)] for i in range(2)]
        UDD = U("dd")
        USQJ = U("sqj")
        UOT = [[U("oT%d_%d" % (b, k)) for k in range(4)] for b in range(NB)]
        USG = [U("sg%d" % c) for c in range(4)]
        UTACC = [U("tacc%d" % c) for c in range(4)]
        UMT = [U("mT%d" % c) for c in range(16)]
        UHNT = U("hnT")
        UROPE = U("rope")
        URT = U("rt")
        URTB = U("rtb")
        URTI = U("rti")
        UXG = U("xg")
        UYB = [U("y%d" % i) for i in range(NST * NB)]
        wslot = {"i": 0}

        def load_w(src_ap, kc):
            i = wslot["i"]
            wslot["i"] = 1 - i
            P.dma("gpsimd", lambda e, i=i: e.dma_start(out=wbuf[i][:, 0:kc, :],
                                                       in_=src_ap.rearrange("(k p) n -> p k n", p=128)),
                  UW[i], w=[UW[i]])
            return i

        xslot = {"i": 0}

        for s in range(NST):
            handoff(UXB[2:4], [u for row in UPT for u in row] + [USQJ] + UQR)
            P.dma("sync", lambda e, s=s: e.dma_start(out=ropet[:], in_=rope[s].rearrange("(b p) c -> p b c", p=128)),
                  UROPE, w=[UROPE])
            for b in range(NBH):
                xi = xslot["i"]
                xslot["i"] = 1 - xi
                xb = hbuf[xi]
                P.dma("sync", lambda e, s=s, b=b, xb=xb: e.dma_start(out=xb[:], in_=x_in[s, b * 128:(b + 1) * 128, :]),
                      UXB[xi], w=[UXB[xi]])
                P.op("scalar", lambda e, xb=xb: e.activation(out=sqj, in_=xb[:], func=AF.Square, accum_out=stat[:, 0:1]),
                     r=[UXB[xi]], w=[USQJ, USTAT])
                P.op("vector", lambda e: e.tensor_scalar(out=stat[:, 1:2], in0=stat[:, 0:1], scalar1=1.0 / D, scalar2=EPS,
                                                         op0=ALU.mult, op1=ALU.add), r=[USTAT], w=[USTAT])
                P.op("scalar", lambda e: e.activation(out=stat[:, 2:3], in_=stat[:, 1:2], func=AF.Sqrt), r=[USTAT], w=[USTAT])
                P.op("vector", lambda e: e.reciprocal(out=stat[:, 3:4], in_=stat[:, 2:3]), r=[USTAT], w=[USTAT])
                P.op("scalar", lambda e, xb=xb: e.activation(out=xs[:], in_=xb[:], func=AF.Copy, scale=stat[:, 3:4]),
                     r=[UXB[xi], USTAT], w=[UXS])
                for half in range(2):
                    bi = nb()
                    fns = [(lambda e, c=c, bi=bi, half=half: e.transpose(
                        out=banks_bf[bi][:, c * 128:(c + 1) * 128],
                        in_=xs[:, (half * 8 + c) * 128:(half * 8 + c + 1) * 128], identity=ident_b[:])) for c in range(8)]
                    P.group("tensor", fns, r=[UXS, UC], w=[BK[bi]])
                    P.op("vector", lambda e, bi=bi, half=half, b=b: e.tensor_tensor(
                        out=xnT[:, half * 8:(half + 1) * 8, b * 128:(b + 1) * 128],
                        in0=banks_bf[bi][:].rearrange("p (c t) -> p c t", c=8),
                        in1=g1T[:, half * 8:(half + 1) * 8].unsqueeze(2).to_broadcast([128, 8, 128]), op=ALU.mult),
                        r=[BK[bi], UC], w=[UXN[b]])

            handoff(USG + UTACC + [UHNT], [UINVC] + UXPC + [USA, USB, UTMPW])
            handoff([u for row in UOT for u in row], UMIX)
            P.dma("sync", lambda e, s=s: e.dma_start(out=invc, in_=invcnt_d[s].rearrange("p (g t) -> p g t", g=4)),
                  UINVC, w=[UINVC])
            for cg in range(2):
                wi = load_w(w_in[:, cg * 512:(cg + 1) * 512], 16)
                for cc in range(4):
                    ch = cg * 4 + cc
                    g = ch // 2
                    xi2 = ch % 2
                    xp = xpc[xi2]
                    for half in range(2):
                        bi = nb()
                        t0 = half * 384
                        fns = [(lambda e, k=k, bi=bi, wi=wi, cc=cc, t0=t0: e.matmul(
                            banks[bi][:, 0:384], lhsT=wbuf[wi][:, k, cc * 128:(cc + 1) * 128],
                            rhs=xnT[:, k, t0:t0 + 384], start=(k == 0), stop=(k == 15))) for k in range(16)]
                        P.group("tensor", fns, r=[UW[wi]] + UXN, w=[BK[bi]])
                        P.op("scalar", lambda e, bi=bi, xp=xp, t0=t0: e.activation(out=xp[:, t0:t0 + 384], in_=banks[bi][:, 0:384],
                                                                                 func=AF.Copy),
                             r=[BK[bi]], w=[UXPC[xi2]])
                    P.op("vector", lambda e, xp=xp: e.tensor_tensor(out=sA[:, 1:TH], in0=xp[:, 0:TH - 1], in1=xp[:, 1:TH], op=ALU.add),
                         r=[UXPC[xi2]], w=[USA])
                    cur, ucur, oth, uoth = sA, USA, sB, USB
                    lo = 1
                    for lvl in range(g):
                        sh = 1 << lvl
                        nlo = lo + sh
                        nhi = TH - lo - sh + 1
                        P.op("vector", lambda e, cur=cur, oth=oth, sh=sh, nlo=nlo, nhi=nhi: e.tensor_tensor(
                            out=oth[:, nlo:nhi], in0=cur[:, nlo - sh:nhi - sh], in1=cur[:, nlo + sh:nhi + sh], op=ALU.add),
                            r=[ucur], w=[uoth])
                        cur, ucur, oth, uoth = oth, uoth, cur, ucur
                        lo = nlo
                    P.op("vector", lambda e, cur=cur, g=g: e.tensor_tensor(out=tmpw[:], in0=cur[:, 128:128 + T], in1=invc[:, g, :],
                                                                          op=ALU.mult), r=[ucur, UINVC], w=[UTMPW])
                    P.op("vector", lambda e, xp=xp, ch=ch: e.tensor_tensor(out=mixT[:, ch, :], in0=tmpw[:], in1=xp[:, 128:128 + T],
                                                                          op=ALU.subtract), r=[UTMPW, UXPC[xi2]], w=[UMIX[ch]])
            for g in range(4):
                for dc in range(2):
                    bi = nb()
                    fns = [(lambda e, cc=cc, g=g, dc=dc, bi=bi: e.matmul(
                        banks[bi][:, :], lhsT=poolw[:, g * 2 + cc, dc * 128:(dc + 1) * 128],
                        rhs=mixT[:, 2 * g + cc, :], start=(cc == 0), stop=(cc == 1))) for cc in range(2)]
                    P.group("tensor", fns, r=[UC, UMIX[2 * g], UMIX[2 * g + 1]], w=[BK[bi]])
                    ch = 2 * g + dc
                    P.op("scalar", lambda e, bi=bi, ch=ch: e.activation(out=yT[:, ch, :], in_=banks[bi][:, :], func=AF.Copy,
                                                                       scale=pscT[:, ch:ch + 1]), r=[BK[bi], UC], w=[UYT[ch]])

            handoff(UMT, [u for row in UQT for u in row])
            qslot = {"i": 0}
            for grp in range(6):
                col0 = 1024 + grp * 512
                wi = load_w(w_in[:, col0:col0 + 512], 16)
                blocks = range(1, 1 + NB) if grp < 4 else range(NBH)
                for b in blocks:
                    bi = nb()
                    fns = [(lambda e, k=k, bi=bi, wi=wi, b=b: e.matmul(
                        banks[bi][:, :], lhsT=xnT[:, k, b * 128:(b + 1) * 128], rhs=wbuf[wi][:, k, :],
                        start=(k == 0), stop=(k == 15))) for k in range(16)]
                    P.group("tensor", fns, r=[UW[wi], UXN[b]], w=[BK[bi]])
                    if grp == 5:
                        P.op("scalar", lambda e, bi=bi, b=b: e.activation(out=v_sb[:, b, :], in_=banks[bi][:, :], func=AF.Copy),
                             r=[BK[bi]], w=[UV[b]])
                        continue
                    qi = qslot["i"]
                    qslot["i"] = 1 - qi
                    gbc = qgbc if grp < 4 else kgbc
                    bk3 = banks[bi][:, :].rearrange("p (h d) -> p h d", h=4)
                    for hh in range(4):
                        P.op("scalar", lambda e, bi=bi, hh=hh: e.activation(
                            out=sqj[:, 0:128], in_=banks[bi][:, hh * 128:(hh + 1) * 128], func=AF.Square,
                            accum_out=stat[:, 8 + hh:9 + hh]), r=[BK[bi]], w=[USQJ, USTAT])
                    P.op("vector", lambda e: e.tensor_scalar(out=stat[:, 12:16], in0=stat[:, 8:12], scalar1=1.0 / 128, scalar2=EPS,
                                                             op0=ALU.mult, op1=ALU.add), r=[USTAT], w=[USTAT])
                    P.op("scalar", lambda e: e.activation(out=stat[:, 16:20], in_=stat[:, 12:16], func=AF.Sqrt), r=[USTAT], w=[USTAT])
                    P.op("vector", lambda e: e.reciprocal(out=stat[:, 20:24], in_=stat[:, 16:20]), r=[USTAT], w=[USTAT])
                    qr3 = qr[qi].rearrange("p (h d) -> p h d", h=4)
                    qb3 = q_bf[qi].rearrange("p (h d) -> p h d", h=4)
                    P.op("vector", lambda e, bk3=bk3, qr3=qr3: e.tensor_tensor(
                        out=qr3, in0=bk3, in1=stat[:, 20:24].unsqueeze(2).to_broadcast([128, 4, 128]), op=ALU.mult),
                        r=[BK[bi], USTAT], w=[UQR[qi]])
                    P.op("vector", lambda e, qr3=qr3, gbc=gbc: e.tensor_tensor(
                        out=qr3, in0=qr3, in1=gbc[:].unsqueeze(1).to_broadcast([128, 4, 128]), op=ALU.mult),
                        r=[UQR[qi], UC], w=[UQR[qi]])
                    P.op("vector", lambda e, qr3=qr3, b=b: e.tensor_tensor(
                        out=rotA, in0=qr3[:, :, 0:32], in1=ropet[:, b, 0:32].unsqueeze(1).to_broadcast([128, 4, 32]), op=ALU.mult),
                        r=[UQR[qi], UROPE], w=[UROT])
                    P.op("vector", lambda e, qr3=qr3, b=b: e.tensor_tensor(
                        out=rotB[:, :, 0:16], in0=qr3[:, :, 16:32], in1=ropet[:, b, 32:48].unsqueeze(1).to_broadcast([128, 4, 16]),
                        op=ALU.mult), r=[UQR[qi], UROPE], w=[UROT])
                    P.op("vector", lambda e, qr3=qr3, b=b: e.tensor_tensor(
                        out=rotB[:, :, 16:32], in0=qr3[:, :, 0:16], in1=ropet[:, b, 48:64].unsqueeze(1).to_broadcast([128, 4, 16]),
                        op=ALU.mult), r=[UQR[qi], UROPE], w=[UROT])
                    P.op("scalar", lambda e, qr3=qr3, qb3=qb3: e.activation(out=qb3[:, :, 32:128], in_=qr3[:, :, 32:128], func=AF.Copy),
                         r=[UQR[qi]], w=[UQBF[qi]])
                    P.op("vector", lambda e, qb3=qb3: e.tensor_tensor(out=qb3[:, :, 0:32], in0=rotA, in1=rotB, op=ALU.add),
                         r=[UROT], w=[UQBF[qi]])
                    bj = nb()
                    fns = [(lambda e, hh=hh, bj=bj, qi=qi: e.transpose(
                        out=banks_bf[bj][:, hh * 128:(hh + 1) * 128], in_=q_bf[qi][:, hh * 128:(hh + 1) * 128],
                        identity=ident_b[:])) for hh in range(4)]
                    P.group("tensor", fns, r=[UQBF[qi], UC], w=[BK[bj]])
                    src = banks_bf[bj][:, 0:512].rearrange("p (h t) -> p h t", h=4)
                    if grp < 4:
                        qb = b - 1
                        P.op("scalar", lambda e, src=src, grp=grp, qb=qb: e.activation(
                            out=qT[:, grp * 4:(grp + 1) * 4, qb * 128:(qb + 1) * 128], in_=src, func=AF.Copy),
                            r=[BK[bj]], w=[UQT[qb][grp]])
                    else:
                        P.op("scalar", lambda e, src=src, b=b: e.activation(
                            out=kT[:, :, b * 128:(b + 1) * 128], in_=src, func=AF.Copy), r=[BK[bj]], w=[UKT[b]])

            handoff(UMIX, [u for row in UOT for u in row])
            scale = 128.0 ** -0.5
            pslot = {"i": 0}
            for qb in range(NB):
                for kh in range(4):
                    pi = pslot["i"]
                    pslot["i"] = 1 - pi
                    for jc in range(3):
                        kb = qb + jc
                        bi = nb()
                        fns = [lambda e, bi=bi, kb=kb, kh=kh, qb=qb, jc=jc: e.matmul(
                            banks[bi][:, :].rearrange("p (h q) -> p h q", h=4), lhsT=kT[:, kh, kb * 128:(kb + 1) * 128],
                            rhs=qT[:, kh * 4:(kh + 1) * 4, qb * 128:(qb + 1) * 128], start=True, stop=(jc == 1))]
                        if jc != 1:
                            mi = 0 if jc == 0 else 1
                            fns.append(lambda e, bi=bi, mi=mi: e.matmul(banks[bi][:, :], lhsT=ident_b[:], rhs=masks[:, mi, :],
                                                                      start=False, stop=True))
                        P.group("tensor", fns, r=[UKT[kb], UC] + UQT[qb], w=[BK[bi]])
                        col = s * NBH + kb
                        P.op("scalar", lambda e, bi=bi, pi=pi, jc=jc, col=col: e.activation(
                            out=pT[pi][:, jc, :], in_=banks[bi][:, :], func=AF.Exp, bias=kbias[:, col:col + 1], scale=scale),
                            r=[BK[bi], UC], w=[UPT[pi][jc]])
                    bo = nb()
                    fns = [(lambda e, jc=jc, bo=bo, pi=pi, qb=qb, kh=kh: e.matmul(
                        banks[bo][:, :], lhsT=v_sb[:, qb + jc, kh * 128:(kh + 1) * 128], rhs=pT[pi][:, jc, :],
                        start=(jc == 0), stop=(jc == 2))) for jc in range(3)]
                    P.group("tensor", fns, r=[UV[qb], UV[qb + 1], UV[qb + 2]] + UPT[pi], w=[BK[bo]])
                    bd = nb()
                    fns = [(lambda e, jc=jc, bd=bd, pi=pi: e.matmul(
                        banks[bd][:, :], lhsT=ones_b[:], rhs=pT[pi][:, jc, :], start=(jc == 0), stop=(jc == 2))) for jc in range(3)]
                    P.group("tensor", fns, r=[UC] + UPT[pi], w=[BK[bd]])
                    dd3 = ddt.rearrange("p (h q) -> p h q", h=4)
                    P.op("vector", lambda e, bd=bd, kh=kh, dd3=dd3: e.tensor_tensor(
                        out=dd3, in0=banks[bd][:, :].rearrange("p (h q) -> p h q", h=4),
                        in1=esink[:, kh * 4:(kh + 1) * 4].unsqueeze(2).to_broadcast([128, 4, 128]), op=ALU.add),
                        r=[BK[bd], UC], w=[UDD])
                    P.op("vector", lambda e: e.reciprocal(out=ddt, in_=ddt), r=[UDD], w=[UDD])
                    P.op("vector", lambda e, bo=bo, qb=qb, kh=kh, dd3=dd3: e.tensor_tensor(
                        out=oT[:, kh * 4:(kh + 1) * 4, qb * 128:(qb + 1) * 128],
                        in0=banks[bo][:, :].rearrange("p (h q) -> p h q", h=4), in1=dd3, op=ALU.mult),
                        r=[BK[bo], UDD], w=[UOT[qb][kh]])

            handoff([UINVC] + UXPC + [USA, USB, UTMPW, UHNT], USG + UTACC)
            handoff([u for row in UQT for u in row], UMT)
            allOT = [u for row in UOT for u in row]
            xn_main = lambda k: xnT[:, k, 128:128 + T]
            for cg in range(4):
                wi = load_w(w_in[:, 4096 + cg * 512: 4096 + (cg + 1) * 512], 16)
                for cc in range(4):
                    bi = nb()
                    fns = [(lambda e, k=k, bi=bi, wi=wi, cc=cc: e.matmul(
                        banks[bi][:, :], lhsT=wbuf[wi][:, k, cc * 128:(cc + 1) * 128], rhs=xn_main(k),
                        start=(k == 0), stop=(k == 15))) for k in range(16)]
                    P.group("tensor", fns, r=[UW[wi]] + UXN, w=[BK[bi]])
                    P.op("scalar", lambda e, bi=bi, cc=cc: e.activation(out=sg[:, cc, :], in_=banks[bi][:, :], func=AF.Sigmoid),
                         r=[BK[bi]], w=[USG[cc]])
                wi = load_w(pool_proj[:, cg * 512:(cg + 1) * 512], 8)
                for cc in range(4):
                    bi = nb()
                    fns = [(lambda e, k=k, bi=bi, wi=wi, cc=cc: e.matmul(
                        banks[bi][:, :], lhsT=wbuf[wi][:, k, cc * 128:(cc + 1) * 128], rhs=yT[:, k, :],
                        start=(k == 0), stop=(k == 7))) for k in range(8)]
                    P.group("tensor", fns, r=[UW[wi]] + UYT, w=[BK[bi]])
                    P.op("vector", lambda e, bi=bi, cc=cc: e.tensor_tensor(out=tacc[:, cc, :], in0=banks[bi][:, :], in1=sg[:, cc, :],
                                                                        op=ALU.mult), r=[BK[bi], USG[cc]], w=[UTACC[cc]])
                wi = load_w(w_in[:, 6144 + cg * 512: 6144 + (cg + 1) * 512], 16)
                for cc in range(4):
                    bi = nb()
                    fns = [(lambda e, k=k, bi=bi, wi=wi, cc=cc: e.matmul(
                        banks[bi][:, :], lhsT=wbuf[wi][:, k, cc * 128:(cc + 1) * 128], rhs=xn_main(k),
                        start=(k == 0), stop=(k == 15))) for k in range(16)]
                    P.group("tensor", fns, r=[UW[wi]] + UXN, w=[BK[bi]])
                    P.op("scalar", lambda e, bi=bi, cc=cc: e.activation(out=sg[:, cc, :], in_=banks[bi][:, :], func=AF.Sigmoid),
                         r=[BK[bi]], w=[USG[cc]])
                wi = load_w(attn_proj[:, cg * 512:(cg + 1) * 512], 16)
                for cc in range(4):
                    bi = nb()
                    fns = [(lambda e, k=k, bi=bi, wi=wi, cc=cc: e.matmul(
                        banks[bi][:, :], lhsT=wbuf[wi][:, k, cc * 128:(cc + 1) * 128], rhs=oT[:, k, :],
                        start=(k == 0), stop=(k == 15))) for k in range(16)]
                    P.group("tensor", fns, r=[UW[wi]] + allOT, w=[BK[bi]])
                    P.op("vector", lambda e, bi=bi, cc=cc: e.tensor_tensor(out=sg[:, cc, :], in0=banks[bi][:, :], in1=sg[:, cc, :],
                                                                        op=ALU.mult), r=[BK[bi], USG[cc]], w=[USG[cc]])
                    c = cg * 4 + cc
                    P.op("vector", lambda e, cc=cc, c=c: e.tensor_tensor(out=mT[:, c, :], in0=sg[:, cc, :], in1=tacc[:, cc, :],
                                                                      op=ALU.add), r=[USG[cc], UTACC[cc]], w=[UMT[c]])

            handoff([u for row in UPT for u in row] + [USQJ] + UQR, UXB[2:4])
            handoff(USG + UTACC, [UHNT])
            for b in range(NB):
                P.dma("sync", lambda e, s=s, b=b: e.dma_start(out=hbuf[b][:], in_=x_in[s, (b + 1) * 128:(b + 2) * 128, :]),
                      UXB[b], w=[UXB[b]])
            for cg in range(4):
                wi = load_w(w_out[:, cg * 512:(cg + 1) * 512], 16)
                for b in range(NB):
                    bi = nb()
                    fns = [(lambda e, k=k, bi=bi, wi=wi, b=b: e.matmul(
                        banks[bi][:, :], lhsT=mT[:, k, b * 128:(b + 1) * 128], rhs=wbuf[wi][:, k, :],
                        start=(k == 0), stop=(k == 15))) for k in range(16)]
                    P.group("tensor", fns, r=[UW[wi]] + UMT, w=[BK[bi]])
                    P.op("vector", lambda e, bi=bi, b=b, cg=cg: e.tensor_tensor(
                        out=hbuf[b][:, cg * 512:(cg + 1) * 512], in0=banks[bi][:, :], in1=hbuf[b][:, cg * 512:(cg + 1) * 512],
                        op=ALU.add), r=[BK[bi]], w=[UXB[b]])
            for b in range(NB):
                tb = s * NB + b
                hb = hbuf[b]
                P.dma("sync", lambda e, tb=tb, hb=hb: e.dma_start(out=y_out[tb * 128:(tb + 1) * 128, :], in_=hb[:]),
                      UXB[b], r=[UXB[b]], w=[UYB[tb]])
                if PHASES < 2:
                    continue
                P.op("scalar", lambda e, hb=hb: e.activation(out=xs[:], in_=hb[:], func=AF.Square, accum_out=stat[:, 0:1]),
                     r=[UXB[b]], w=[UXS, USTAT])
                P.op("vector", lambda e: e.tensor_scalar(out=stat[:, 1:2], in0=stat[:, 0:1], scalar1=1.0 / D, scalar2=EPS,
                                                         op0=ALU.mult, op1=ALU.add), r=[USTAT], w=[USTAT])
                P.op("scalar", lambda e: e.activation(out=stat[:, 2:3], in_=stat[:, 1:2], func=AF.Sqrt), r=[USTAT], w=[USTAT])
                P.op("vector", lambda e: e.reciprocal(out=stat[:, 3:4], in_=stat[:, 2:3]), r=[USTAT], w=[USTAT])
                P.op("vector", lambda e, hb=hb: e.scalar_tensor_tensor(out=hb[:], in0=hb[:], scalar=stat[:, 3:4], in1=g2bc[:],
                                                                      op0=ALU.mult, op1=ALU.mult), r=[USTAT, UC], w=[UXB[b]])
                P.op("scalar", lambda e, hb=hb: e.activation(out=xs[:], in_=hb[:], func=AF.Copy), r=[UXB[b]], w=[UXS])
                for q4 in range(4):
                    bi = nb()
                    fns = [(lambda e, c=c, bi=bi, q4=q4, hb=hb: e.transpose(
                        out=banks[bi][:, c * 128:(c + 1) * 128], in_=hb[:, (q4 * 4 + c) * 128:(q4 * 4 + c + 1) * 128],
                        identity=ident_f[:])) for c in range(4)]
                    P.group("tensor", fns, r=[UXB[b], UC], w=[BK[bi]])
                    P.op("scalar", lambda e, bi=bi, q4=q4: e.activation(
                        out=hnT[:, q4 * 4:(q4 + 1) * 4, :], in_=banks[bi][:, :].rearrange("p (c t) -> p c t", c=4), func=AF.Copy),
                        r=[BK[bi]], w=[UHNT])
                bi = nb()
                fns = [(lambda e, k=k, bi=bi: e.matmul(banks[bi][:, 0:36], lhsT=hnT[:, k, :], rhs=wr[:, k, :],
                                                       start=(k == 0), stop=(k == 15))) for k in range(16)]
                P.group("tensor", fns, r=[UHNT, UC], w=[BK[bi]])
                L = rtmp
                P.op("vector", lambda e, bi=bi: e.tensor_tensor(out=L[:, 0:36], in0=banks[bi][:, 0:36], in1=brbc[:], op=ALU.add),
                     r=[BK[bi], UC], w=[URT])
                P.op("vector", lambda e: e.memset(L[:, 40:48], -1e30), r=[], w=[URT])
                P.op("vector", lambda e: e.tensor_copy(out=L[:, 40:44], in_=L[:, 0:4]), r=[URT], w=[URT])
                P.op("vector", lambda e: e.max(out=L[:, 48:56], in_=L[:, 40:48]), r=[URT], w=[URT])
                P.op("vector", lambda e: e.tensor_scalar(out=L[:, 56:60], in0=L[:, 0:4], scalar1=L[:, 48:49], scalar2=None,
                                                         op0=ALU.is_equal), r=[URT], w=[URT])
                P.op("vector", lambda e: e.tensor_scalar(out=L[:, 240:241], in0=L[:, 48:49], scalar1=-1.0, scalar2=None, op0=ALU.mult),
                     r=[URT], w=[URT])
                P.op("scalar", lambda e: e.activation(out=L[:, 244:248], in_=L[:, 0:4], func=AF.Exp, bias=L[:, 240:241], scale=1.0,
                                                      accum_out=L[:, 241:242]), r=[URT], w=[URT])
                P.op("vector", lambda e: e.reciprocal(out=L[:, 242:243], in_=L[:, 241:242]), r=[URT], w=[URT])
                P.op("vector", lambda e: e.tensor_scalar(out=L[:, 60:64], in0=L[:, 56:60], scalar1=-1.0, scalar2=1e30,
                                                         op0=ALU.add, op1=ALU.mult), r=[URT], w=[URT])
                P.op("vector", lambda e: e.tensor_tensor(
                    out=L[:, 64:96].rearrange("p (g e) -> p g e", g=4), in0=L[:, 4:36].rearrange("p (g e) -> p g e", g=4),
                    in1=L[:, 60:64].unsqueeze(2).to_broadcast([128, 4, 8]), op=ALU.add), r=[URT], w=[URT])
                P.op("vector", lambda e: e.max(out=L[:, 96:104], in_=L[:, 64:96]), r=[URT], w=[URT])
                P.op("vector", lambda e: e.tensor_scalar(out=L[:, 104:136], in0=L[:, 64:96], scalar1=L[:, 96:97], scalar2=None,
                                                         op0=ALU.is_equal), r=[URT], w=[URT])
                P.op("vector", lambda e: e.tensor_scalar(out=L[:, 136:168], in0=L[:, 64:96], scalar1=L[:, 97:98], scalar2=None,
                                                         op0=ALU.is_equal), r=[URT], w=[URT])
                P.op("vector", lambda e: e.tensor_tensor(out=L[:, 248:249], in0=L[:, 97:98], in1=L[:, 96:97], op=ALU.subtract),
                     r=[URT], w=[URT])
                P.op("scalar", lambda e: e.activation(out=L[:, 249:250], in_=L[:, 248:249], func=AF.Exp), r=[URT], w=[URT])
                P.op("vector", lambda e: e.tensor_scalar(out=L[:, 250:251], in0=L[:, 249:250], scalar1=1.0, scalar2=None, op0=ALU.add),
                     r=[URT], w=[URT])
                P.op("vector", lambda e: e.reciprocal(out=L[:, 251:252], in_=L[:, 250:251]), r=[URT], w=[URT])
                P.op("vector", lambda e: e.tensor_tensor(out=L[:, 252:253], in0=L[:, 249:250], in1=L[:, 251:252], op=ALU.mult),
                     r=[URT], w=[URT])
                P.op("vector", lambda e, tb=tb: e.tensor_scalar(out=twt[:, tb, 0:1], in0=L[:, 251:252], scalar1=L[:, 242:243],
                                                                scalar2=None, op0=ALU.mult), r=[URT], w=[UTS[tb]])
                P.op("vector", lambda e, tb=tb: e.tensor_scalar(out=twt[:, tb, 1:2], in0=L[:, 252:253], scalar1=L[:, 242:243],
                                                                scalar2=None, op0=ALU.mult), r=[URT], wacc=[UTS[tb]])
                P.op("vector", lambda e: e.tensor_tensor(out=rtb[:, 0:32], in0=L[:, 104:136], in1=L[:, 136:168], op=ALU.add),
                     r=[URT], w=[URTB])
                bi = nb()
                P.group("tensor", [lambda e, bi=bi: e.matmul(banks[bi][:, 0:32], lhsT=lstrict[:], rhs=rtb[:, 0:32], start=True, stop=True),
                                   lambda e, bi=bi: e.matmul(banks[bi][:, 32:64], lhsT=ones_b[:], rhs=rtb[:, 0:32], start=True, stop=True)],
                        r=[URTB, UC], w=[BK[bi]])
                P.op("vector", lambda e, bi=bi: e.tensor_tensor(out=L[:, 168:200], in0=banks[bi][:, 0:32], in1=basebc[:], op=ALU.add),
                     r=[BK[bi], UBASE], w=[URT])
                P.op("vector", lambda e, bi=bi: e.tensor_tensor(out=basebc[:], in0=banks[bi][:, 32:64], in1=basebc[:], op=ALU.add),
                     r=[BK[bi]], w=[UBASE])
                for kk in range(2):
                    oh = L[:, 104 + 32 * kk:136 + 32 * kk]
                    P.op("vector", lambda e, oh=oh: e.tensor_tensor(out=L[:, 200:232], in0=oh, in1=L[:, 168:200], op=ALU.mult),
                         r=[URT], w=[URT])
                    P.op("vector", lambda e: e.tensor_reduce(out=L[:, 253:254], in_=L[:, 200:232], axis=AX.X, op=ALU.add),
                         r=[URT], w=[URT])
                    P.op("vector", lambda e, oh=oh: e.tensor_tensor(out=L[:, 200:232], in0=oh, in1=ecap[:], op=ALU.mult),
                         r=[URT, UC], w=[URT])
                    P.op("vector", lambda e: e.tensor_reduce(out=L[:, 254:255], in_=L[:, 200:232], axis=AX.X, op=ALU.add),
                         r=[URT], w=[URT])
                    P.op("vector", lambda e: e.tensor_scalar(out=L[:, 255:256], in0=L[:, 253:254], scalar1=float(CAP), scalar2=None,
                                                             op0=ALU.is_lt), r=[URT], w=[URT])
                    P.op("vector", lambda e: e.tensor_tensor(out=L[:, 256:257], in0=L[:, 253:254], in1=L[:, 254:255], op=ALU.add),
                         r=[URT], w=[URT])
                    P.op("vector", lambda e: e.tensor_tensor(out=L[:, 256:257], in0=L[:, 256:257], in1=pidx[:], op=ALU.subtract),
                         r=[URT, UC], w=[URT])
                    P.op("vector", lambda e: e.scalar_tensor_tensor(out=L[:, 257:258], in0=L[:, 256:257], scalar=L[:, 255:256],
                                                                    in1=pidx[:], op0=ALU.mult, op1=ALU.add), r=[URT, UC], w=[URT])
                    P.op("vector", lambda e, tb=tb, kk=kk: e.tensor_copy(out=tslot[:, tb, kk:kk + 1], in_=L[:, 257:258]),
                         r=[URT], wacc=[UTS[tb]])
                for kk in range(2):
                    P.dma("gpsimd", lambda e, tb=tb, kk=kk: e.indirect_dma_start(
                        out=xg, out_offset=bass.IndirectOffsetOnAxis(ap=tslot[:, tb, kk:kk + 1], axis=0),
                        in_=xs[:], in_offset=None), UXS, r=[UXS, UTS[tb], UXGZ], wacc=[UXG])

        P.barrier()
        p1.close()

        if PHASES >= 2:
            p2 = contextlib.ExitStack()
            sb2 = lambda name, shape, d: sb(name, shape, d, p2)
            xgt = [sb2("xgt%d" % i, [128, D], BF16) for i in range(4)]
            xgT = sb2("xgT", [128, 16, CAP], BF16)
            wgu = [sb2("wgu%d" % i, [128, 16, 256], BF16) for i in range(4)]
            wd = [sb2("wd%d" % i, [128, 8, 512], BF16) for i in range(2)]
            hT = sb2("hT", [128, 8, CAP], BF16)
            sil = [sb2("sil%d" % i, [128, CAP], F32) for i in range(2)]
            yet = [sb2("yet%d" % i, [128, D], F32) for i in range(4)]
            zt = sb2("zt", [128, D], F32)
            UXGT = [U("xgt%d" % i) for i in range(4)]
            UXGTT = [U("xgT%d" % i) for i in range(4)]
            UWGU = [U("wgu%d" % i) for i in range(4)]
            UWD = [U("wd%d" % i) for i in range(2)]
            UHT = [U("hT%d" % f) for f in range(8)]
            USIL = [U("sil%d" % i) for i in range(2)]
            UYET = [U("yet%d" % i) for i in range(4)]
            UYE = U("ye")
            UZT = U("zt")
            P.op("vector", lambda e: e.memset(zt[:], 0.0), w=[UZT])
            P.dma("sync", lambda e: e.dma_start(out=ye[NSLOT:NSLOT + 128, :], in_=zt[:]), UZT, r=[UZT], wacc=[UYE])
            gslot = {"i": 0}
            dslot = {"i": 0}
            sslot = {"i": 0}
            for ex in range(NE):
                for sbk in range(4):
                    r0 = ex * CAP + sbk * 128
                    P.dma("sync", lambda e, r0=r0, sbk=sbk: e.dma_start(out=xgt[sbk][:], in_=xg[r0:r0 + 128, :]),
                          UXGT[sbk], r=[UXG], w=[UXGT[sbk]])
                    for half in range(2):
                        bi = nb()
                        fns = [(lambda e, c=c, bi=bi, half=half, sbk=sbk: e.transpose(
                            out=banks_bf[bi][:, c * 128:(c + 1) * 128],
                            in_=xgt[sbk][:, (half * 8 + c) * 128:(half * 8 + c + 1) * 128], identity=ident_b[:])) for c in range(8)]
                        P.group("tensor", fns, r=[UXGT[sbk], UC], w=[BK[bi]])
                        P.op("vector", lambda e, bi=bi, half=half, sbk=sbk: e.tensor_copy(
                            out=xgT[:, half * 8:(half + 1) * 8, sbk * 128:(sbk + 1) * 128],
                            in_=banks_bf[bi][:].rearrange("p (c t) -> p c t", c=8)), r=[BK[bi]], w=[UXGTT[sbk]])
                for pc in range(4):
                    gi = gslot["i"]
                    ui = (gi + 1) % 4
                    gslot["i"] = (gi + 2) % 4
                    P.dma("gpsimd", lambda e, ex=ex, pc=pc, gi=gi: e.dma_start(
                        out=wgu[gi][:], in_=w_gate[ex].rearrange("(k p) n -> p k n", p=128)[:, :, pc * 256:(pc + 1) * 256]),
                        UWGU[gi], w=[UWGU[gi]])
                    P.dma("gpsimd", lambda e, ex=ex, pc=pc, ui=ui: e.dma_start(
                        out=wgu[ui][:], in_=w_up[ex].rearrange("(k p) n -> p k n", p=128)[:, :, pc * 256:(pc + 1) * 256]),
                        UWGU[ui], w=[UWGU[ui]])
                    for fc in range(2):
                        f = pc * 2 + fc
                        bg = nb()
                        fns = [(lambda e, k=k, bg=bg, gi=gi, fc=fc: e.matmul(
                            banks[bg][:, :], lhsT=wgu[gi][:, k, fc * 128:(fc + 1) * 128], rhs=xgT[:, k, :],
                            start=(k == 0), stop=(k == 15))) for k in range(16)]
                        P.group("tensor", fns, r=[UWGU[gi]] + UXGTT, w=[BK[bg]])
                        bu = nb()
                        fns = [(lambda e, k=k, bu=bu, ui=ui, fc=fc: e.matmul(
                            banks[bu][:, :], lhsT=wgu[ui][:, k, fc * 128:(fc + 1) * 128], rhs=xgT[:, k, :],
                            start=(k == 0), stop=(k == 15))) for k in range(16)]
                        P.group("tensor", fns, r=[UWGU[ui]] + UXGTT, w=[BK[bu]])
                        si = sslot["i"]
                        sslot["i"] = 1 - si
                        P.op("scalar", lambda e, bg=bg, si=si: e.activation(out=sil[si][:], in_=banks[bg][:, :], func=AF.Silu),
                             r=[BK[bg]], w=[USIL[si]])
                        P.op("vector", lambda e, bu=bu, si=si, f=f: e.tensor_tensor(out=hT[:, f, :], in0=banks[bu][:, :], in1=sil[si][:],
                                                                                 op=ALU.mult), r=[BK[bu], USIL[si]], w=[UHT[f]])
                for cg in range(4):
                    di = dslot["i"]
                    dslot["i"] = 1 - di
                    P.dma("gpsimd", lambda e, ex=ex, cg=cg, di=di: e.dma_start(
                        out=wd[di][:], in_=w_down[ex].rearrange("(k p) n -> p k n", p=128)[:, :, cg * 512:(cg + 1) * 512]),
                        UWD[di], w=[UWD[di]])
                    for sbk in range(4):
                        bi = nb()
                        fns = [(lambda e, f=f, bi=bi, di=di, sbk=sbk: e.matmul(
                            banks[bi][:, :], lhsT=hT[:, f, sbk * 128:(sbk + 1) * 128], rhs=wd[di][:, f, :],
                            start=(f == 0), stop=(f == 7))) for f in range(8)]
                        P.group("tensor", fns, r=[UWD[di]] + UHT, w=[BK[bi]])
                        P.op("scalar", lambda e, bi=bi, sbk=sbk, cg=cg: e.activation(
                            out=yet[sbk][:, cg * 512:(cg + 1) * 512], in_=banks[bi][:, :], func=AF.Copy),
                            r=[BK[bi]], w=[UYET[sbk]])
                for sbk in range(4):
                    r0 = ex * CAP + sbk * 128
                    P.dma("sync", lambda e, r0=r0, sbk=sbk: e.dma_start(out=ye[r0:r0 + 128, :], in_=yet[sbk][:]),
                          UYET[sbk], r=[UYET[sbk]], wacc=[UYE])
            P.barrier()
            p2.close()

            p3 = contextlib.ExitStack()
            sb3 = lambda name, shape, d: sb(name, shape, d, p3)
            hb3 = [sb3("hb3_%d" % i, [128, D], F32) for i in range(2)]
            r1 = [sb3("r1_%d" % i, [128, D], F32) for i in range(2)]
            r2 = [sb3("r2_%d" % i, [128, D], F32) for i in range(2)]
            UH3 = [U("h3_%d" % i) for i in range(2)]
            UR1 = [U("r1_%d" % i) for i in range(2)]
            UR2 = [U("r2_%d" % i) for i in range(2)]
            for tb in range(NST * NB):
                i = tb % 2
                P.dma("sync", lambda e, tb=tb, i=i: e.dma_start(out=hb3[i][:], in_=y_out[tb * 128:(tb + 1) * 128, :]),
                      UH3[i], r=[UYB[tb]], w=[UH3[i]])
                P.dma("gpsimd", lambda e, tb=tb, i=i: e.indirect_dma_start(
                    out=r1[i][:], out_offset=None, in_=ye,
                    in_offset=bass.IndirectOffsetOnAxis(ap=tslot[:, tb, 0:1], axis=0)), UR1[i], r=[UYE, UTS[tb]], w=[UR1[i]])
                P.dma("gpsimd", lambda e, tb=tb, i=i: e.indirect_dma_start(
                    out=r2[i][:], out_offset=None, in_=ye,
                    in_offset=bass.IndirectOffsetOnAxis(ap=tslot[:, tb, 1:2], axis=0)), UR2[i], r=[UYE, UTS[tb]], w=[UR2[i]])
                P.op("vector", lambda e, tb=tb, i=i: e.scalar_tensor_tensor(
                    out=hb3[i][:], in0=r1[i][:], scalar=twt[:, tb, 0:1], in1=hb3[i][:], op0=ALU.mult, op1=ALU.add),
                    r=[UR1[i], UTS[tb]], w=[UH3[i]])
                P.op("vector", lambda e, tb=tb, i=i: e.scalar_tensor_tensor(
                    out=hb3[i][:], in0=r2[i][:], scalar=twt[:, tb, 1:2], in1=hb3[i][:], op0=ALU.mult, op1=ALU.add),
                    r=[UR2[i], UTS[tb]], w=[UH3[i]])
                P.dma("sync", lambda e, tb=tb, i=i: e.dma_start(out=y_out[tb * 128:(tb + 1) * 128, :], in_=hb3[i][:]),
                      UH3[i], r=[UH3[i]], w=[UYB[tb]])
            P.barrier()
            p3.close()

        block = st.enter_context(nc.Block())
        P.emit(block)
    return nc


def _core_layout(c):
    b = c // 4
    qtr = c % 4
    sts = []
    for i in range(2):
        sts.append(("p", b, qtr * 1024 + i * T, 4096))
    for i in range(8):
        sts.append(("s", b, qtr * 4096 + i * T, 16384))
    return sts


def _rope_table(pos):
    half = 16
    inv = (np.float32(500000.0) ** (-np.arange(half, dtype=np.float32) / np.float32(half))).astype(np.float32)
    ang = (pos.astype(np.float32)[:, None] * inv[None, :]).astype(np.float32)
    cos = np.cos(ang).astype(np.float32)
    sin = np.sin(ang).astype(np.float32)
    return np.concatenate([cos, cos, -sin, sin], axis=1)


_NC_CACHE = {}


def kernel(x_prompt, x_sample, norm1_g, w_in, pool_w, pool_scale, pool_proj, q_norm_g, k_norm_g, sink,
           attn_proj, w_out, norm2_g, router_group_w, router_group_b, router_expert_w, router_expert_b,
           w_gate, w_up, w_down):
    f32 = np.float32
    xp = np.asarray(x_prompt, f32)
    xsm = np.asarray(x_sample, f32)
    bc = lambda v, n: np.ascontiguousarray(np.broadcast_to(np.asarray(v, f32).reshape(1, n), (128, n)))
    shared = {
        "w_in": np.ascontiguousarray(np.asarray(w_in, f32)[0]),
        "pool_w": np.ascontiguousarray(np.asarray(pool_w, f32)[0]),
        "pool_proj": np.ascontiguousarray(np.asarray(pool_proj, f32)[0]),
        "attn_proj": np.ascontiguousarray(np.asarray(attn_proj, f32)[0]),
        "w_out": np.ascontiguousarray(np.asarray(w_out, f32)[0]),
        "w_gate": np.ascontiguousarray(np.asarray(w_gate, f32)[0]),
        "w_up": np.ascontiguousarray(np.asarray(w_up, f32)[0]),
        "w_down": np.ascontiguousarray(np.asarray(w_down, f32)[0]),
        "g1T": np.ascontiguousarray(np.asarray(norm1_g, f32)[0].reshape(16, 128).T),
        "g2bc": bc(np.asarray(norm2_g)[0], D),
        "pscT": np.ascontiguousarray(np.asarray(pool_scale, f32)[0].reshape(8, 128).T),
        "qgbc": bc(np.asarray(q_norm_g)[0], 128),
        "kgbc": bc(np.asarray(k_norm_g)[0], 128),
        "sinkbc": bc(np.asarray(sink)[0], 16),
        "wr": np.ascontiguousarray(np.concatenate([np.asarray(router_group_w, f32)[0], np.asarray(router_expert_w, f32)[0]], axis=1)),
        "brbc": bc(np.concatenate([np.asarray(router_group_b, f32)[0], np.asarray(router_expert_b, f32)[0]]), 36),
        "ident": np.eye(128, dtype=f32),
        "lstrict": np.triu(np.ones((128, 128), f32), 1),
        "ones": np.ones((128, 128), f32),
        "ecap": bc(np.arange(NE, dtype=f32) * CAP, NE),
        "pidx": (DUMP + np.arange(128, dtype=f32)).reshape(128, 1),
    }
    jj = np.arange(128)[:, None]
    qq = np.arange(128)[None, :]
    mL = np.where(qq <= jj, 0.0, NEG).astype(f32)
    mR = np.where(jj <= qq, 0.0, NEG).astype(f32)
    shared["masks"] = np.ascontiguousarray(np.concatenate([np.tile(mL, (1, 4)), np.tile(mR, (1, 4))], axis=1))

    in_maps = []
    for c in range(NCORE):
        sts = _core_layout(c)
        x_in = np.zeros((NST, TH, D), f32)
        rope = np.zeros((NST, TH, 64), f32)
        kb = np.zeros((128, NST * NBH), f32)
        invc = np.zeros((NST, 128, 4 * T), f32)
        for si, (which, b, s0, S) in enumerate(sts):
            src = xp[b] if which == "p" else xsm[b]
            lo = s0 - 128
            hi = s0 + T + 128
            a = max(lo, 0)
            z = min(hi, S)
            x_in[si, a - lo:z - lo] = src[a:z]
            pos = np.arange(lo, hi)
            rope[si] = _rope_table(np.clip(pos, 0, S - 1))
            for blk in range(NBH):
                p0 = lo + blk * 128
                if p0 < 0 or p0 >= S:
                    kb[:, si * NBH + blk] = NEG
            t = np.arange(s0, s0 + T)
            for g, w in enumerate((2, 4, 8, 16)):
                h = w // 2
                cnt = np.clip(t + h, 0, S) - np.clip(t - h, 0, S)
                invc[si, :, g * T:(g + 1) * T] = (1.0 / cnt.astype(f32))[None, :]
        m = dict(shared)
        m.update({"x_in": x_in, "rope": rope, "kbias": kb, "invcnt": invc})
        in_maps.append(m)

    if "nc" not in _NC_CACHE:
        _NC_CACHE["nc"] = build_program()
    nc = _NC_CACHE["nc"]
    res = run_bass_kernel_spmd(nc, in_maps, core_ids=list(range(NCORE)))
    y_prompt = np.zeros((2, 4096, D), f32)
    y_sample = np.zeros((2, 16384, D), f32)
    for c in range(NCORE):
        y = np.asarray(res.results[c]["y"], f32)
        b = c // 4
        qtr = c % 4
        y_prompt[b, qtr * 1024:(qtr + 1) * 1024] = y[0:1024]
        y_sample[b, qtr * 4096:(qtr + 1) * 4096] = y[1024:5120]
    return (y_prompt, y_sample)
```

```python
import contextlib
import numpy as np
import concourse.bass as bass
import concourse.mybir as mybir
from concourse.bass_utils import run_bass_kernel_spmd

F32 = mybir.dt.float32
BF16 = mybir.dt.bfloat16
I32 = mybir.dt.int32
AF = mybir.ActivationFunctionType
ALU = mybir.AluOpType
AX = mybir.AxisListType

ENGS = ("sync", "tensor", "scalar", "vector", "gpsimd")

D = 2048
NCORE = 8
NB = 4
NBH = NB + 2
T = NB * 128
TH = NBH * 128
NST = 10
NTOK = NST * T
NE = 32
CAP = 512
NSLOT = NE * CAP
DUMP = NSLOT
DFF = 1024
EPS = 1e-6
NEG = -30000.0
import os
PHASES = int(os.environ.get('MK_PHASES', '3'))


class Sem:
    def __init__(self, handle, name):
        self.h = handle
        self.name = name
        self.count = 0


class U:
    def __init__(self, name):
        self.name = name
        self.lw = []
        self.rd = []
        self.sem = None


class Prog:
    def __init__(self, nc, stack):
        self.nc = nc
        self.stack = stack
        self.q = {e: [] for e in ENGS}
        self.sems = []
        self.esem = {}
        for e in ("tensor", "scalar", "vector", "gpsimd"):
            self.esem[e] = self.new_sem("e_" + e)
        self.waited = {e: {} for e in ENGS}

    def new_sem(self, name):
        h = self.stack.enter_context(self.nc.semaphore(name))
        s = Sem(h, name)
        self.sems.append(s)
        return s

    def _waits(self, eng, r, w, extra=()):
        need = {}

        def add(t):
            s, v = t
            if need.get(s, 0) < v:
                need[s] = v
        for u in r:
            for t in u.lw:
                add(t)
        for u in w:
            for t in u.lw:
                add(t)
            for t in u.rd:
                add(t)
        for t in extra:
            add(t)
        out = []
        wd = self.waited[eng]
        for s, v in need.items():
            if wd.get(s, 0) >= v:
                continue
            wd[s] = v
            out.append((s, v))
        return out

    def _record(self, ticket, r, w, wacc):
        for u in r:
            u.rd.append(ticket)
            if len(u.rd) > 64:
                u.rd = _compress(u.rd)
        for u in w:
            u.lw = [ticket]
            u.rd = []
        for u in wacc:
            u.lw.append(ticket)
            if len(u.lw) > 64:
                u.lw = _compress(u.lw)

    def op(self, eng, fn, r=(), w=(), wacc=(), extra=()):
        waits = self._waits(eng, list(r) + list(wacc), w, extra)
        s = self.esem[eng]
        s.count += 1
        ticket = (s, s.count)
        self._record(ticket, r, w, wacc)

        def run(e, waits=waits, fn=fn, s=s):
            for (ws, v) in waits:
                e.wait_ge(ws.h, v)
            fn(e).then_inc(s.h, 1)
        self.q[eng].append(run)
        return ticket

    def group(self, eng, fns, r=(), w=(), extra=()):
        waits = self._waits(eng, r, w, extra)
        s = self.esem[eng]
        s.count += 1
        ticket = (s, s.count)
        self._record(ticket, r, w, ())

        def run(e, waits=waits, fns=fns, s=s):
            for (ws, v) in waits:
                e.wait_ge(ws.h, v)
            for f in fns[:-1]:
                f(e)
            fns[-1](e).then_inc(s.h, 1)
        self.q[eng].append(run)
        return ticket

    def dma(self, eng, fn, su, r=(), w=(), wacc=(), extra=()):
        if su.sem is None:
            su.sem = self.new_sem("d_" + su.name)
        waits = self._waits(eng, list(r) + list(wacc), w, extra)
        s = su.sem
        s.count += 16
        ticket = (s, s.count)
        self._record(ticket, r, w, wacc)

        def run(e, waits=waits, fn=fn, s=s):
            for (ws, v) in waits:
                e.wait_ge(ws.h, v)
            fn(e).then_inc(s.h, 16)
        self.q[eng].append(run)
        return ticket

    def barrier(self, engs=ENGS):
        for eng in engs:
            lst = []
            wd = self.waited[eng]
            for s in self.sems:
                if s.count > 0 and wd.get(s, 0) < s.count:
                    wd[s] = s.count
                    lst.append((s, s.count))

            def run(e, lst=lst):
                for (ws, v) in lst:
                    e.wait_ge(ws.h, v)
            self.q[eng].append(run)

    def emit(self, block):
        q = self.q

        @block.sync
        def _(e):
            for f in q["sync"]:
                f(e)

        @block.tensor
        def _(e):
            for f in q["tensor"]:
                f(e)

        @block.scalar
        def _(e):
            for f in q["scalar"]:
                f(e)

        @block.vector
        def _(e):
            for f in q["vector"]:
                f(e)

        @block.gpsimd
        def _(e):
            for f in q["gpsimd"]:
                f(e)


def _compress(tickets):
    need = {}
    for s, v in tickets:
        if need.get(s, 0) < v:
            need[s] = v
    return list(need.items())


def handoff(olds, news):
    ts = []
    for ou in olds:
        ts.extend(ou.lw)
        ts.extend(ou.rd)
    ts = _compress(ts)
    for nu in news:
        nu.rd.extend(ts)


def build_program():
    nc = bass.Bass("TRN2", target_bir_lowering=False)
    dt = nc.dram_tensor

    def din(name, shape, d=F32):
        return dt(name, list(shape), d, kind="ExternalInput").ap()

    x_in = din("x_in", [NST, TH, D])
    rope = din("rope", [NST, TH, 64])
    kbias_d = din("kbias", [128, NST * NBH])
    invcnt_d = din("invcnt", [NST, 128, 4 * T])
    w_in = din("w_in", [D, 8192])
    pool_w = din("pool_w", [4, 256, 256])
    pool_proj = din("pool_proj", [1024, D])
    attn_proj = din("attn_proj", [D, D])
    w_out = din("w_out", [D, D])
    w_gate = din("w_gate", [NE, D, DFF])
    w_up = din("w_up", [NE, D, DFF])
    w_down = din("w_down", [NE, DFF, D])
    g1T_d = din("g1T", [128, 16])
    g2_d = din("g2bc", [128, D])
    pscT_d = din("pscT", [128, 8])
    qg_d = din("qgbc", [128, 128])
    kg_d = din("kgbc", [128, 128])
    sink_d = din("sinkbc", [128, 16])
    wr_d = din("wr", [D, 36])
    br_d = din("brbc", [128, 36])
    ident_d = din("ident", [128, 128])
    masks_d = din("masks", [128, 2 * 512])
    lstrict_d = din("lstrict", [128, 128])
    ones_d = din("ones", [128, 128])
    ecap_d = din("ecap", [128, NE])
    pidx_d = din("pidx", [128, 1])

    y_out = dt("y", [NTOK, D], F32, kind="ExternalOutput").ap()
    xg = dt("xg", [NSLOT + 128, D], BF16, kind="Internal").ap()
    ye = dt("ye", [NSLOT + 128, D], F32, kind="Internal").ap()

    with contextlib.ExitStack() as st:
        P = Prog(nc, st)
        block = None

        def sb(name, shape, d, stack=st):
            return stack.enter_context(nc.sbuf_tensor("s_" + name, list(shape), d))

        banks = [st.enter_context(nc.psum_tensor("bank%d" % i, [128, 512], F32)) for i in range(8)]
        banks_bf = [b.bitcast(BF16) for b in banks]
        BK = [U("bk%d" % i) for i in range(8)]
        bstate = {"i": 0}

        def nb():
            i = bstate["i"]
            bstate["i"] = (i + 1) % 8
            return i

        UC = U("const")
        g1T = sb("g1T", [128, 16], F32)
        g2bc = sb("g2bc", [128, D], F32)
        pscT = sb("pscT", [128, 8], F32)
        qgbc = sb("qgbc", [128, 128], F32)
        kgbc = sb("kgbc", [128, 128], F32)
        esink = sb("esink", [128, 16], F32)
        wr = sb("wr", [128, 16, 36], F32)
        brbc = sb("brbc", [128, 36], F32)
        ident_f = sb("ident_f", [128, 128], F32)
        ident_b = sb("ident_b", [128, 128], BF16)
        masks = sb("masks", [128, 2, 512], BF16)
        lstrict = sb("lstrict", [128, 128], BF16)
        ones_b = sb("ones_b", [128, 128], BF16)
        ecap = sb("ecap", [128, NE], F32)
        pidx = sb("pidx", [128, 1], F32)
        kbias = sb("kbias", [128, NST * NBH], F32)
        poolw = sb("poolw", [128, 8, 256], BF16)
        tslot = sb("tslot", [128, NST * NB, 2], I32)
        twt = sb("twt", [128, NST * NB, 2], F32)
        basebc = sb("basebc", [128, NE], F32)
        UTS = [U("ts%d" % i) for i in range(NST * NB)]
        UBASE = U("base")

        def cload(eng, out, in_):
            P.dma(eng, lambda e: e.dma_start(out=out, in_=in_), UC, wacc=[UC])

        cload("sync", g1T[:], g1T_d)
        cload("sync", g2bc[:], g2_d)
        cload("sync", pscT[:], pscT_d)
        cload("sync", qgbc[:], qg_d)
        cload("sync", kgbc[:], kg_d)
        cload("sync", esink[:], sink_d)
        cload("sync", wr[:], wr_d.rearrange("(k p) n -> p k n", p=128))
        cload("sync", brbc[:], br_d)
        cload("sync", ident_f[:], ident_d)
        cload("sync", ecap[:], ecap_d)
        cload("sync", pidx[:], pidx_d)
        cload("sync", kbias[:], kbias_d)
        cload("gpsimd", ident_b[:], ident_d)
        cload("gpsimd", masks[:], masks_d.rearrange("p (a n) -> p a n", a=2))
        cload("gpsimd", lstrict[:], lstrict_d)
        cload("gpsimd", ones_b[:], ones_d)
        cload("gpsimd", poolw[:], pool_w.rearrange("g (cc p) d -> p (g cc) d", p=128))
        P.op("scalar", lambda e: e.activation(out=esink[:], in_=esink[:], func=AF.Exp), r=[UC], w=[UC])
        P.op("vector", lambda e: e.memset(basebc[:], 0.0), w=[UBASE])

        UXGZ = U("xgz")
        if PHASES >= 2:
            p0 = contextlib.ExitStack()
            zt0 = sb("zt0", [128, 8, D], BF16, p0)
            UZ0 = U("zt0")
            P.op("vector", lambda e: e.memset(zt0[:], 0.0), w=[UZ0])
            nfull = (NSLOT + 128) // 1024
            for kz in range(nfull):
                P.dma("sync", lambda e, kz=kz: e.dma_start(
                    out=xg[kz * 1024:(kz + 1) * 1024, :].rearrange("(j p) d -> p j d", p=128), in_=zt0[:]),
                    UZ0, r=[UZ0], wacc=[UXGZ])
            rem0 = nfull * 1024
            P.dma("sync", lambda e: e.dma_start(out=xg[rem0:rem0 + 128, :], in_=zt0[:, 0, :]), UZ0, r=[UZ0], wacc=[UXGZ])
            P.barrier()
            p0.close()

        p1 = contextlib.ExitStack()
        sb1 = lambda name, shape, d: sb(name, shape, d, p1)
        xnT = sb1("xnT", [128, 16, TH], BF16)
        NWS = 5
        wbuf = [sb1("wbuf%d" % i, [128, 16, 256], BF16) for i in range(NWS)]
        hbuf = [sb1("hbuf%d" % i, [128, D], F32) for i in range(4)]
        xs = sb1("xs", [128, D], BF16)
        yT = sb1("yT", [128, 8, T], BF16)
        qT = sb1("qT", [128, 16, T], BF16)
        mT = qT
        kT = sb1("kT", [128, 4, TH], BF16)
        v_sb = sb1("v_sb", [128, NBH, 512], BF16)
        oT = sb1("oT", [128, 16, T], BF16)
        mixT = oT[:, 0:8, :]
        R1 = sb1("R1", [128, 11264], BF16)
        R2x = sb1("R2x", [128, 1024], BF16)
        stat = sb1("stat", [128, 64], F32)
        ropet = sb1("ropet", [128, NBH, 64], F32)
        rtmp = sb1("rtmp", [128, 512], F32)
        rtb = sb1("rtb", [128, 64], BF16)
        rti = sb1("rti", [128, 8], I32)

        def view(ap2d, d, pattern=None, **kw):
            v = ap2d.bitcast(d) if d != BF16 else ap2d
            if pattern:
                v = v.rearrange(pattern, **kw)
            return v

        invc = view(R1[:, 0:4096], F32, "p (g t) -> p g t", g=4)
        xpc = [view(R1[:, 4096 + i * 1536: 4096 + (i + 1) * 1536], F32) for i in range(2)]
        sA = view(R1[:, 7168:8704], F32)
        sB = view(R1[:, 8704:10240], F32)
        tmpw = view(R1[:, 10240:11264], F32)
        sg = view(R1[:, 0:2048], F32, "p (c t) -> p c t", c=2)
        tacc = view(R1[:, 2048:4096], F32, "p (c t) -> p c t", c=2)
        hnT = view(R1[:, 0:4096], F32, "p (k t) -> p k t", k=16)
        h2b = hbuf[2][:].bitcast(BF16)
        h3b = hbuf[3][:].bitcast(BF16)
        pT = [view(h2b[:, i * 1536:(i + 1) * 1536], BF16, "p (j n) -> p j n", j=3) for i in range(2)]
        sqj = h3b[:, 0:2048]
        ddt = view(R2x[:, 0:1024], F32)
        qr2 = [view(R1[:, i * 512:(i + 1) * 512], F32) for i in range(6)]
        qbf2 = [R1[:, 3072 + i * 256: 3072 + (i + 1) * 256] for i in range(6)]
        rotA2 = [view(R1[:, 4608 + i * 128: 4608 + (i + 1) * 128], F32, "p (h d) -> p h d", h=2) for i in range(6)]
        rotB2 = [view(R1[:, 5376 + i * 128: 5376 + (i + 1) * 128], F32, "p (h d) -> p h d", h=2) for i in range(6)]

        UXB = [U("xb%d" % i) for i in range(4)]
        UXS = U("xs")
        USTAT = U("stat")
        UXN = [U("xn%d" % b) for b in range(NBH)]
        UW = [U("w%d" % i) for i in range(5)]
        UXPC = [U("xpc%d" % i) for i in range(2)]
        USA, USB, UTMPW, UINVC = U("sA"), U("sB"), U("tmpw"), U("invc")
        UMIX = [U("mix%d" % c) for c in range(8)]
        UYT = [U("yT%d" % c) for c in range(8)]
        UQR = [U("qr%d" % i) for i in range(6)]
        UQBF = [U("qbf%d" % i) for i in range(6)]
        UROT = [U("rot%d" % i) for i in range(6)]
        UB2 = UQR + UQBF + UROT
        UQT = [[U("qT%d_%d" % (b, g)) for g in range(4)] for b in range(NB)]
        UKT = [U("kT%d" % b) for b in range(NBH)]
        UV = [U("v%d" % b) for b in range(NBH)]
        UPT = [[U("pT%d_%d" % (i, j)) for j in range(3)] for i in range(2)]
        UDD = U("dd")
        USQJ = U("sqj")
        UOT = [[U("oT%d_%d" % (b, k)) for k in range(4)] for b in range(NB)]
        USG = [U("sg%d" % c) for c in range(2)]
        UTACC = [U("tacc%d" % c) for c in range(2)]
        UMT = [U("mT%d" % c) for c in range(16)]
        UHNT = U("hnT")
        UROPE = U("rope")
        URT = U("rt")
        URTB = U("rtb")
        URTI = U("rti")
        UXG = U("xg")
        UYB = [U("y%d" % i) for i in range(NST * NB)]
        wslot = {"i": 0}

        def load_w(src_ap, kc):
            i = wslot["i"]
            wslot["i"] = (i + 1) % NWS
            P.dma("gpsimd", lambda e, i=i: e.dma_start(out=wbuf[i][:, 0:kc, :],
                                                       in_=src_ap.rearrange("(k p) n -> p k n", p=128)),
                  UW[i], w=[UW[i]])
            return i

        xslot = {"i": 0}

        for s in range(NST):
            handoff(UXB[2:4], [u for row in UPT for u in row] + [USQJ])
            P.dma("sync", lambda e, s=s: e.dma_start(out=ropet[:], in_=rope[s].rearrange("(b p) c -> p b c", p=128)),
                  UROPE, w=[UROPE])
            for b in range(NBH):
                xi = xslot["i"]
                xslot["i"] = 1 - xi
                xb = hbuf[xi]
                P.dma("sync", lambda e, s=s, b=b, xb=xb: e.dma_start(out=xb[:], in_=x_in[s, b * 128:(b + 1) * 128, :]),
                      UXB[xi], w=[UXB[xi]])
                P.op("scalar", lambda e, xb=xb: e.activation(out=sqj, in_=xb[:], func=AF.Square, accum_out=stat[:, 0:1]),
                     r=[UXB[xi]], w=[USQJ, USTAT])
                P.op("vector", lambda e: e.tensor_scalar(out=stat[:, 1:2], in0=stat[:, 0:1], scalar1=1.0 / D, scalar2=EPS,
                                                         op0=ALU.mult, op1=ALU.add), r=[USTAT], w=[USTAT])
                P.op("scalar", lambda e: e.activation(out=stat[:, 2:3], in_=stat[:, 1:2], func=AF.Sqrt), r=[USTAT], w=[USTAT])
                P.op("vector", lambda e: e.reciprocal(out=stat[:, 3:4], in_=stat[:, 2:3]), r=[USTAT], w=[USTAT])
                P.op("scalar", lambda e, xb=xb: e.activation(out=xs[:], in_=xb[:], func=AF.Copy, scale=stat[:, 3:4]),
                     r=[UXB[xi], USTAT], w=[UXS])
                for half in range(2):
                    bi = nb()
                    fns = [(lambda e, c=c, bi=bi, half=half: e.transpose(
                        out=banks_bf[bi][:, c * 128:(c + 1) * 128],
                        in_=xs[:, (half * 8 + c) * 128:(half * 8 + c + 1) * 128], identity=ident_b[:])) for c in range(8)]
                    P.group("tensor", fns, r=[UXS, UC], w=[BK[bi]])
                    P.op("vector", lambda e, bi=bi, half=half, b=b: e.tensor_tensor(
                        out=xnT[:, half * 8:(half + 1) * 8, b * 128:(b + 1) * 128],
                        in0=banks_bf[bi][:].rearrange("p (c t) -> p c t", c=8),
                        in1=g1T[:, half * 8:(half + 1) * 8].unsqueeze(2).to_broadcast([128, 8, 128]), op=ALU.mult),
                        r=[BK[bi], UC], w=[UXN[b]])

            handoff(USG + UTACC + [UHNT] + UB2, [UINVC] + UXPC + [USA, USB, UTMPW])
            handoff([u for row in UOT for u in row], UMIX)
            P.dma("sync", lambda e, s=s: e.dma_start(out=invc, in_=invcnt_d[s].rearrange("p (g t) -> p g t", g=4)),
                  UINVC, w=[UINVC])
            for pc in range(4):
                wi = load_w(w_in[:, pc * 256:(pc + 1) * 256], 16)
                for cc in range(2):
                    ch = pc * 2 + cc
                    g = ch // 2
                    xi2 = ch % 2
                    xp = xpc[xi2]
                    for half in range(2):
                        bi = nb()
                        t0 = half * 384
                        fns = [(lambda e, k=k, bi=bi, wi=wi, cc=cc, t0=t0: e.matmul(
                            banks[bi][:, 0:384], lhsT=wbuf[wi][:, k, cc * 128:(cc + 1) * 128],
                            rhs=xnT[:, k, t0:t0 + 384], start=(k == 0), stop=(k == 15))) for k in range(16)]
                        P.group("tensor", fns, r=[UW[wi]] + UXN, w=[BK[bi]])
                        P.op("scalar", lambda e, bi=bi, xp=xp, t0=t0: e.activation(out=xp[:, t0:t0 + 384], in_=banks[bi][:, 0:384],
                                                                                 func=AF.Copy),
                             r=[BK[bi]], w=[UXPC[xi2]])
                    P.op("vector", lambda e, xp=xp: e.tensor_tensor(out=sA[:, 1:TH], in0=xp[:, 0:TH - 1], in1=xp[:, 1:TH], op=ALU.add),
                         r=[UXPC[xi2]], w=[USA])
                    cur, ucur, oth, uoth = sA, USA, sB, USB
                    lo = 1
                    for lvl in range(g):
                        sh = 1 << lvl
                        nlo = lo + sh
                        nhi = TH - lo - sh + 1
                        P.op("vector", lambda e, cur=cur, oth=oth, sh=sh, nlo=nlo, nhi=nhi: e.tensor_tensor(
                            out=oth[:, nlo:nhi], in0=cur[:, nlo - sh:nhi - sh], in1=cur[:, nlo + sh:nhi + sh], op=ALU.add),
                            r=[ucur], w=[uoth])
                        cur, ucur, oth, uoth = oth, uoth, cur, ucur
                        lo = nlo
                    P.op("vector", lambda e, cur=cur, g=g: e.tensor_tensor(out=tmpw[:], in0=cur[:, 128:128 + T], in1=invc[:, g, :],
                                                                          op=ALU.mult), r=[ucur, UINVC], w=[UTMPW])
                    P.op("vector", lambda e, xp=xp, ch=ch: e.tensor_tensor(out=mixT[:, ch, :], in0=tmpw[:], in1=xp[:, 128:128 + T],
                                                                          op=ALU.subtract), r=[UTMPW, UXPC[xi2]], w=[UMIX[ch]])
            for g in range(4):
                for dc in range(2):
                    bi = nb()
                    fns = [(lambda e, cc=cc, g=g, dc=dc, bi=bi: e.matmul(
                        banks[bi][:, :], lhsT=poolw[:, g * 2 + cc, dc * 128:(dc + 1) * 128],
                        rhs=mixT[:, 2 * g + cc, :], start=(cc == 0), stop=(cc == 1))) for cc in range(2)]
                    P.group("tensor", fns, r=[UC, UMIX[2 * g], UMIX[2 * g + 1]], w=[BK[bi]])
                    ch = 2 * g + dc
                    P.op("scalar", lambda e, bi=bi, ch=ch: e.activation(out=yT[:, ch, :], in_=banks[bi][:, :], func=AF.Copy,
                                                                       scale=pscT[:, ch:ch + 1]), r=[BK[bi], UC], w=[UYT[ch]])

            handoff([UINVC] + UXPC + [USA, USB, UTMPW], UB2)
            handoff(UMT, [u for row in UQT for u in row])
            pieces = [("q", i) for i in range(8)] + [("k", i) for i in range(2)] + [("v", i) for i in range(2)]
            b2a = {"i": 0}
            b2b = {"i": 0}

            def nbA():
                i = b2a["i"]
                b2a["i"] = (i + 1) % 6
                return i

            def nbB():
                i = b2b["i"]
                b2b["i"] = (i + 1) % 2
                return 6 + i

            def b2_mm(kind, pi_):
                col0 = {"q": 1024, "k": 3072, "v": 3584}[kind] + pi_ * 256
                wi = load_w(w_in[:, col0:col0 + 256], 16)
                blocks = list(range(1, 1 + NB)) if kind == "q" else list(range(NBH))
                pairs = []
                for p0 in range(0, len(blocks), 2):
                    bi = nbA()
                    fns = []
                    for j in range(2):
                        b = blocks[p0 + j]
                        fns += [(lambda e, k=k, bi=bi, wi=wi, b=b, j=j: e.matmul(
                            banks[bi][:, j * 256:(j + 1) * 256], lhsT=xnT[:, k, b * 128:(b + 1) * 128], rhs=wbuf[wi][:, k, :],
                            start=(k == 0), stop=(k == 15))) for k in range(16)]
                    P.group("tensor", fns, r=[UW[wi], UXN[blocks[p0]], UXN[blocks[p0 + 1]]], w=[BK[bi]])
                    pairs.append((bi, blocks[p0], blocks[p0 + 1]))
                return pairs

            def b2_post(kind, pi_, pairs):
                if kind == "v":
                    for (bi, b0, b1) in pairs:
                        P.op("scalar", lambda e, bi=bi, b0=b0, pi_=pi_: e.activation(
                            out=v_sb[:, b0:b0 + 2, pi_ * 256:(pi_ + 1) * 256],
                            in_=banks[bi][:, :].rearrange("p (j c) -> p j c", j=2), func=AF.Copy),
                            r=[BK[bi]], w=[UV[b0], UV[b1]])
                    return
                gbc = qgbc if kind == "q" else kgbc
                nblk = 2 * len(pairs)
                for pj, (bi, b0, b1) in enumerate(pairs):
                    for j in range(2):
                        for hh in range(2):
                            c = 8 + (pj * 2 + j) * 2 + hh
                            P.op("scalar", lambda e, bi=bi, j=j, hh=hh, c=c: e.activation(
                                out=sqj[:, 0:128], in_=banks[bi][:, j * 256 + hh * 128: j * 256 + (hh + 1) * 128], func=AF.Square,
                                accum_out=stat[:, c:c + 1]), r=[BK[bi]], w=[USQJ, USTAT])
                n2 = nblk * 2
                P.op("vector", lambda e, n2=n2: e.tensor_scalar(out=stat[:, 20:20 + n2], in0=stat[:, 8:8 + n2], scalar1=1.0 / 128,
                                                                scalar2=EPS, op0=ALU.mult, op1=ALU.add), r=[USTAT], w=[USTAT])
                P.op("scalar", lambda e, n2=n2: e.activation(out=stat[:, 32:32 + n2], in_=stat[:, 20:20 + n2], func=AF.Sqrt),
                     r=[USTAT], w=[USTAT])
                P.op("vector", lambda e, n2=n2: e.reciprocal(out=stat[:, 44:44 + n2], in_=stat[:, 32:32 + n2]), r=[USTAT], w=[USTAT])
                items = []
                for pj, (bi, b0, b1) in enumerate(pairs):
                    for j, b in enumerate((b0, b1)):
                        items.append((pj * 2 + j, bi, j, b))
                for (ti, bi, j, b) in items:
                    qr3 = qr2[ti].rearrange("p (h d) -> p h d", h=2)
                    P.op("vector", lambda e, ti=ti, bi=bi, j=j, qr3=qr3: e.tensor_tensor(
                        out=qr3, in0=banks[bi][:, j * 256:(j + 1) * 256].rearrange("p (h d) -> p h d", h=2),
                        in1=stat[:, 44 + ti * 2:44 + ti * 2 + 2].unsqueeze(2).to_broadcast([128, 2, 128]), op=ALU.mult),
                        r=[BK[bi], USTAT], w=[UQR[ti]])
                for (ti, bi, j, b) in items:
                    qr3 = qr2[ti].rearrange("p (h d) -> p h d", h=2)
                    qb3 = qbf2[ti].rearrange("p (h d) -> p h d", h=2)
                    P.op("vector", lambda e, qr3=qr3, qb3=qb3, gbc=gbc: e.tensor_tensor(
                        out=qb3[:, :, 32:128], in0=qr3[:, :, 32:128],
                        in1=gbc[:, 32:128].unsqueeze(1).to_broadcast([128, 2, 96]), op=ALU.mult),
                        r=[UQR[ti], UC], w=[UQBF[ti]])
                    P.op("vector", lambda e, qr3=qr3, gbc=gbc: e.tensor_tensor(
                        out=qr3[:, :, 0:32], in0=qr3[:, :, 0:32],
                        in1=gbc[:, 0:32].unsqueeze(1).to_broadcast([128, 2, 32]), op=ALU.mult),
                        r=[UC], w=[UQR[ti]])
                for (ti, bi, j, b) in items:
                    qr3 = qr2[ti].rearrange("p (h d) -> p h d", h=2)
                    P.op("vector", lambda e, ti=ti, qr3=qr3, b=b: e.tensor_tensor(
                        out=rotA2[ti], in0=qr3[:, :, 0:32], in1=ropet[:, b, 0:32].unsqueeze(1).to_broadcast([128, 2, 32]), op=ALU.mult),
                        r=[UQR[ti], UROPE], w=[UROT[ti]])
                    P.op("vector", lambda e, ti=ti, qr3=qr3, b=b: e.tensor_tensor(
                        out=rotB2[ti][:, :, 0:16], in0=qr3[:, :, 16:32],
                        in1=ropet[:, b, 32:48].unsqueeze(1).to_broadcast([128, 2, 16]), op=ALU.mult),
                        r=[UQR[ti], UROPE], wacc=[UROT[ti]])
                    P.op("vector", lambda e, ti=ti, qr3=qr3, b=b: e.tensor_tensor(
                        out=rotB2[ti][:, :, 16:32], in0=qr3[:, :, 0:16],
                        in1=ropet[:, b, 48:64].unsqueeze(1).to_broadcast([128, 2, 16]), op=ALU.mult),
                        r=[UQR[ti], UROPE], wacc=[UROT[ti]])
                for (ti, bi, j, b) in items:
                    qb3 = qbf2[ti].rearrange("p (h d) -> p h d", h=2)
                    P.op("vector", lambda e, ti=ti, qb3=qb3: e.tensor_tensor(out=qb3[:, :, 0:32], in0=rotA2[ti], in1=rotB2[ti], op=ALU.add),
                         r=[UROT[ti]], wacc=[UQBF[ti]])
                for pj, (bi, b0, b1) in enumerate(pairs):
                    bj = nbB()
                    fns = []
                    for j in range(2):
                        ti = pj * 2 + j
                        for hh in range(2):
                            fns.append(lambda e, ti=ti, hh=hh, j=j, bj=bj: e.transpose(
                                out=banks_bf[bj][:, hh * 256 + j * 128: hh * 256 + (j + 1) * 128],
                                in_=qbf2[ti][:, hh * 128:(hh + 1) * 128], identity=ident_b[:]))
                    P.group("tensor", fns, r=[UQBF[pj * 2], UQBF[pj * 2 + 1], UC], w=[BK[bj]])
                    src = banks_bf[bj][:, 0:512].rearrange("p (h t) -> p h t", h=2)
                    if kind == "q":
                        qb = b0 - 1
                        P.op("scalar", lambda e, src=src, pi_=pi_, qb=qb: e.activation(
                            out=qT[:, pi_ * 2:(pi_ + 1) * 2, qb * 128:(qb + 2) * 128], in_=src, func=AF.Copy),
                            r=[BK[bj]], wacc=[UQT[qb][pi_ // 2], UQT[qb + 1][pi_ // 2]])
                    else:
                        P.op("scalar", lambda e, src=src, pi_=pi_, b0=b0: e.activation(
                            out=kT[:, pi_ * 2:(pi_ + 1) * 2, b0 * 128:(b0 + 2) * 128], in_=src, func=AF.Copy),
                            r=[BK[bj]], wacc=[UKT[b0], UKT[b1]])

            for row in UQT:
                for u in row:
                    u.lw = list(u.rd) + list(u.lw)
                    u.rd = []
            for u in UKT:
                u.lw = list(u.rd) + list(u.lw)
                u.rd = []
            prev = None
            for (kind, pi_) in pieces:
                pairs = b2_mm(kind, pi_)
                if prev is not None:
                    b2_post(*prev)
                prev = (kind, pi_, pairs)
            b2_post(*prev)

            handoff(UMIX, [u for row in UOT for u in row])
            scale = 128.0 ** -0.5
            pslot = {"i": 0}
            for qb in range(NB):
                for kh in range(4):
                    pi = pslot["i"]
                    pslot["i"] = 1 - pi
                    for jc in range(3):
                        kb = qb + jc
                        bi = nb()
                        fns = [lambda e, bi=bi, kb=kb, kh=kh, qb=qb, jc=jc: e.matmul(
                            banks[bi][:, :].rearrange("p (h q) -> p h q", h=4), lhsT=kT[:, kh, kb * 128:(kb + 1) * 128],
                            rhs=qT[:, kh * 4:(kh + 1) * 4, qb * 128:(qb + 1) * 128], start=True, stop=(jc == 1))]
                        if jc != 1:
                            mi = 0 if jc == 0 else 1
                            fns.append(lambda e, bi=bi, mi=mi: e.matmul(banks[bi][:, :], lhsT=ident_b[:], rhs=masks[:, mi, :],
                                                                      start=False, stop=True))
                        P.group("tensor", fns, r=[UKT[kb], UC] + UQT[qb], w=[BK[bi]])
                        col = s * NBH + kb
                        P.op("scalar", lambda e, bi=bi, pi=pi, jc=jc, col=col: e.activation(
                            out=pT[pi][:, jc, :], in_=banks[bi][:, :], func=AF.Exp, bias=kbias[:, col:col + 1], scale=scale),
                            r=[BK[bi], UC], w=[UPT[pi][jc]])
                    bo = nb()
                    fns = [(lambda e, jc=jc, bo=bo, pi=pi, qb=qb, kh=kh: e.matmul(
                        banks[bo][:, :], lhsT=v_sb[:, qb + jc, kh * 128:(kh + 1) * 128], rhs=pT[pi][:, jc, :],
                        start=(jc == 0), stop=(jc == 2))) for jc in range(3)]
                    P.group("tensor", fns, r=[UV[qb], UV[qb + 1], UV[qb + 2]] + UPT[pi], w=[BK[bo]])
                    bd = nb()
                    fns = [(lambda e, jc=jc, bd=bd, pi=pi: e.matmul(
                        banks[bd][:, :], lhsT=ones_b[:], rhs=pT[pi][:, jc, :], start=(jc == 0), stop=(jc == 2))) for jc in range(3)]
                    P.group("tensor", fns, r=[UC] + UPT[pi], w=[BK[bd]])
                    dd3 = ddt.rearrange("p (h q) -> p h q", h=4)
                    P.op("vector", lambda e, bd=bd, kh=kh, dd3=dd3: e.tensor_tensor(
                        out=dd3, in0=banks[bd][:, :].rearrange("p (h q) -> p h q", h=4),
                        in1=esink[:, kh * 4:(kh + 1) * 4].unsqueeze(2).to_broadcast([128, 4, 128]), op=ALU.add),
                        r=[BK[bd], UC], w=[UDD])
                    P.op("vector", lambda e: e.reciprocal(out=ddt, in_=ddt), r=[UDD], w=[UDD])
                    P.op("vector", lambda e, bo=bo, qb=qb, kh=kh, dd3=dd3: e.tensor_tensor(
                        out=oT[:, kh * 4:(kh + 1) * 4, qb * 128:(qb + 1) * 128],
                        in0=banks[bo][:, :].rearrange("p (h q) -> p h q", h=4), in1=dd3, op=ALU.mult),
                        r=[BK[bo], UDD], w=[UOT[qb][kh]])

            handoff([UINVC] + UXPC + [USA, USB, UTMPW, UHNT] + UB2, USG + UTACC)
            handoff([u for row in UQT for u in row], UMT)
            allOT = [u for row in UOT for u in row]
            xn_main = lambda k: xnT[:, k, 128:128 + T]
            for cg in range(8):
                wi = load_w(w_in[:, 4096 + cg * 256: 4096 + (cg + 1) * 256], 16)
                for cc in range(2):
                    bi = nb()
                    fns = [(lambda e, k=k, bi=bi, wi=wi, cc=cc: e.matmul(
                        banks[bi][:, :], lhsT=wbuf[wi][:, k, cc * 128:(cc + 1) * 128], rhs=xn_main(k),
                        start=(k == 0), stop=(k == 15))) for k in range(16)]
                    P.group("tensor", fns, r=[UW[wi]] + UXN, w=[BK[bi]])
                    P.op("scalar", lambda e, bi=bi, cc=cc: e.activation(out=sg[:, cc, :], in_=banks[bi][:, :], func=AF.Sigmoid),
                         r=[BK[bi]], w=[USG[cc]])
                wi = load_w(pool_proj[:, cg * 256:(cg + 1) * 256], 8)
                for cc in range(2):
                    bi = nb()
                    fns = [(lambda e, k=k, bi=bi, wi=wi, cc=cc: e.matmul(
                        banks[bi][:, :], lhsT=wbuf[wi][:, k, cc * 128:(cc + 1) * 128], rhs=yT[:, k, :],
                        start=(k == 0), stop=(k == 7))) for k in range(8)]
                    P.group("tensor", fns, r=[UW[wi]] + UYT, w=[BK[bi]])
                    P.op("vector", lambda e, bi=bi, cc=cc: e.tensor_tensor(out=tacc[:, cc, :], in0=banks[bi][:, :], in1=sg[:, cc, :],
                                                                        op=ALU.mult), r=[BK[bi], USG[cc]], w=[UTACC[cc]])
                wi = load_w(w_in[:, 6144 + cg * 256: 6144 + (cg + 1) * 256], 16)
                for cc in range(2):
                    bi = nb()
                    fns = [(lambda e, k=k, bi=bi, wi=wi, cc=cc: e.matmul(
                        banks[bi][:, :], lhsT=wbuf[wi][:, k, cc * 128:(cc + 1) * 128], rhs=xn_main(k),
                        start=(k == 0), stop=(k == 15))) for k in range(16)]
                    P.group("tensor", fns, r=[UW[wi]] + UXN, w=[BK[bi]])
                    P.op("scalar", lambda e, bi=bi, cc=cc: e.activation(out=sg[:, cc, :], in_=banks[bi][:, :], func=AF.Sigmoid),
                         r=[BK[bi]], w=[USG[cc]])
                wi = load_w(attn_proj[:, cg * 256:(cg + 1) * 256], 16)
                for cc in range(2):
                    bi = nb()
                    fns = [(lambda e, k=k, bi=bi, wi=wi, cc=cc: e.matmul(
                        banks[bi][:, :], lhsT=wbuf[wi][:, k, cc * 128:(cc + 1) * 128], rhs=oT[:, k, :],
                        start=(k == 0), stop=(k == 15))) for k in range(16)]
                    P.group("tensor", fns, r=[UW[wi]] + allOT, w=[BK[bi]])
                    P.op("vector", lambda e, bi=bi, cc=cc: e.tensor_tensor(out=sg[:, cc, :], in0=banks[bi][:, :], in1=sg[:, cc, :],
                                                                        op=ALU.mult), r=[BK[bi], USG[cc]], w=[USG[cc]])
                    c = cg * 2 + cc
                    P.op("vector", lambda e, cc=cc, c=c: e.tensor_tensor(out=mT[:, c, :], in0=sg[:, cc, :], in1=tacc[:, cc, :],
                                                                      op=ALU.add), r=[USG[cc], UTACC[cc]], w=[UMT[c]])

            handoff([u for row in UPT for u in row] + [USQJ], UXB[2:4])
            handoff(USG + UTACC, [UHNT])
            for b in range(NB):
                P.dma("sync", lambda e, s=s, b=b: e.dma_start(out=hbuf[b][:], in_=x_in[s, (b + 1) * 128:(b + 2) * 128, :]),
                      UXB[b], w=[UXB[b]])
            for cg in range(8):
                wi = load_w(w_out[:, cg * 256:(cg + 1) * 256], 16)
                for b0 in (0, 2):
                    bi = nb()
                    fns = []
                    for j in range(2):
                        b = b0 + j
                        fns += [(lambda e, k=k, bi=bi, wi=wi, b=b, j=j: e.matmul(
                            banks[bi][:, j * 256:(j + 1) * 256], lhsT=mT[:, k, b * 128:(b + 1) * 128], rhs=wbuf[wi][:, k, :],
                            start=(k == 0), stop=(k == 15))) for k in range(16)]
                    P.group("tensor", fns, r=[UW[wi]] + UMT, w=[BK[bi]])
                    for j in range(2):
                        b = b0 + j
                        P.op("vector", lambda e, bi=bi, b=b, cg=cg, j=j: e.tensor_tensor(
                            out=hbuf[b][:, cg * 256:(cg + 1) * 256], in0=banks[bi][:, j * 256:(j + 1) * 256],
                            in1=hbuf[b][:, cg * 256:(cg + 1) * 256], op=ALU.add), r=[BK[bi]], w=[UXB[b]])
            for b in range(NB):
                tb = s * NB + b
                hb = hbuf[b]
                P.dma("sync", lambda e, tb=tb, hb=hb: e.dma_start(out=y_out[tb * 128:(tb + 1) * 128, :], in_=hb[:]),
                      UXB[b], r=[UXB[b]], w=[UYB[tb]])
                if PHASES < 2:
                    continue
                P.op("scalar", lambda e, hb=hb: e.activation(out=xs[:], in_=hb[:], func=AF.Square, accum_out=stat[:, 0:1]),
                     r=[UXB[b]], w=[UXS, USTAT])
                P.op("vector", lambda e: e.tensor_scalar(out=stat[:, 1:2], in0=stat[:, 0:1], scalar1=1.0 / D, scalar2=EPS,
                                                         op0=ALU.mult, op1=ALU.add), r=[USTAT], w=[USTAT])
                P.op("scalar", lambda e: e.activation(out=stat[:, 2:3], in_=stat[:, 1:2], func=AF.Sqrt), r=[USTAT], w=[USTAT])
                P.op("vector", lambda e: e.reciprocal(out=stat[:, 3:4], in_=stat[:, 2:3]), r=[USTAT], w=[USTAT])
                P.op("vector", lambda e, hb=hb: e.scalar_tensor_tensor(out=hb[:], in0=hb[:], scalar=stat[:, 3:4], in1=g2bc[:],
                                                                      op0=ALU.mult, op1=ALU.mult), r=[USTAT, UC], w=[UXB[b]])
                P.op("scalar", lambda e, hb=hb: e.activation(out=xs[:], in_=hb[:], func=AF.Copy), r=[UXB[b]], w=[UXS])
                for q4 in range(4):
                    bi = nb()
                    fns = [(lambda e, c=c, bi=bi, q4=q4, hb=hb: e.transpose(
                        out=banks[bi][:, c * 128:(c + 1) * 128], in_=hb[:, (q4 * 4 + c) * 128:(q4 * 4 + c + 1) * 128],
                        identity=ident_f[:])) for c in range(4)]
                    P.group("tensor", fns, r=[UXB[b], UC], w=[BK[bi]])
                    P.op("scalar", lambda e, bi=bi, q4=q4: e.activation(
                        out=hnT[:, q4 * 4:(q4 + 1) * 4, :], in_=banks[bi][:, :].rearrange("p (c t) -> p c t", c=4), func=AF.Copy),
                        r=[BK[bi]], w=[UHNT])
                bi = nb()
                fns = [(lambda e, k=k, bi=bi: e.matmul(banks[bi][:, 0:36], lhsT=hnT[:, k, :], rhs=wr[:, k, :],
                                                       start=(k == 0), stop=(k == 15))) for k in range(16)]
                P.group("tensor", fns, r=[UHNT, UC], w=[BK[bi]])
                L = rtmp
                P.op("vector", lambda e, bi=bi: e.tensor_tensor(out=L[:, 0:36], in0=banks[bi][:, 0:36], in1=brbc[:], op=ALU.add),
                     r=[BK[bi], UC], w=[URT])
                P.op("vector", lambda e: e.memset(L[:, 40:48], -1e30), r=[], w=[URT])
                P.op("vector", lambda e: e.tensor_copy(out=L[:, 40:44], in_=L[:, 0:4]), r=[URT], w=[URT])
                P.op("vector", lambda e: e.max(out=L[:, 48:56], in_=L[:, 40:48]), r=[URT], w=[URT])
                P.op("vector", lambda e: e.tensor_scalar(out=L[:, 56:60], in0=L[:, 0:4], scalar1=L[:, 48:49], scalar2=None,
                                                         op0=ALU.is_equal), r=[URT], w=[URT])
                P.op("vector", lambda e: e.tensor_scalar(out=L[:, 240:241], in0=L[:, 48:49], scalar1=-1.0, scalar2=None, op0=ALU.mult),
                     r=[URT], w=[URT])
                P.op("scalar", lambda e: e.activation(out=L[:, 244:248], in_=L[:, 0:4], func=AF.Exp, bias=L[:, 240:241], scale=1.0,
                                                      accum_out=L[:, 241:242]), r=[URT], w=[URT])
                P.op("vector", lambda e: e.reciprocal(out=L[:, 242:243], in_=L[:, 241:242]), r=[URT], w=[URT])
                P.op("vector", lambda e: e.tensor_scalar(out=L[:, 60:64], in0=L[:, 56:60], scalar1=-1.0, scalar2=1e30,
                                                         op0=ALU.add, op1=ALU.mult), r=[URT], w=[URT])
                P.op("vector", lambda e: e.tensor_tensor(
                    out=L[:, 64:96].rearrange("p (g e) -> p g e", g=4), in0=L[:, 4:36].rearrange("p (g e) -> p g e", g=4),
                    in1=L[:, 60:64].unsqueeze(2).to_broadcast([128, 4, 8]), op=ALU.add), r=[URT], w=[URT])
                P.op("vector", lambda e: e.max(out=L[:, 96:104], in_=L[:, 64:96]), r=[URT], w=[URT])
                P.op("vector", lambda e: e.tensor_scalar(out=L[:, 104:136], in0=L[:, 64:96], scalar1=L[:, 96:97], scalar2=None,
                                                         op0=ALU.is_equal), r=[URT], w=[URT])
                P.op("vector", lambda e: e.tensor_scalar(out=L[:, 136:168], in0=L[:, 64:96], scalar1=L[:, 97:98], scalar2=None,
                                                         op0=ALU.is_equal), r=[URT], w=[URT])
                P.op("vector", lambda e: e.tensor_tensor(out=L[:, 248:249], in0=L[:, 97:98], in1=L[:, 96:97], op=ALU.subtract),
                     r=[URT], w=[URT])
                P.op("scalar", lambda e: e.activation(out=L[:, 249:250], in_=L[:, 248:249], func=AF.Exp), r=[URT], w=[URT])
                P.op("vector", lambda e: e.tensor_scalar(out=L[:, 250:251], in0=L[:, 249:250], scalar1=1.0, scalar2=None, op0=ALU.add),
                     r=[URT], w=[URT])
                P.op("vector", lambda e: e.reciprocal(out=L[:, 251:252], in_=L[:, 250:251]), r=[URT], w=[URT])
                P.op("vector", lambda e: e.tensor_tensor(out=L[:, 252:253], in0=L[:, 249:250], in1=L[:, 251:252], op=ALU.mult),
                     r=[URT], w=[URT])
                P.op("vector", lambda e, tb=tb: e.tensor_scalar(out=twt[:, tb, 0:1], in0=L[:, 251:252], scalar1=L[:, 242:243],
                                                                scalar2=None, op0=ALU.mult), r=[URT], w=[UTS[tb]])
                P.op("vector", lambda e, tb=tb: e.tensor_scalar(out=twt[:, tb, 1:2], in0=L[:, 252:253], scalar1=L[:, 242:243],
                                                                scalar2=None, op0=ALU.mult), r=[URT], wacc=[UTS[tb]])
                P.op("vector", lambda e: e.tensor_tensor(out=rtb[:, 0:32], in0=L[:, 104:136], in1=L[:, 136:168], op=ALU.add),
                     r=[URT], w=[URTB])
                bi = nb()
                P.group("tensor", [lambda e, bi=bi: e.matmul(banks[bi][:, 0:32], lhsT=lstrict[:], rhs=rtb[:, 0:32], start=True, stop=True),
                                   lambda e, bi=bi: e.matmul(banks[bi][:, 32:64], lhsT=ones_b[:], rhs=rtb[:, 0:32], start=True, stop=True)],
                        r=[URTB, UC], w=[BK[bi]])
                P.op("vector", lambda e, bi=bi: e.tensor_tensor(out=L[:, 168:200], in0=banks[bi][:, 0:32], in1=basebc[:], op=ALU.add),
                     r=[BK[bi], UBASE], w=[URT])
                P.op("vector", lambda e, bi=bi: e.tensor_tensor(out=basebc[:], in0=banks[bi][:, 32:64], in1=basebc[:], op=ALU.add),
                     r=[BK[bi]], w=[UBASE])
                for kk in range(2):
                    oh = L[:, 104 + 32 * kk:136 + 32 * kk]
                    P.op("vector", lambda e, oh=oh: e.tensor_tensor(out=L[:, 200:232], in0=oh, in1=L[:, 168:200], op=ALU.mult),
                         r=[URT], w=[URT])
                    P.op("vector", lambda e: e.tensor_reduce(out=L[:, 253:254], in_=L[:, 200:232], axis=AX.X, op=ALU.add),
                         r=[URT], w=[URT])
                    P.op("vector", lambda e, oh=oh: e.tensor_tensor(out=L[:, 200:232], in0=oh, in1=ecap[:], op=ALU.mult),
                         r=[URT, UC], w=[URT])
                    P.op("vector", lambda e: e.tensor_reduce(out=L[:, 254:255], in_=L[:, 200:232], axis=AX.X, op=ALU.add),
                         r=[URT], w=[URT])
                    P.op("vector", lambda e: e.tensor_scalar(out=L[:, 255:256], in0=L[:, 253:254], scalar1=float(CAP), scalar2=None,
                                                             op0=ALU.is_lt), r=[URT], w=[URT])
                    P.op("vector", lambda e: e.tensor_tensor(out=L[:, 256:257], in0=L[:, 253:254], in1=L[:, 254:255], op=ALU.add),
                         r=[URT], w=[URT])
                    P.op("vector", lambda e: e.tensor_tensor(out=L[:, 256:257], in0=L[:, 256:257], in1=pidx[:], op=ALU.subtract),
                         r=[URT, UC], w=[URT])
                    P.op("vector", lambda e: e.scalar_tensor_tensor(out=L[:, 257:258], in0=L[:, 256:257], scalar=L[:, 255:256],
                                                                    in1=pidx[:], op0=ALU.mult, op1=ALU.add), r=[URT, UC], w=[URT])
                    P.op("vector", lambda e, tb=tb, kk=kk: e.tensor_copy(out=tslot[:, tb, kk:kk + 1], in_=L[:, 257:258]),
                         r=[URT], wacc=[UTS[tb]])
                for kk in range(2):
                    P.dma("gpsimd", lambda e, tb=tb, kk=kk: e.indirect_dma_start(
                        out=xg, out_offset=bass.IndirectOffsetOnAxis(ap=tslot[:, tb, kk:kk + 1], axis=0),
                        in_=xs[:], in_offset=None), UXS, r=[UXS, UTS[tb], UXGZ], wacc=[UXG])

        P.barrier()
        p1.close()

        if PHASES >= 2:
            p2 = contextlib.ExitStack()
            sb2 = lambda name, shape, d: sb(name, shape, d, p2)
            xgt = [sb2("xgt%d" % i, [128, D], BF16) for i in range(4)]
            xgT = sb2("xgT", [128, 16, CAP], BF16)
            wgu = [sb2("wgu%d" % i, [128, 16, 512], BF16) for i in range(4)]
            wd = [sb2("wd%d" % i, [128, 8, 1024], BF16) for i in range(2)]
            hT = sb2("hT", [128, 8, CAP], BF16)
            sil = [sb2("sil%d" % i, [128, CAP], F32) for i in range(2)]
            yet = [sb2("yet%d" % i, [128, D], F32) for i in range(4)]
            zt = sb2("zt", [128, D], F32)
            UXGT = [U("xgt%d" % i) for i in range(4)]
            UXGTT = [U("xgT%d" % i) for i in range(4)]
            UWGU = [U("wgu%d" % i) for i in range(4)]
            UWD = [U("wd%d" % i) for i in range(2)]
            UHT = [U("hT%d" % f) for f in range(8)]
            USIL = [U("sil%d" % i) for i in range(2)]
            UYET = [U("yet%d" % i) for i in range(4)]
            UYE = U("ye")
            UZT = U("zt")
            P.op("vector", lambda e: e.memset(zt[:], 0.0), w=[UZT])
            P.dma("sync", lambda e: e.dma_start(out=ye[NSLOT:NSLOT + 128, :], in_=zt[:]), UZT, r=[UZT], wacc=[UYE])
            gslot = {"i": 0}
            dslot = {"i": 0}
            sslot = {"i": 0}
            for ex in range(NE):
                for sbk in range(4):
                    r0 = ex * CAP + sbk * 128
                    P.dma("sync", lambda e, r0=r0, sbk=sbk: e.dma_start(out=xgt[sbk][:], in_=xg[r0:r0 + 128, :]),
                          UXGT[sbk], r=[UXG], w=[UXGT[sbk]])
                    for half in range(2):
                        bi = nb()
                        fns = [(lambda e, c=c, bi=bi, half=half, sbk=sbk: e.transpose(
                            out=banks_bf[bi][:, c * 128:(c + 1) * 128],
                            in_=xgt[sbk][:, (half * 8 + c) * 128:(half * 8 + c + 1) * 128], identity=ident_b[:])) for c in range(8)]
                        P.group("tensor", fns, r=[UXGT[sbk], UC], w=[BK[bi]])
                        P.op("vector", lambda e, bi=bi, half=half, sbk=sbk: e.tensor_copy(
                            out=xgT[:, half * 8:(half + 1) * 8, sbk * 128:(sbk + 1) * 128],
                            in_=banks_bf[bi][:].rearrange("p (c t) -> p c t", c=8)), r=[BK[bi]], w=[UXGTT[sbk]])
                for pc in range(2):
                    gi = gslot["i"]
                    ui = (gi + 1) % 4
                    gslot["i"] = (gi + 2) % 4
                    P.dma("gpsimd", lambda e, ex=ex, pc=pc, gi=gi: e.dma_start(
                        out=wgu[gi][:], in_=w_gate[ex].rearrange("(k p) n -> p k n", p=128)[:, :, pc * 512:(pc + 1) * 512]),
                        UWGU[gi], w=[UWGU[gi]])
                    P.dma("gpsimd", lambda e, ex=ex, pc=pc, ui=ui: e.dma_start(
                        out=wgu[ui][:], in_=w_up[ex].rearrange("(k p) n -> p k n", p=128)[:, :, pc * 512:(pc + 1) * 512]),
                        UWGU[ui], w=[UWGU[ui]])
                    for fc in range(4):
                        f = pc * 4 + fc
                        bg = nb()
                        fns = [(lambda e, k=k, bg=bg, gi=gi, fc=fc: e.matmul(
                            banks[bg][:, :], lhsT=wgu[gi][:, k, fc * 128:(fc + 1) * 128], rhs=xgT[:, k, :],
                            start=(k == 0), stop=(k == 15))) for k in range(16)]
                        P.group("tensor", fns, r=[UWGU[gi]] + UXGTT, w=[BK[bg]])
                        bu = nb()
                        fns = [(lambda e, k=k, bu=bu, ui=ui, fc=fc: e.matmul(
                            banks[bu][:, :], lhsT=wgu[ui][:, k, fc * 128:(fc + 1) * 128], rhs=xgT[:, k, :],
                            start=(k == 0), stop=(k == 15))) for k in range(16)]
                        P.group("tensor", fns, r=[UWGU[ui]] + UXGTT, w=[BK[bu]])
                        si = sslot["i"]
                        sslot["i"] = 1 - si
                        P.op("scalar", lambda e, bg=bg, si=si: e.activation(out=sil[si][:], in_=banks[bg][:, :], func=AF.Silu),
                             r=[BK[bg]], w=[USIL[si]])
                        P.op("vector", lambda e, bu=bu, si=si, f=f: e.tensor_tensor(out=hT[:, f, :], in0=banks[bu][:, :], in1=sil[si][:],
                                                                                 op=ALU.mult), r=[BK[bu], USIL[si]], w=[UHT[f]])
                for cg2 in range(2):
                    di = dslot["i"]
                    dslot["i"] = 1 - di
                    P.dma("gpsimd", lambda e, ex=ex, cg2=cg2, di=di: e.dma_start(
                        out=wd[di][:], in_=w_down[ex].rearrange("(k p) n -> p k n", p=128)[:, :, cg2 * 1024:(cg2 + 1) * 1024]),
                        UWD[di], w=[UWD[di]])
                    for cgh in range(2):
                        cg = cg2 * 2 + cgh
                        for sbk in range(4):
                            bi = nb()
                            fns = [(lambda e, f=f, bi=bi, di=di, sbk=sbk, cgh=cgh: e.matmul(
                                banks[bi][:, :], lhsT=hT[:, f, sbk * 128:(sbk + 1) * 128], rhs=wd[di][:, f, cgh * 512:(cgh + 1) * 512],
                                start=(f == 0), stop=(f == 7))) for f in range(8)]
                            P.group("tensor", fns, r=[UWD[di]] + UHT, w=[BK[bi]])
                            P.op("scalar", lambda e, bi=bi, sbk=sbk, cg=cg: e.activation(
                                out=yet[sbk][:, cg * 512:(cg + 1) * 512], in_=banks[bi][:, :], func=AF.Copy),
                                r=[BK[bi]], w=[UYET[sbk]])
                for sbk in range(4):
                    r0 = ex * CAP + sbk * 128
                    P.dma("sync", lambda e, r0=r0, sbk=sbk: e.dma_start(out=ye[r0:r0 + 128, :], in_=yet[sbk][:]),
                          UYET[sbk], r=[UYET[sbk]], wacc=[UYE])
            P.barrier()
            p2.close()

            p3 = contextlib.ExitStack()
            sb3 = lambda name, shape, d: sb(name, shape, d, p3)
            hb3 = [sb3("hb3_%d" % i, [128, D], F32) for i in range(2)]
            r1 = [sb3("r1_%d" % i, [128, D], F32) for i in range(2)]
            r2 = [sb3("r2_%d" % i, [128, D], F32) for i in range(2)]
            UH3 = [U("h3_%d" % i) for i in range(2)]
            UR1 = [U("r1_%d" % i) for i in range(2)]
            UR2 = [U("r2_%d" % i) for i in range(2)]
            for tb in range(NST * NB):
                i = tb % 2
                P.dma("sync", lambda e, tb=tb, i=i: e.dma_start(out=hb3[i][:], in_=y_out[tb * 128:(tb + 1) * 128, :]),
                      UH3[i], r=[UYB[tb]], w=[UH3[i]])
                P.dma("gpsimd", lambda e, tb=tb, i=i: e.indirect_dma_start(
                    out=r1[i][:], out_offset=None, in_=ye,
                    in_offset=bass.IndirectOffsetOnAxis(ap=tslot[:, tb, 0:1], axis=0)), UR1[i], r=[UYE, UTS[tb]], w=[UR1[i]])
                P.dma("gpsimd", lambda e, tb=tb, i=i: e.indirect_dma_start(
                    out=r2[i][:], out_offset=None, in_=ye,
                    in_offset=bass.IndirectOffsetOnAxis(ap=tslot[:, tb, 1:2], axis=0)), UR2[i], r=[UYE, UTS[tb]], w=[UR2[i]])
                P.op("vector", lambda e, tb=tb, i=i: e.scalar_tensor_tensor(
                    out=hb3[i][:], in0=r1[i][:], scalar=twt[:, tb, 0:1], in1=hb3[i][:], op0=ALU.mult, op1=ALU.add),
                    r=[UR1[i], UTS[tb]], w=[UH3[i]])
                P.op("vector", lambda e, tb=tb, i=i: e.scalar_tensor_tensor(
                    out=hb3[i][:], in0=r2[i][:], scalar=twt[:, tb, 1:2], in1=hb3[i][:], op0=ALU.mult, op1=ALU.add),
                    r=[UR2[i], UTS[tb]], w=[UH3[i]])
                P.dma("sync", lambda e, tb=tb, i=i: e.dma_start(out=y_out[tb * 128:(tb + 1) * 128, :], in_=hb3[i][:]),
                      UH3[i], r=[UH3[i]], w=[UYB[tb]])
            P.barrier()
            p3.close()

        block = st.enter_context(nc.Block())
        P.emit(block)
    return nc


def _core_layout(c):
    b = c // 4
    qtr = c % 4
    sts = []
    for i in range(2):
        sts.append(("p", b, qtr * 1024 + i * T, 4096))
    for i in range(8):
        sts.append(("s", b, qtr * 4096 + i * T, 16384))
    return sts


def _rope_table(pos):
    half = 16
    inv = (np.float32(500000.0) ** (-np.arange(half, dtype=np.float32) / np.float32(half))).astype(np.float32)
    ang = (pos.astype(np.float32)[:, None] * inv[None, :]).astype(np.float32)
    cos = np.cos(ang).astype(np.float32)
    sin = np.sin(ang).astype(np.float32)
    return np.concatenate([cos, cos, -sin, sin], axis=1)


_NC_CACHE = {}


def kernel(x_prompt, x_sample, norm1_g, w_in, pool_w, pool_scale, pool_proj, q_norm_g, k_norm_g, sink,
           attn_proj, w_out, norm2_g, router_group_w, router_group_b, router_expert_w, router_expert_b,
           w_gate, w_up, w_down):
    f32 = np.float32
    xp = np.asarray(x_prompt, f32)
    xsm = np.asarray(x_sample, f32)
    bc = lambda v, n: np.ascontiguousarray(np.broadcast_to(np.asarray(v, f32).reshape(1, n), (128, n)))
    shared = {
        "w_in": np.ascontiguousarray(np.asarray(w_in, f32)[0]),
        "pool_w": np.ascontiguousarray(np.asarray(pool_w, f32)[0]),
        "pool_proj": np.ascontiguousarray(np.asarray(pool_proj, f32)[0]),
        "attn_proj": np.ascontiguousarray(np.asarray(attn_proj, f32)[0]),
        "w_out": np.ascontiguousarray(np.asarray(w_out, f32)[0]),
        "w_gate": np.ascontiguousarray(np.asarray(w_gate, f32)[0]),
        "w_up": np.ascontiguousarray(np.asarray(w_up, f32)[0]),
        "w_down": np.ascontiguousarray(np.asarray(w_down, f32)[0]),
        "g1T": np.ascontiguousarray(np.asarray(norm1_g, f32)[0].reshape(16, 128).T),
        "g2bc": bc(np.asarray(norm2_g)[0], D),
        "pscT": np.ascontiguousarray(np.asarray(pool_scale, f32)[0].reshape(8, 128).T),
        "qgbc": bc(np.asarray(q_norm_g)[0], 128),
        "kgbc": bc(np.asarray(k_norm_g)[0], 128),
        "sinkbc": bc(np.asarray(sink)[0], 16),
        "wr": np.ascontiguousarray(np.concatenate([np.asarray(router_group_w, f32)[0], np.asarray(router_expert_w, f32)[0]], axis=1)),
        "brbc": bc(np.concatenate([np.asarray(router_group_b, f32)[0], np.asarray(router_expert_b, f32)[0]]), 36),
        "ident": np.eye(128, dtype=f32),
        "lstrict": np.triu(np.ones((128, 128), f32), 1),
        "ones": np.ones((128, 128), f32),
        "ecap": bc(np.arange(NE, dtype=f32) * CAP, NE),
        "pidx": (DUMP + np.arange(128, dtype=f32)).reshape(128, 1),
    }
    jj = np.arange(128)[:, None]
    qq = np.arange(128)[None, :]
    mL = np.where(qq <= jj, 0.0, NEG).astype(f32)
    mR = np.where(jj <= qq, 0.0, NEG).astype(f32)
    shared["masks"] = np.ascontiguousarray(np.concatenate([np.tile(mL, (1, 4)), np.tile(mR, (1, 4))], axis=1))

    in_maps = []
    for c in range(NCORE):
        sts = _core_layout(c)
        x_in = np.zeros((NST, TH, D), f32)
        rope = np.zeros((NST, TH, 64), f32)
        kb = np.zeros((128, NST * NBH), f32)
        invc = np.zeros((NST, 128, 4 * T), f32)
        for si, (which, b, s0, S) in enumerate(sts):
            src = xp[b] if which == "p" else xsm[b]
            lo = s0 - 128
            hi = s0 + T + 128
            a = max(lo, 0)
            z = min(hi, S)
            x_in[si, a - lo:z - lo] = src[a:z]
            pos = np.arange(lo, hi)
            rope[si] = _rope_table(np.clip(pos, 0, S - 1))
            for blk in range(NBH):
                p0 = lo + blk * 128
                if p0 < 0 or p0 >= S:
                    kb[:, si * NBH + blk] = NEG
            t = np.arange(s0, s0 + T)
            for g, w in enumerate((2, 4, 8, 16)):
                h = w // 2
                cnt = np.clip(t + h, 0, S) - np.clip(t - h, 0, S)
                invc[si, :, g * T:(g + 1) * T] = (1.0 / cnt.astype(f32))[None, :]
        m = dict(shared)
        m.update({"x_in": x_in, "rope": rope, "kbias": kb, "invcnt": invc})
        in_maps.append(m)

    if "nc" not in _NC_CACHE:
        _NC_CACHE["nc"] = build_program()
    nc = _NC_CACHE["nc"]
    res = run_bass_kernel_spmd(nc, in_maps, core_ids=list(range(NCORE)))
    y_prompt = np.zeros((2, 4096, D), f32)
    y_sample = np.zeros((2, 16384, D), f32)
    for c in range(NCORE):
        y = np.asarray(res.results[c]["y"], f32)
        b = c // 4
        qtr = c % 4
        y_prompt[b, qtr * 1024:(qtr + 1) * 1024] = y[0:1024]
        y_sample[b, qtr * 4096:(qtr + 1) * 4096] = y[1024:5120]
    return (y_prompt, y_sample)
```

```python
import contextlib
import numpy as np
import concourse.bass as bass
import concourse.mybir as mybir
from concourse.bass_utils import run_bass_kernel_spmd

F32 = mybir.dt.float32
BF16 = mybir.dt.bfloat16
I32 = mybir.dt.int32
AF = mybir.ActivationFunctionType
ALU = mybir.AluOpType
AX = mybir.AxisListType

ENGS = ("sync", "tensor", "scalar", "vector", "gpsimd")

D = 2048
NCORE = 8
NB = 4
NBH = NB + 2
T = NB * 128
TH = NBH * 128
NST = 10
NTOK = NST * T
NE = 32
CAP = 512
NSLOT = NE * CAP
DUMP = NSLOT
DFF = 1024
EPS = 1e-6
NEG = -30000.0
import os
PHASES = int(os.environ.get('MK_PHASES', '3'))


class Sem:
    def __init__(self, handle, name):
        self.h = handle
        self.name = name
        self.count = 0


class U:
    def __init__(self, name):
        self.name = name
        self.lw = []
        self.rd = []
        self.sem = None


class Prog:
    def __init__(self, nc, stack):
        self.nc = nc
        self.stack = stack
        self.q = {e: [] for e in ENGS}
        self.sems = []
        self.esem = {}
        for e in ("tensor", "scalar", "vector", "gpsimd"):
            self.esem[e] = self.new_sem("e_" + e)
        self.waited = {e: {} for e in ENGS}

    def new_sem(self, name):
        h = self.stack.enter_context(self.nc.semaphore(name))
        s = Sem(h, name)
        self.sems.append(s)
        return s

    def _waits(self, eng, r, w, extra=()):
        need = {}

        def add(t):
            s, v = t
            if need.get(s, 0) < v:
                need[s] = v
        for u in r:
            for t in u.lw:
                add(t)
        for u in w:
            for t in u.lw:
                add(t)
            for t in u.rd:
                add(t)
        for t in extra:
            add(t)
        out = []
        wd = self.waited[eng]
        for s, v in need.items():
            if wd.get(s, 0) >= v:
                continue
            wd[s] = v
            out.append((s, v))
        return out

    def _record(self, ticket, r, w, wacc):
        for u in r:
            u.rd.append(ticket)
            if len(u.rd) > 64:
                u.rd = _compress(u.rd)
        for u in w:
            u.lw = [ticket]
            u.rd = []
        for u in wacc:
            u.lw.append(ticket)
            if len(u.lw) > 64:
                u.lw = _compress(u.lw)

    def op(self, eng, fn, r=(), w=(), wacc=(), extra=()):
        waits = self._waits(eng, list(r) + list(wacc), w, extra)
        s = self.esem[eng]
        s.count += 1
        ticket = (s, s.count)
        self._record(ticket, r, w, wacc)

        def run(e, waits=waits, fn=fn, s=s):
            for (ws, v) in waits:
                e.wait_ge(ws.h, v)
            fn(e).then_inc(s.h, 1)
        self.q[eng].append(run)
        return ticket

    def group(self, eng, fns, r=(), w=(), extra=()):
        waits = self._waits(eng, r, w, extra)
        s = self.esem[eng]
        s.count += 1
        ticket = (s, s.count)
        self._record(ticket, r, w, ())

        def run(e, waits=waits, fns=fns, s=s):
            for (ws, v) in waits:
                e.wait_ge(ws.h, v)
            for f in fns[:-1]:
                f(e)
            fns[-1](e).then_inc(s.h, 1)
        self.q[eng].append(run)
        return ticket

    def dma(self, eng, fn, su, r=(), w=(), wacc=(), extra=()):
        if su.sem is None:
            su.sem = self.new_sem("d_" + su.name)
        waits = self._waits(eng, list(r) + list(wacc), w, extra)
        s = su.sem
        s.count += 16
        ticket = (s, s.count)
        self._record(ticket, r, w, wacc)

        def run(e, waits=waits, fn=fn, s=s):
            for (ws, v) in waits:
                e.wait_ge(ws.h, v)
            fn(e).then_inc(s.h, 16)
        self.q[eng].append(run)
        return ticket

    def barrier(self, engs=ENGS):
        for eng in engs:
            lst = []
            wd = self.waited[eng]
            for s in self.sems:
                if s.count > 0 and wd.get(s, 0) < s.count:
                    wd[s] = s.count
                    lst.append((s, s.count))

            def run(e, lst=lst):
                for (ws, v) in lst:
                    e.wait_ge(ws.h, v)
            self.q[eng].append(run)

    def emit(self, block):
        q = self.q

        @block.sync
        def _(e):
            for f in q["sync"]:
                f(e)

        @block.tensor
        def _(e):
            for f in q["tensor"]:
                f(e)

        @block.scalar
        def _(e):
            for f in q["scalar"]:
                f(e)

        @block.vector
        def _(e):
            for f in q["vector"]:
                f(e)

        @block.gpsimd
        def _(e):
            for f in q["gpsimd"]:
                f(e)


def _compress(tickets):
    need = {}
    for s, v in tickets:
        if need.get(s, 0) < v:
            need[s] = v
    return list(need.items())


def handoff(olds, news):
    ts = []
    for ou in olds:
        ts.extend(ou.lw)
        ts.extend(ou.rd)
    ts = _compress(ts)
    for nu in news:
        nu.rd.extend(ts)


def build_program():
    nc = bass.Bass("TRN2", target_bir_lowering=False)
    dt = nc.dram_tensor

    def din(name, shape, d=F32):
        return dt(name, list(shape), d, kind="ExternalInput").ap()

    x_in = din("x_in", [NST, TH, D])
    rope = din("rope", [NST, TH, 64])
    kbias_d = din("kbias", [128, NST * NBH])
    invcnt_d = din("invcnt", [NST, 128, 4 * T])
    w_in = din("w_in", [D, 8192])
    pool_w = din("pool_w", [4, 256, 256])
    pool_proj = din("pool_proj", [1024, D])
    attn_proj = din("attn_proj", [D, D])
    w_out = din("w_out", [D, D])
    w_gate = din("w_gate", [NE, D, DFF])
    w_up = din("w_up", [NE, D, DFF])
    w_down = din("w_down", [NE, DFF, D])
    g1T_d = din("g1T", [128, 16])
    g2_d = din("g2bc", [128, D])
    pscT_d = din("pscT", [128, 8])
    qg_d = din("qgbc", [128, 128])
    kg_d = din("kgbc", [128, 128])
    sink_d = din("sinkbc", [128, 16])
    wr_d = din("wr", [D, 36])
    br_d = din("brbc", [128, 36])
    ident_d = din("ident", [128, 128])
    masks_d = din("masks", [128, 2 * 512])
    lstrict_d = din("lstrict", [128, 128])
    ones_d = din("ones", [128, 128])
    ecap_d = din("ecap", [128, NE])
    pidx_d = din("pidx", [128, 1])

    y_out = dt("y", [NTOK, D], F32, kind="ExternalOutput").ap()
    xg = dt("xg", [NSLOT + 128, D], BF16, kind="Internal").ap()
    ye = dt("ye", [NSLOT + 128, D], F32, kind="Internal").ap()

    with contextlib.ExitStack() as st:
        P = Prog(nc, st)
        block = None

        def sb(name, shape, d, stack=st):
            return stack.enter_context(nc.sbuf_tensor("s_" + name, list(shape), d))

        banks = [st.enter_context(nc.psum_tensor("bank%d" % i, [128, 512], F32)) for i in range(8)]
        banks_bf = [b.bitcast(BF16) for b in banks]
        BK = [U("bk%d" % i) for i in range(8)]
        bstate = {"i": 0}

        def nb():
            i = bstate["i"]
            bstate["i"] = (i + 1) % 8
            return i

        UC = U("const")
        g1T = sb("g1T", [128, 16], F32)
        g2bc = sb("g2bc", [128, D], F32)
        pscT = sb("pscT", [128, 8], F32)
        qgbc = sb("qgbc", [128, 128], F32)
        kgbc = sb("kgbc", [128, 128], F32)
        esink = sb("esink", [128, 16], F32)
        wr = sb("wr", [128, 16, 36], F32)
        brbc = sb("brbc", [128, 36], F32)
        ident_f = sb("ident_f", [128, 128], F32)
        ident_b = sb("ident_b", [128, 128], BF16)
        masks = sb("masks", [128, 2, 512], BF16)
        lstrict = sb("lstrict", [128, 128], BF16)
        ones_b = sb("ones_b", [128, 128], BF16)
        ecap = sb("ecap", [128, NE], F32)
        pidx = sb("pidx", [128, 1], F32)
        kbias = sb("kbias", [128, NST * NBH], F32)
        poolw = sb("poolw", [128, 8, 256], BF16)
        tslot = sb("tslot", [128, NST * NB, 2], I32)
        twt = sb("twt", [128, NST * NB, 2], F32)
        basebc = sb("basebc", [128, NE], F32)
        UTS = [U("ts%d" % i) for i in range(NST * NB)]
        UBASE = U("base")

        def cload(eng, out, in_):
            P.dma(eng, lambda e: e.dma_start(out=out, in_=in_), UC, wacc=[UC])

        cload("sync", g1T[:], g1T_d)
        cload("sync", g2bc[:], g2_d)
        cload("sync", pscT[:], pscT_d)
        cload("sync", qgbc[:], qg_d)
        cload("sync", kgbc[:], kg_d)
        cload("sync", esink[:], sink_d)
        cload("sync", wr[:], wr_d.rearrange("(k p) n -> p k n", p=128))
        cload("sync", brbc[:], br_d)
        cload("sync", ident_f[:], ident_d)
        cload("sync", ecap[:], ecap_d)
        cload("sync", pidx[:], pidx_d)
        cload("sync", kbias[:], kbias_d)
        cload("gpsimd", ident_b[:], ident_d)
        cload("gpsimd", masks[:], masks_d.rearrange("p (a n) -> p a n", a=2))
        cload("gpsimd", lstrict[:], lstrict_d)
        cload("gpsimd", ones_b[:], ones_d)
        cload("gpsimd", poolw[:], pool_w.rearrange("g (cc p) d -> p (g cc) d", p=128))
        P.op("scalar", lambda e: e.activation(out=esink[:], in_=esink[:], func=AF.Exp), r=[UC], w=[UC])
        P.op("vector", lambda e: e.memset(basebc[:], 0.0), w=[UBASE])

        UXGZ = U("xgz")
        if PHASES >= 2:
            p0 = contextlib.ExitStack()
            zt0 = sb("zt0", [128, 8, D], BF16, p0)
            UZ0 = U("zt0")
            P.op("vector", lambda e: e.memset(zt0[:], 0.0), w=[UZ0])
            nfull = (NSLOT + 128) // 1024
            for kz in range(nfull):
                P.dma("sync", lambda e, kz=kz: e.dma_start(
                    out=xg[kz * 1024:(kz + 1) * 1024, :].rearrange("(j p) d -> p j d", p=128), in_=zt0[:]),
                    UZ0, r=[UZ0], wacc=[UXGZ])
            rem0 = nfull * 1024
            P.dma("sync", lambda e: e.dma_start(out=xg[rem0:rem0 + 128, :], in_=zt0[:, 0, :]), UZ0, r=[UZ0], wacc=[UXGZ])
            P.barrier()
            p0.close()

        p1 = contextlib.ExitStack()
        sb1 = lambda name, shape, d: sb(name, shape, d, p1)
        xnT = sb1("xnT", [128, 16, TH], BF16)
        NWS = 5
        wbuf = [sb1("wbuf%d" % i, [128, 16, 256], BF16) for i in range(NWS)]
        hbuf = [sb1("hbuf%d" % i, [128, D], F32) for i in range(4)]
        xs = sb1("xs", [128, D], BF16)
        yT = sb1("yT", [128, 8, T], BF16)
        qT = sb1("qT", [128, 16, T], BF16)
        mT = qT
        kT = sb1("kT", [128, 4, TH], BF16)
        v_sb = sb1("v_sb", [128, NBH, 512], BF16)
        oT = sb1("oT", [128, 16, T], BF16)
        mixT = oT[:, 0:8, :]
        R1 = sb1("R1", [128, 11264], BF16)
        R2x = sb1("R2x", [128, 1024], BF16)
        stat = sb1("stat", [128, 64], F32)
        ropet = sb1("ropet", [128, NBH, 64], F32)
        rtmp = sb1("rtmp", [128, NB, 264], F32)
        rtb = sb1("rtb", [128, NB, 32], BF16)
        rti = sb1("rti", [128, 8], I32)

        def view(ap2d, d, pattern=None, **kw):
            v = ap2d.bitcast(d) if d != BF16 else ap2d
            if pattern:
                v = v.rearrange(pattern, **kw)
            return v

        invc = view(R1[:, 0:4096], F32, "p (g t) -> p g t", g=4)
        xpc = [view(R1[:, 4096 + i * 1536: 4096 + (i + 1) * 1536], F32) for i in range(2)]
        sA = view(R1[:, 7168:8704], F32)
        sB = view(R1[:, 8704:10240], F32)
        tmpw = view(R1[:, 10240:11264], F32)
        sg = view(R1[:, 0:2048], F32, "p (c t) -> p c t", c=2)
        tacc = view(R1[:, 2048:4096], F32, "p (c t) -> p c t", c=2)
        hnT = view(R1[:, 0:4096], F32, "p (k t) -> p k t", k=16)
        hnT_alt = view(R1[:, 4096:8192], F32, "p (k t) -> p k t", k=16)
        hnTs = [hnT, hnT_alt]
        stat2 = sb1("stat2", [128, 16], F32)
        h2b = hbuf[2][:].bitcast(BF16)
        h3b = hbuf[3][:].bitcast(BF16)
        pT = [view(h2b[:, i * 1536:(i + 1) * 1536], BF16, "p (j n) -> p j n", j=3) for i in range(2)]
        sqj = h3b[:, 0:2048]
        ddt = view(R2x[:, 0:1024], F32)
        qr2 = [view(R1[:, i * 512:(i + 1) * 512], F32) for i in range(6)]
        qbf2 = [R1[:, 3072 + i * 256: 3072 + (i + 1) * 256] for i in range(6)]
        rotA2 = [view(R1[:, 4608 + i * 128: 4608 + (i + 1) * 128], F32, "p (h d) -> p h d", h=2) for i in range(6)]
        rotB2 = [view(R1[:, 5376 + i * 128: 5376 + (i + 1) * 128], F32, "p (h d) -> p h d", h=2) for i in range(6)]

        UXB = [U("xb%d" % i) for i in range(4)]
        UXS = U("xs")
        USTAT = U("stat")
        UXN = [U("xn%d" % b) for b in range(NBH)]
        UW = [U("w%d" % i) for i in range(5)]
        UXPC = [U("xpc%d" % i) for i in range(2)]
        USA, USB, UTMPW, UINVC = U("sA"), U("sB"), U("tmpw"), U("invc")
        UMIX = [U("mix%d" % c) for c in range(8)]
        UYT = [U("yT%d" % c) for c in range(8)]
        UQR = [U("qr%d" % i) for i in range(6)]
        UQBF = [U("qbf%d" % i) for i in range(6)]
        UROT = [U("rot%d" % i) for i in range(6)]
        UB2 = UQR + UQBF + UROT
        UQT = [[U("qT%d_%d" % (b, g)) for g in range(4)] for b in range(NB)]
        UKT = [U("kT%d" % b) for b in range(NBH)]
        UV = [U("v%d" % b) for b in range(NBH)]
        UPT = [[U("pT%d_%d" % (i, j)) for j in range(3)] for i in range(2)]
        UDD = U("dd")
        USQJ = U("sqj")
        UOT = [[U("oT%d_%d" % (b, k)) for k in range(4)] for b in range(NB)]
        USG = [U("sg%d" % c) for c in range(2)]
        UTACC = [U("tacc%d" % c) for c in range(2)]
        UMT = [U("mT%d" % c) for c in range(16)]
        UHNT = U("hnT")
        UHNT1 = U("hnT1")
        UHNTs = [UHNT, UHNT1]
        USTAT2 = [U("stat2_%d" % b) for b in range(NB)]
        UROPE = U("rope")
        URT = [U("rt%d" % b) for b in range(NB)]
        URTB = [U("rtb%d" % b) for b in range(NB)]
        URTI = U("rti")
        UXG = U("xg")
        UYB = [U("y%d" % i) for i in range(NST * NB)]
        wslot = {"i": 0}

        def load_w(src_ap, kc):
            i = wslot["i"]
            wslot["i"] = (i + 1) % NWS
            P.dma("gpsimd", lambda e, i=i: e.dma_start(out=wbuf[i][:, 0:kc, :],
                                                       in_=src_ap.rearrange("(k p) n -> p k n", p=128)),
                  UW[i], w=[UW[i]])
            return i

        xslot = {"i": 0}

        for s in range(NST):
            handoff(UXB[2:4], [u for row in UPT for u in row] + [USQJ])
            P.dma("sync", lambda e, s=s: e.dma_start(out=ropet[:], in_=rope[s].rearrange("(b p) c -> p b c", p=128)),
                  UROPE, w=[UROPE])
            for b in range(NBH):
                xi = xslot["i"]
                xslot["i"] = 1 - xi
                xb = hbuf[xi]
                P.dma("sync", lambda e, s=s, b=b, xb=xb: e.dma_start(out=xb[:], in_=x_in[s, b * 128:(b + 1) * 128, :]),
                      UXB[xi], w=[UXB[xi]])
                P.op("scalar", lambda e, xb=xb: e.activation(out=sqj, in_=xb[:], func=AF.Square, accum_out=stat[:, 0:1]),
                     r=[UXB[xi]], w=[USQJ, USTAT])
                P.op("vector", lambda e: e.tensor_scalar(out=stat[:, 1:2], in0=stat[:, 0:1], scalar1=1.0 / D, scalar2=EPS,
                                                         op0=ALU.mult, op1=ALU.add), r=[USTAT], w=[USTAT])
                P.op("scalar", lambda e: e.activation(out=stat[:, 2:3], in_=stat[:, 1:2], func=AF.Sqrt), r=[USTAT], w=[USTAT])
                P.op("vector", lambda e: e.reciprocal(out=stat[:, 3:4], in_=stat[:, 2:3]), r=[USTAT], w=[USTAT])
                P.op("scalar", lambda e, xb=xb: e.activation(out=xs[:], in_=xb[:], func=AF.Copy, scale=stat[:, 3:4]),
                     r=[UXB[xi], USTAT], w=[UXS])
                for half in range(2):
                    bi = nb()
                    fns = [(lambda e, c=c, bi=bi, half=half: e.transpose(
                        out=banks_bf[bi][:, c * 128:(c + 1) * 128],
                        in_=xs[:, (half * 8 + c) * 128:(half * 8 + c + 1) * 128], identity=ident_b[:])) for c in range(8)]
                    P.group("tensor", fns, r=[UXS, UC], w=[BK[bi]])
                    P.op("vector", lambda e, bi=bi, half=half, b=b: e.tensor_tensor(
                        out=xnT[:, half * 8:(half + 1) * 8, b * 128:(b + 1) * 128],
                        in0=banks_bf[bi][:].rearrange("p (c t) -> p c t", c=8),
                        in1=g1T[:, half * 8:(half + 1) * 8].unsqueeze(2).to_broadcast([128, 8, 128]), op=ALU.mult),
                        r=[BK[bi], UC], w=[UXN[b]])

            handoff(USG + UTACC + [UHNT, UHNT1] + UB2, [UINVC] + UXPC + [USA, USB, UTMPW])
            handoff([u for row in UOT for u in row], UMIX)
            P.dma("sync", lambda e, s=s: e.dma_start(out=invc, in_=invcnt_d[s].rearrange("p (g t) -> p g t", g=4)),
                  UINVC, w=[UINVC])
            for pc in range(4):
                wi = load_w(w_in[:, pc * 256:(pc + 1) * 256], 16)
                for cc in range(2):
                    ch = pc * 2 + cc
                    g = ch // 2
                    xi2 = ch % 2
                    xp = xpc[xi2]
                    for half in range(2):
                        bi = nb()
                        t0 = half * 384
                        fns = [(lambda e, k=k, bi=bi, wi=wi, cc=cc, t0=t0: e.matmul(
                            banks[bi][:, 0:384], lhsT=wbuf[wi][:, k, cc * 128:(cc + 1) * 128],
                            rhs=xnT[:, k, t0:t0 + 384], start=(k == 0), stop=(k == 15))) for k in range(16)]
                        P.group("tensor", fns, r=[UW[wi]] + UXN, w=[BK[bi]])
                        P.op("scalar", lambda e, bi=bi, xp=xp, t0=t0: e.activation(out=xp[:, t0:t0 + 384], in_=banks[bi][:, 0:384],
                                                                                 func=AF.Copy),
                             r=[BK[bi]], w=[UXPC[xi2]])
                    P.op("vector", lambda e, xp=xp: e.tensor_tensor(out=sA[:, 1:TH], in0=xp[:, 0:TH - 1], in1=xp[:, 1:TH], op=ALU.add),
                         r=[UXPC[xi2]], w=[USA])
                    cur, ucur, oth, uoth = sA, USA, sB, USB
                    lo = 1
                    for lvl in range(g):
                        sh = 1 << lvl
                        nlo = lo + sh
                        nhi = TH - lo - sh + 1
                        P.op("vector", lambda e, cur=cur, oth=oth, sh=sh, nlo=nlo, nhi=nhi: e.tensor_tensor(
                            out=oth[:, nlo:nhi], in0=cur[:, nlo - sh:nhi - sh], in1=cur[:, nlo + sh:nhi + sh], op=ALU.add),
                            r=[ucur], w=[uoth])
                        cur, ucur, oth, uoth = oth, uoth, cur, ucur
                        lo = nlo
                    P.op("vector", lambda e, cur=cur, g=g: e.tensor_tensor(out=tmpw[:], in0=cur[:, 128:128 + T], in1=invc[:, g, :],
                                                                          op=ALU.mult), r=[ucur, UINVC], w=[UTMPW])
                    P.op("vector", lambda e, xp=xp, ch=ch: e.tensor_tensor(out=mixT[:, ch, :], in0=tmpw[:], in1=xp[:, 128:128 + T],
                                                                          op=ALU.subtract), r=[UTMPW, UXPC[xi2]], w=[UMIX[ch]])
            for g in range(4):
                for dc in range(2):
                    bi = nb()
                    fns = [(lambda e, cc=cc, g=g, dc=dc, bi=bi: e.matmul(
                        banks[bi][:, :], lhsT=poolw[:, g * 2 + cc, dc * 128:(dc + 1) * 128],
                        rhs=mixT[:, 2 * g + cc, :], start=(cc == 0), stop=(cc == 1))) for cc in range(2)]
                    P.group("tensor", fns, r=[UC, UMIX[2 * g], UMIX[2 * g + 1]], w=[BK[bi]])
                    ch = 2 * g + dc
                    P.op("scalar", lambda e, bi=bi, ch=ch: e.activation(out=yT[:, ch, :], in_=banks[bi][:, :], func=AF.Copy,
                                                                       scale=pscT[:, ch:ch + 1]), r=[BK[bi], UC], w=[UYT[ch]])

            handoff([UINVC] + UXPC + [USA, USB, UTMPW], UB2)
            handoff(UMT, [u for row in UQT for u in row])
            pieces = [("q", i) for i in range(8)] + [("k", i) for i in range(2)] + [("v", i) for i in range(2)]
            b2a = {"i": 0}
            b2b = {"i": 0}

            def nbA():
                i = b2a["i"]
                b2a["i"] = (i + 1) % 6
                return i

            def nbB():
                i = b2b["i"]
                b2b["i"] = (i + 1) % 2
                return 6 + i

            def b2_mm(kind, pi_):
                col0 = {"q": 1024, "k": 3072, "v": 3584}[kind] + pi_ * 256
                wi = load_w(w_in[:, col0:col0 + 256], 16)
                blocks = list(range(1, 1 + NB)) if kind == "q" else list(range(NBH))
                pairs = []
                for p0 in range(0, len(blocks), 2):
                    bi = nbA()
                    fns = []
                    for j in range(2):
                        b = blocks[p0 + j]
                        fns += [(lambda e, k=k, bi=bi, wi=wi, b=b, j=j: e.matmul(
                            banks[bi][:, j * 256:(j + 1) * 256], lhsT=xnT[:, k, b * 128:(b + 1) * 128], rhs=wbuf[wi][:, k, :],
                            start=(k == 0), stop=(k == 15))) for k in range(16)]
                    P.group("tensor", fns, r=[UW[wi], UXN[blocks[p0]], UXN[blocks[p0 + 1]]], w=[BK[bi]])
                    pairs.append((bi, blocks[p0], blocks[p0 + 1]))
                return pairs

            def b2_post(kind, pi_, pairs):
                if kind == "v":
                    for (bi, b0, b1) in pairs:
                        P.op("scalar", lambda e, bi=bi, b0=b0, pi_=pi_: e.activation(
                            out=v_sb[:, b0:b0 + 2, pi_ * 256:(pi_ + 1) * 256],
                            in_=banks[bi][:, :].rearrange("p (j c) -> p j c", j=2), func=AF.Copy),
                            r=[BK[bi]], w=[UV[b0], UV[b1]])
                    return
                gbc = qgbc if kind == "q" else kgbc
                nblk = 2 * len(pairs)
                for pj, (bi, b0, b1) in enumerate(pairs):
                    for j in range(2):
                        for hh in range(2):
                            c = 8 + (pj * 2 + j) * 2 + hh
                            P.op("scalar", lambda e, bi=bi, j=j, hh=hh, c=c: e.activation(
                                out=sqj[:, 0:128], in_=banks[bi][:, j * 256 + hh * 128: j * 256 + (hh + 1) * 128], func=AF.Square,
                                accum_out=stat[:, c:c + 1]), r=[BK[bi]], w=[USQJ, USTAT])
                n2 = nblk * 2
                P.op("vector", lambda e, n2=n2: e.tensor_scalar(out=stat[:, 20:20 + n2], in0=stat[:, 8:8 + n2], scalar1=1.0 / 128,
                                                                scalar2=EPS, op0=ALU.mult, op1=ALU.add), r=[USTAT], w=[USTAT])
                P.op("scalar", lambda e, n2=n2: e.activation(out=stat[:, 32:32 + n2], in_=stat[:, 20:20 + n2], func=AF.Sqrt),
                     r=[USTAT], w=[USTAT])
                P.op("vector", lambda e, n2=n2: e.reciprocal(out=stat[:, 44:44 + n2], in_=stat[:, 32:32 + n2]), r=[USTAT], w=[USTAT])
                items = []
                for pj, (bi, b0, b1) in enumerate(pairs):
                    for j, b in enumerate((b0, b1)):
                        items.append((pj * 2 + j, bi, j, b))
                for (ti, bi, j, b) in items:
                    qr3 = qr2[ti].rearrange("p (h d) -> p h d", h=2)
                    P.op("vector", lambda e, ti=ti, bi=bi, j=j, qr3=qr3: e.tensor_tensor(
                        out=qr3, in0=banks[bi][:, j * 256:(j + 1) * 256].rearrange("p (h d) -> p h d", h=2),
                        in1=stat[:, 44 + ti * 2:44 + ti * 2 + 2].unsqueeze(2).to_broadcast([128, 2, 128]), op=ALU.mult),
                        r=[BK[bi], USTAT], w=[UQR[ti]])
                for (ti, bi, j, b) in items:
                    qr3 = qr2[ti].rearrange("p (h d) -> p h d", h=2)
                    qb3 = qbf2[ti].rearrange("p (h d) -> p h d", h=2)
                    P.op("vector", lambda e, qr3=qr3, qb3=qb3, gbc=gbc: e.tensor_tensor(
                        out=qb3[:, :, 32:128], in0=qr3[:, :, 32:128],
                        in1=gbc[:, 32:128].unsqueeze(1).to_broadcast([128, 2, 96]), op=ALU.mult),
                        r=[UQR[ti], UC], w=[UQBF[ti]])
                    P.op("vector", lambda e, qr3=qr3, gbc=gbc: e.tensor_tensor(
                        out=qr3[:, :, 0:32], in0=qr3[:, :, 0:32],
                        in1=gbc[:, 0:32].unsqueeze(1).to_broadcast([128, 2, 32]), op=ALU.mult),
                        r=[UC], w=[UQR[ti]])
                for (ti, bi, j, b) in items:
                    qr3 = qr2[ti].rearrange("p (h d) -> p h d", h=2)
                    P.op("vector", lambda e, ti=ti, qr3=qr3, b=b: e.tensor_tensor(
                        out=rotA2[ti], in0=qr3[:, :, 0:32], in1=ropet[:, b, 0:32].unsqueeze(1).to_broadcast([128, 2, 32]), op=ALU.mult),
                        r=[UQR[ti], UROPE], w=[UROT[ti]])
                    P.op("vector", lambda e, ti=ti, qr3=qr3, b=b: e.tensor_tensor(
                        out=rotB2[ti][:, :, 0:16], in0=qr3[:, :, 16:32],
                        in1=ropet[:, b, 32:48].unsqueeze(1).to_broadcast([128, 2, 16]), op=ALU.mult),
                        r=[UQR[ti], UROPE], wacc=[UROT[ti]])
                    P.op("vector", lambda e, ti=ti, qr3=qr3, b=b: e.tensor_tensor(
                        out=rotB2[ti][:, :, 16:32], in0=qr3[:, :, 0:16],
                        in1=ropet[:, b, 48:64].unsqueeze(1).to_broadcast([128, 2, 16]), op=ALU.mult),
                        r=[UQR[ti], UROPE], wacc=[UROT[ti]])
                for (ti, bi, j, b) in items:
                    qb3 = qbf2[ti].rearrange("p (h d) -> p h d", h=2)
                    P.op("vector", lambda e, ti=ti, qb3=qb3: e.tensor_tensor(out=qb3[:, :, 0:32], in0=rotA2[ti], in1=rotB2[ti], op=ALU.add),
                         r=[UROT[ti]], wacc=[UQBF[ti]])
                for pj, (bi, b0, b1) in enumerate(pairs):
                    bj = nbB()
                    fns = []
                    for j in range(2):
                        ti = pj * 2 + j
                        for hh in range(2):
                            fns.append(lambda e, ti=ti, hh=hh, j=j, bj=bj: e.transpose(
                                out=banks_bf[bj][:, hh * 256 + j * 128: hh * 256 + (j + 1) * 128],
                                in_=qbf2[ti][:, hh * 128:(hh + 1) * 128], identity=ident_b[:]))
                    P.group("tensor", fns, r=[UQBF[pj * 2], UQBF[pj * 2 + 1], UC], w=[BK[bj]])
                    src = banks_bf[bj][:, 0:512].rearrange("p (h t) -> p h t", h=2)
                    if kind == "q":
                        qb = b0 - 1
                        P.op("scalar", lambda e, src=src, pi_=pi_, qb=qb: e.activation(
                            out=qT[:, pi_ * 2:(pi_ + 1) * 2, qb * 128:(qb + 2) * 128], in_=src, func=AF.Copy),
                            r=[BK[bj]], wacc=[UQT[qb][pi_ // 2], UQT[qb + 1][pi_ // 2]])
                    else:
                        P.op("scalar", lambda e, src=src, pi_=pi_, b0=b0: e.activation(
                            out=kT[:, pi_ * 2:(pi_ + 1) * 2, b0 * 128:(b0 + 2) * 128], in_=src, func=AF.Copy),
                            r=[BK[bj]], wacc=[UKT[b0], UKT[b1]])

            for row in UQT:
                for u in row:
                    u.lw = list(u.rd) + list(u.lw)
                    u.rd = []
            for u in UKT:
                u.lw = list(u.rd) + list(u.lw)
                u.rd = []
            prev = None
            for (kind, pi_) in pieces:
                pairs = b2_mm(kind, pi_)
                if prev is not None:
                    b2_post(*prev)
                prev = (kind, pi_, pairs)
            b2_post(*prev)

            handoff(UMIX, [u for row in UOT for u in row])
            scale = 128.0 ** -0.5
            pslot = {"i": 0}
            for qb in range(NB):
                for kh in range(4):
                    pi = pslot["i"]
                    pslot["i"] = 1 - pi
                    for jc in range(3):
                        kb = qb + jc
                        bi = nb()
                        fns = [lambda e, bi=bi, kb=kb, kh=kh, qb=qb, jc=jc: e.matmul(
                            banks[bi][:, :].rearrange("p (h q) -> p h q", h=4), lhsT=kT[:, kh, kb * 128:(kb + 1) * 128],
                            rhs=qT[:, kh * 4:(kh + 1) * 4, qb * 128:(qb + 1) * 128], start=True, stop=(jc == 1))]
                        if jc != 1:
                            mi = 0 if jc == 0 else 1
                            fns.append(lambda e, bi=bi, mi=mi: e.matmul(banks[bi][:, :], lhsT=ident_b[:], rhs=masks[:, mi, :],
                                                                      start=False, stop=True))
                        P.group("tensor", fns, r=[UKT[kb], UC] + UQT[qb], w=[BK[bi]])
                        col = s * NBH + kb
                        P.op("scalar", lambda e, bi=bi, pi=pi, jc=jc, col=col: e.activation(
                            out=pT[pi][:, jc, :], in_=banks[bi][:, :], func=AF.Exp, bias=kbias[:, col:col + 1], scale=scale),
                            r=[BK[bi], UC], w=[UPT[pi][jc]])
                    bo = nb()
                    fns = [(lambda e, jc=jc, bo=bo, pi=pi, qb=qb, kh=kh: e.matmul(
                        banks[bo][:, :], lhsT=v_sb[:, qb + jc, kh * 128:(kh + 1) * 128], rhs=pT[pi][:, jc, :],
                        start=(jc == 0), stop=(jc == 2))) for jc in range(3)]
                    P.group("tensor", fns, r=[UV[qb], UV[qb + 1], UV[qb + 2]] + UPT[pi], w=[BK[bo]])
                    bd = nb()
                    fns = [(lambda e, jc=jc, bd=bd, pi=pi: e.matmul(
                        banks[bd][:, :], lhsT=ones_b[:], rhs=pT[pi][:, jc, :], start=(jc == 0), stop=(jc == 2))) for jc in range(3)]
                    P.group("tensor", fns, r=[UC] + UPT[pi], w=[BK[bd]])
                    dd3 = ddt.rearrange("p (h q) -> p h q", h=4)
                    P.op("vector", lambda e, bd=bd, kh=kh, dd3=dd3: e.tensor_tensor(
                        out=dd3, in0=banks[bd][:, :].rearrange("p (h q) -> p h q", h=4),
                        in1=esink[:, kh * 4:(kh + 1) * 4].unsqueeze(2).to_broadcast([128, 4, 128]), op=ALU.add),
                        r=[BK[bd], UC], w=[UDD])
                    P.op("vector", lambda e: e.reciprocal(out=ddt, in_=ddt), r=[UDD], w=[UDD])
                    P.op("vector", lambda e, bo=bo, qb=qb, kh=kh, dd3=dd3: e.tensor_tensor(
                        out=oT[:, kh * 4:(kh + 1) * 4, qb * 128:(qb + 1) * 128],
                        in0=banks[bo][:, :].rearrange("p (h q) -> p h q", h=4), in1=dd3, op=ALU.mult),
                        r=[BK[bo], UDD], w=[UOT[qb][kh]])

            handoff([UINVC] + UXPC + [USA, USB, UTMPW, UHNT] + UB2, USG + UTACC)
            handoff([u for row in UQT for u in row], UMT)
            allOT = [u for row in UOT for u in row]
            xn_main = lambda k: xnT[:, k, 128:128 + T]
            for cg in range(8):
                wi = load_w(w_in[:, 4096 + cg * 256: 4096 + (cg + 1) * 256], 16)
                for cc in range(2):
                    bi = nb()
                    fns = [(lambda e, k=k, bi=bi, wi=wi, cc=cc: e.matmul(
                        banks[bi][:, :], lhsT=wbuf[wi][:, k, cc * 128:(cc + 1) * 128], rhs=xn_main(k),
                        start=(k == 0), stop=(k == 15))) for k in range(16)]
                    P.group("tensor", fns, r=[UW[wi]] + UXN, w=[BK[bi]])
                    P.op("scalar", lambda e, bi=bi, cc=cc: e.activation(out=sg[:, cc, :], in_=banks[bi][:, :], func=AF.Sigmoid),
                         r=[BK[bi]], w=[USG[cc]])
                wi = load_w(pool_proj[:, cg * 256:(cg + 1) * 256], 8)
                for cc in range(2):
                    bi = nb()
                    fns = [(lambda e, k=k, bi=bi, wi=wi, cc=cc: e.matmul(
                        banks[bi][:, :], lhsT=wbuf[wi][:, k, cc * 128:(cc + 1) * 128], rhs=yT[:, k, :],
                        start=(k == 0), stop=(k == 7))) for k in range(8)]
                    P.group("tensor", fns, r=[UW[wi]] + UYT, w=[BK[bi]])
                    P.op("vector", lambda e, bi=bi, cc=cc: e.tensor_tensor(out=tacc[:, cc, :], in0=banks[bi][:, :], in1=sg[:, cc, :],
                                                                        op=ALU.mult), r=[BK[bi], USG[cc]], w=[UTACC[cc]])
                wi = load_w(w_in[:, 6144 + cg * 256: 6144 + (cg + 1) * 256], 16)
                for cc in range(2):
                    bi = nb()
                    fns = [(lambda e, k=k, bi=bi, wi=wi, cc=cc: e.matmul(
                        banks[bi][:, :], lhsT=wbuf[wi][:, k, cc * 128:(cc + 1) * 128], rhs=xn_main(k),
                        start=(k == 0), stop=(k == 15))) for k in range(16)]
                    P.group("tensor", fns, r=[UW[wi]] + UXN, w=[BK[bi]])
                    P.op("scalar", lambda e, bi=bi, cc=cc: e.activation(out=sg[:, cc, :], in_=banks[bi][:, :], func=AF.Sigmoid),
                         r=[BK[bi]], w=[USG[cc]])
                wi = load_w(attn_proj[:, cg * 256:(cg + 1) * 256], 16)
                for cc in range(2):
                    bi = nb()
                    fns = [(lambda e, k=k, bi=bi, wi=wi, cc=cc: e.matmul(
                        banks[bi][:, :], lhsT=wbuf[wi][:, k, cc * 128:(cc + 1) * 128], rhs=oT[:, k, :],
                        start=(k == 0), stop=(k == 15))) for k in range(16)]
                    P.group("tensor", fns, r=[UW[wi]] + allOT, w=[BK[bi]])
                    P.op("vector", lambda e, bi=bi, cc=cc: e.tensor_tensor(out=sg[:, cc, :], in0=banks[bi][:, :], in1=sg[:, cc, :],
                                                                        op=ALU.mult), r=[BK[bi], USG[cc]], w=[USG[cc]])
                    c = cg * 2 + cc
                    P.op("vector", lambda e, cc=cc, c=c: e.tensor_tensor(out=mT[:, c, :], in0=sg[:, cc, :], in1=tacc[:, cc, :],
                                                                      op=ALU.add), r=[USG[cc], UTACC[cc]], w=[UMT[c]])

            handoff([u for row in UPT for u in row] + [USQJ], UXB[2:4])
            handoff(USG + UTACC, [UHNT])
            handoff(UB2 + [UINVC] + UXPC + [USA, USB, UTMPW], [UHNT1])
            for b in range(NB):
                P.dma("sync", lambda e, s=s, b=b: e.dma_start(out=hbuf[b][:], in_=x_in[s, (b + 1) * 128:(b + 2) * 128, :]),
                      UXB[b], w=[UXB[b]])
            for cg in range(8):
                wi = load_w(w_out[:, cg * 256:(cg + 1) * 256], 16)
                for b0 in (0, 2):
                    bi = nb()
                    fns = []
                    for j in range(2):
                        b = b0 + j
                        fns += [(lambda e, k=k, bi=bi, wi=wi, b=b, j=j: e.matmul(
                            banks[bi][:, j * 256:(j + 1) * 256], lhsT=mT[:, k, b * 128:(b + 1) * 128], rhs=wbuf[wi][:, k, :],
                            start=(k == 0), stop=(k == 15))) for k in range(16)]
                    P.group("tensor", fns, r=[UW[wi]] + UMT, w=[BK[bi]])
                    for j in range(2):
                        b = b0 + j
                        P.op("vector", lambda e, bi=bi, b=b, cg=cg, j=j: e.tensor_tensor(
                            out=hbuf[b][:, cg * 256:(cg + 1) * 256], in0=banks[bi][:, j * 256:(j + 1) * 256],
                            in1=hbuf[b][:, cg * 256:(cg + 1) * 256], op=ALU.add), r=[BK[bi]], w=[UXB[b]])
            for b in range(NB):
                tb = s * NB + b
                hb = hbuf[b]
                L = rtmp[:, b, :]
                hnTb = hnTs[b % 2]
                UH = UHNTs[b % 2]
                st2 = stat2[:, 4 * b:4 * b + 4]
                US = USTAT2[b]
                P.dma("sync", lambda e, tb=tb, hb=hb: e.dma_start(out=y_out[tb * 128:(tb + 1) * 128, :], in_=hb[:]),
                      UXB[b], r=[UXB[b]], w=[UYB[tb]])
                if PHASES < 2:
                    continue
                P.op("scalar", lambda e, hb=hb, st2=st2: e.activation(out=xs[:], in_=hb[:], func=AF.Square, accum_out=st2[:, 0:1]),
                     r=[UXB[b]], w=[UXS, US])
                P.op("vector", lambda e, st2=st2: e.tensor_scalar(out=st2[:, 1:2], in0=st2[:, 0:1], scalar1=1.0 / D, scalar2=EPS,
                                                                  op0=ALU.mult, op1=ALU.add), r=[US], w=[US])
                P.op("scalar", lambda e, st2=st2: e.activation(out=st2[:, 2:3], in_=st2[:, 1:2], func=AF.Sqrt), r=[US], w=[US])
                P.op("vector", lambda e, st2=st2: e.reciprocal(out=st2[:, 3:4], in_=st2[:, 2:3]), r=[US], w=[US])
                P.op("vector", lambda e, hb=hb, st2=st2: e.scalar_tensor_tensor(out=hb[:], in0=hb[:], scalar=st2[:, 3:4], in1=g2bc[:],
                                                                               op0=ALU.mult, op1=ALU.mult), r=[US, UC], w=[UXB[b]])
                for q4 in range(4):
                    bi = nb()
                    fns = [(lambda e, c=c, bi=bi, q4=q4, hb=hb: e.transpose(
                        out=banks[bi][:, c * 128:(c + 1) * 128], in_=hb[:, (q4 * 4 + c) * 128:(q4 * 4 + c + 1) * 128],
                        identity=ident_f[:])) for c in range(4)]
                    P.group("tensor", fns, r=[UXB[b], UC], w=[BK[bi]])
                    P.op("scalar", lambda e, bi=bi, q4=q4, hnTb=hnTb: e.activation(
                        out=hnTb[:, q4 * 4:(q4 + 1) * 4, :], in_=banks[bi][:, :].rearrange("p (c t) -> p c t", c=4), func=AF.Copy),
                        r=[BK[bi]], **({"w": [UH]} if q4 == 0 else {"wacc": [UH]}))
                bi = nb()
                fns = [(lambda e, k=k, bi=bi, hnTb=hnTb: e.matmul(banks[bi][:, 0:36], lhsT=hnTb[:, k, :], rhs=wr[:, k, :],
                                                                   start=(k == 0), stop=(k == 15))) for k in range(16)]
                P.group("tensor", fns, r=[UH, UC], w=[BK[bi]])
                P.op("vector", lambda e, bi=bi, L=L: e.tensor_tensor(out=L[:, 0:36], in0=banks[bi][:, 0:36], in1=brbc[:], op=ALU.add),
                     r=[BK[bi], UC], w=[URT[b]])
            if PHASES >= 2:
                BL = range(NB)
                Ls = [rtmp[:, b, :] for b in BL]
                tbs = [s * NB + b for b in BL]

                def vop(fn, extra_r=(), w_of=None):
                    for b in BL:
                        L = Ls[b]
                        wl = [URT[b]] if w_of is None else w_of(b)
                        P.op("vector", lambda e, L=L, b=b, fn=fn: fn(e, L, b), r=[URT[b]] + list(extra_r), **wl) \
                            if isinstance(wl, dict) else P.op("vector", lambda e, L=L, b=b, fn=fn: fn(e, L, b),
                                                              r=[URT[b]] + list(extra_r), w=wl)

                def aop(fn):
                    for b in BL:
                        L = Ls[b]
                        P.op("scalar", lambda e, L=L, b=b, fn=fn: fn(e, L, b), r=[URT[b]], w=[URT[b]])

                vop(lambda e, L, b: e.memset(L[:, 40:48], -1e30))
                vop(lambda e, L, b: e.tensor_copy(out=L[:, 40:44], in_=L[:, 0:4]))
                vop(lambda e, L, b: e.max(out=L[:, 48:56], in_=L[:, 40:48]))
                vop(lambda e, L, b: e.tensor_scalar(out=L[:, 56:60], in0=L[:, 0:4], scalar1=L[:, 48:49], scalar2=None, op0=ALU.is_equal))
                vop(lambda e, L, b: e.tensor_scalar(out=L[:, 240:241], in0=L[:, 48:49], scalar1=-1.0, scalar2=None, op0=ALU.mult))
                aop(lambda e, L, b: e.activation(out=L[:, 244:248], in_=L[:, 0:4], func=AF.Exp, bias=L[:, 240:241], scale=1.0,
                                                 accum_out=L[:, 241:242]))
                vop(lambda e, L, b: e.tensor_scalar(out=L[:, 60:64], in0=L[:, 56:60], scalar1=-1.0, scalar2=1e30,
                                                    op0=ALU.add, op1=ALU.mult))
                vop(lambda e, L, b: e.tensor_tensor(
                    out=L[:, 64:96].rearrange("p (g e) -> p g e", g=4), in0=L[:, 4:36].rearrange("p (g e) -> p g e", g=4),
                    in1=L[:, 60:64].unsqueeze(2).to_broadcast([128, 4, 8]), op=ALU.add))
                vop(lambda e, L, b: e.max(out=L[:, 96:104], in_=L[:, 64:96]))
                vop(lambda e, L, b: e.tensor_scalar(out=L[:, 104:136], in0=L[:, 64:96], scalar1=L[:, 96:97], scalar2=None, op0=ALU.is_equal))
                vop(lambda e, L, b: e.tensor_scalar(out=L[:, 136:168], in0=L[:, 64:96], scalar1=L[:, 97:98], scalar2=None, op0=ALU.is_equal))
                vop(lambda e, L, b: e.tensor_tensor(out=L[:, 248:249], in0=L[:, 97:98], in1=L[:, 96:97], op=ALU.subtract))
                aop(lambda e, L, b: e.activation(out=L[:, 249:250], in_=L[:, 248:249], func=AF.Exp))
                vop(lambda e, L, b: e.reciprocal(out=L[:, 242:243], in_=L[:, 241:242]))
                vop(lambda e, L, b: e.tensor_scalar(out=L[:, 250:251], in0=L[:, 249:250], scalar1=1.0, scalar2=None, op0=ALU.add))
                vop(lambda e, L, b: e.reciprocal(out=L[:, 251:252], in_=L[:, 250:251]))
                vop(lambda e, L, b: e.tensor_tensor(out=L[:, 252:253], in0=L[:, 249:250], in1=L[:, 251:252], op=ALU.mult))
                for b in BL:
                    L, tb = Ls[b], tbs[b]
                    P.op("vector", lambda e, L=L, tb=tb: e.tensor_scalar(out=twt[:, tb, 0:1], in0=L[:, 251:252], scalar1=L[:, 242:243],
                                                                         scalar2=None, op0=ALU.mult), r=[URT[b]], w=[UTS[tb]])
                for b in BL:
                    L, tb = Ls[b], tbs[b]
                    P.op("vector", lambda e, L=L, tb=tb: e.tensor_scalar(out=twt[:, tb, 1:2], in0=L[:, 252:253], scalar1=L[:, 242:243],
                                                                         scalar2=None, op0=ALU.mult), r=[URT[b]], wacc=[UTS[tb]])
                for b in BL:
                    L = Ls[b]
                    P.op("vector", lambda e, L=L, b=b: e.tensor_tensor(out=rtb[:, b, :], in0=L[:, 104:136], in1=L[:, 136:168], op=ALU.add),
                         r=[URT[b]], w=[URTB[b]])
                cb = []
                for b in BL:
                    bi = nb()
                    cb.append(bi)
                    P.group("tensor", [lambda e, bi=bi, b=b: e.matmul(banks[bi][:, 0:32], lhsT=lstrict[:], rhs=rtb[:, b, :], start=True, stop=True),
                                       lambda e, bi=bi, b=b: e.matmul(banks[bi][:, 32:64], lhsT=ones_b[:], rhs=rtb[:, b, :], start=True, stop=True)],
                            r=[URTB[b], UC], w=[BK[bi]])
                for b in BL:
                    L, bi = Ls[b], cb[b]
                    P.op("vector", lambda e, bi=bi, L=L: e.tensor_tensor(out=L[:, 168:200], in0=banks[bi][:, 0:32], in1=basebc[:], op=ALU.add),
                         r=[BK[bi], UBASE, URT[b]], w=[URT[b]])
                    P.op("vector", lambda e, bi=bi: e.tensor_tensor(out=basebc[:], in0=banks[bi][:, 32:64], in1=basebc[:], op=ALU.add),
                         r=[BK[bi]], w=[UBASE])
                for kk in range(2):
                    o0 = 104 + 32 * kk
                    vop(lambda e, L, b, o0=o0: e.tensor_tensor(out=L[:, 200:232], in0=L[:, o0:o0 + 32], in1=L[:, 168:200], op=ALU.mult))
                    vop(lambda e, L, b: e.tensor_reduce(out=L[:, 253:254], in_=L[:, 200:232], axis=AX.X, op=ALU.add))
                    vop(lambda e, L, b, o0=o0: e.tensor_tensor(out=L[:, 200:232], in0=L[:, o0:o0 + 32], in1=ecap[:], op=ALU.mult), extra_r=[UC])
                    vop(lambda e, L, b: e.tensor_reduce(out=L[:, 254:255], in_=L[:, 200:232], axis=AX.X, op=ALU.add))
                    vop(lambda e, L, b: e.tensor_scalar(out=L[:, 255:256], in0=L[:, 253:254], scalar1=float(CAP), scalar2=None, op0=ALU.is_lt))
                    vop(lambda e, L, b: e.tensor_tensor(out=L[:, 256:257], in0=L[:, 253:254], in1=L[:, 254:255], op=ALU.add))
                    vop(lambda e, L, b: e.tensor_tensor(out=L[:, 256:257], in0=L[:, 256:257], in1=pidx[:], op=ALU.subtract), extra_r=[UC])
                    vop(lambda e, L, b: e.scalar_tensor_tensor(out=L[:, 257:258], in0=L[:, 256:257], scalar=L[:, 255:256],
                                                               in1=pidx[:], op0=ALU.mult, op1=ALU.add), extra_r=[UC])
                    for b in BL:
                        L, tb = Ls[b], tbs[b]
                        P.op("vector", lambda e, L=L, tb=tb, kk=kk: e.tensor_copy(out=tslot[:, tb, kk:kk + 1], in_=L[:, 257:258]),
                             r=[URT[b]], wacc=[UTS[tb]])
                for b in BL:
                    tb = tbs[b]
                    hb = hbuf[b]
                    P.op("scalar", lambda e, hb=hb: e.activation(out=xs[:], in_=hb[:], func=AF.Copy), r=[UXB[b]], w=[UXS])
                    for kk in range(2):
                        P.dma("gpsimd", lambda e, tb=tb, kk=kk: e.indirect_dma_start(
                            out=xg, out_offset=bass.IndirectOffsetOnAxis(ap=tslot[:, tb, kk:kk + 1], axis=0),
                            in_=xs[:], in_offset=None), UXS, r=[UXS, UTS[tb], UXGZ], wacc=[UXG])

        P.barrier()
        p1.close()

        if PHASES >= 2:
            p2 = contextlib.ExitStack()
            sb2 = lambda name, shape, d: sb(name, shape, d, p2)
            xgt = [sb2("xgt%d" % i, [128, D], BF16) for i in range(4)]
            xgT = sb2("xgT", [128, 16, CAP], BF16)
            wgu = [sb2("wgu%d" % i, [128, 16, 512], BF16) for i in range(4)]
            wd = [sb2("wd%d" % i, [128, 8, 1024], BF16) for i in range(2)]
            hT = sb2("hT", [128, 8, CAP], BF16)
            sil = [sb2("sil%d" % i, [128, CAP], F32) for i in range(2)]
            yet = [sb2("yet%d" % i, [128, D], F32) for i in range(4)]
            zt = sb2("zt", [128, D], F32)
            UXGT = [U("xgt%d" % i) for i in range(4)]
            UXGTT = [U("xgT%d" % i) for i in range(4)]
            UWGU = [U("wgu%d" % i) for i in range(4)]
            UWD = [U("wd%d" % i) for i in range(2)]
            UHT = [U("hT%d" % f) for f in range(8)]
            USIL = [U("sil%d" % i) for i in range(2)]
            UYET = [U("yet%d" % i) for i in range(4)]
            UYE = U("ye")
            UZT = U("zt")
            P.op("vector", lambda e: e.memset(zt[:], 0.0), w=[UZT])
            P.dma("sync", lambda e: e.dma_start(out=ye[NSLOT:NSLOT + 128, :], in_=zt[:]), UZT, r=[UZT], wacc=[UYE])
            gslot = {"i": 0}
            dslot = {"i": 0}
            sslot = {"i": 0}
            for ex in range(NE):
                for sbk in range(4):
                    r0 = ex * CAP + sbk * 128
                    P.dma("sync", lambda e, r0=r0, sbk=sbk: e.dma_start(out=xgt[sbk][:], in_=xg[r0:r0 + 128, :]),
                          UXGT[sbk], r=[UXG], w=[UXGT[sbk]])
                    for half in range(2):
                        bi = nb()
                        fns = [(lambda e, c=c, bi=bi, half=half, sbk=sbk: e.transpose(
                            out=banks_bf[bi][:, c * 128:(c + 1) * 128],
                            in_=xgt[sbk][:, (half * 8 + c) * 128:(half * 8 + c + 1) * 128], identity=ident_b[:])) for c in range(8)]
                        P.group("tensor", fns, r=[UXGT[sbk], UC], w=[BK[bi]])
                        P.op("vector", lambda e, bi=bi, half=half, sbk=sbk: e.tensor_copy(
                            out=xgT[:, half * 8:(half + 1) * 8, sbk * 128:(sbk + 1) * 128],
                            in_=banks_bf[bi][:].rearrange("p (c t) -> p c t", c=8)), r=[BK[bi]], w=[UXGTT[sbk]])
                for pc in range(2):
                    gi = gslot["i"]
                    ui = (gi + 1) % 4
                    gslot["i"] = (gi + 2) % 4
                    P.dma("gpsimd", lambda e, ex=ex, pc=pc, gi=gi: e.dma_start(
                        out=wgu[gi][:], in_=w_gate[ex].rearrange("(k p) n -> p k n", p=128)[:, :, pc * 512:(pc + 1) * 512]),
                        UWGU[gi], w=[UWGU[gi]])
                    P.dma("gpsimd", lambda e, ex=ex, pc=pc, ui=ui: e.dma_start(
                        out=wgu[ui][:], in_=w_up[ex].rearrange("(k p) n -> p k n", p=128)[:, :, pc * 512:(pc + 1) * 512]),
                        UWGU[ui], w=[UWGU[ui]])
                    for fc in range(4):
                        f = pc * 4 + fc
                        bg = nb()
                        fns = [(lambda e, k=k, bg=bg, gi=gi, fc=fc: e.matmul(
                            banks[bg][:, :], lhsT=wgu[gi][:, k, fc * 128:(fc + 1) * 128], rhs=xgT[:, k, :],
                            start=(k == 0), stop=(k == 15))) for k in range(16)]
                        P.group("tensor", fns, r=[UWGU[gi]] + UXGTT, w=[BK[bg]])
                        bu = nb()
                        fns = [(lambda e, k=k, bu=bu, ui=ui, fc=fc: e.matmul(
                            banks[bu][:, :], lhsT=wgu[ui][:, k, fc * 128:(fc + 1) * 128], rhs=xgT[:, k, :],
                            start=(k == 0), stop=(k == 15))) for k in range(16)]
                        P.group("tensor", fns, r=[UWGU[ui]] + UXGTT, w=[BK[bu]])
                        si = sslot["i"]
                        sslot["i"] = 1 - si
                        P.op("scalar", lambda e, bg=bg, si=si: e.activation(out=sil[si][:], in_=banks[bg][:, :], func=AF.Silu),
                             r=[BK[bg]], w=[USIL[si]])
                        P.op("vector", lambda e, bu=bu, si=si, f=f: e.tensor_tensor(out=hT[:, f, :], in0=banks[bu][:, :], in1=sil[si][:],
                                                                                 op=ALU.mult), r=[BK[bu], USIL[si]], w=[UHT[f]])
                for cg2 in range(2):
                    di = dslot["i"]
                    dslot["i"] = 1 - di
                    P.dma("gpsimd", lambda e, ex=ex, cg2=cg2, di=di: e.dma_start(
                        out=wd[di][:], in_=w_down[ex].rearrange("(k p) n -> p k n", p=128)[:, :, cg2 * 1024:(cg2 + 1) * 1024]),
                        UWD[di], w=[UWD[di]])
                    for cgh in range(2):
                        cg = cg2 * 2 + cgh
                        for sbk in range(4):
                            bi = nb()
                            fns = [(lambda e, f=f, bi=bi, di=di, sbk=sbk, cgh=cgh: e.matmul(
                                banks[bi][:, :], lhsT=hT[:, f, sbk * 128:(sbk + 1) * 128], rhs=wd[di][:, f, cgh * 512:(cgh + 1) * 512],
                                start=(f == 0), stop=(f == 7))) for f in range(8)]
                            P.group("tensor", fns, r=[UWD[di]] + UHT, w=[BK[bi]])
                            P.op("scalar", lambda e, bi=bi, sbk=sbk, cg=cg: e.activation(
                                out=yet[sbk][:, cg * 512:(cg + 1) * 512], in_=banks[bi][:, :], func=AF.Copy),
                                r=[BK[bi]], w=[UYET[sbk]])
                for sbk in range(4):
                    r0 = ex * CAP + sbk * 128
                    P.dma("sync", lambda e, r0=r0, sbk=sbk: e.dma_start(out=ye[r0:r0 + 128, :], in_=yet[sbk][:]),
                          UYET[sbk], r=[UYET[sbk]], wacc=[UYE])
            P.barrier()
            p2.close()

            p3 = contextlib.ExitStack()
            sb3 = lambda name, shape, d: sb(name, shape, d, p3)
            hb3 = [sb3("hb3_%d" % i, [128, D], F32) for i in range(2)]
            r1 = [sb3("r1_%d" % i, [128, D], F32) for i in range(2)]
            r2 = [sb3("r2_%d" % i, [128, D], F32) for i in range(2)]
            UH3 = [U("h3_%d" % i) for i in range(2)]
            UR1 = [U("r1_%d" % i) for i in range(2)]
            UR2 = [U("r2_%d" % i) for i in range(2)]
            for tb in range(NST * NB):
                i = tb % 2
                P.dma("sync", lambda e, tb=tb, i=i: e.dma_start(out=hb3[i][:], in_=y_out[tb * 128:(tb + 1) * 128, :]),
                      UH3[i], r=[UYB[tb]], w=[UH3[i]])
                P.dma("gpsimd", lambda e, tb=tb, i=i: e.indirect_dma_start(
                    out=r1[i][:], out_offset=None, in_=ye,
                    in_offset=bass.IndirectOffsetOnAxis(ap=tslot[:, tb, 0:1], axis=0)), UR1[i], r=[UYE, UTS[tb]], w=[UR1[i]])
                P.dma("gpsimd", lambda e, tb=tb, i=i: e.indirect_dma_start(
                    out=r2[i][:], out_offset=None, in_=ye,
                    in_offset=bass.IndirectOffsetOnAxis(ap=tslot[:, tb, 1:2], axis=0)), UR2[i], r=[UYE, UTS[tb]], w=[UR2[i]])
                P.op("vector", lambda e, tb=tb, i=i: e.scalar_tensor_tensor(
                    out=hb3[i][:], in0=r1[i][:], scalar=twt[:, tb, 0:1], in1=hb3[i][:], op0=ALU.mult, op1=ALU.add),
                    r=[UR1[i], UTS[tb]], w=[UH3[i]])
                P.op("vector", lambda e, tb=tb, i=i: e.scalar_tensor_tensor(
                    out=hb3[i][:], in0=r2[i][:], scalar=twt[:, tb, 1:2], in1=hb3[i][:], op0=ALU.mult, op1=ALU.add),
                    r=[UR2[i], UTS[tb]], w=[UH3[i]])
                P.dma("sync", lambda e, tb=tb, i=i: e.dma_start(out=y_out[tb * 128:(tb + 1) * 128, :], in_=hb3[i][:]),
                      UH3[i], r=[UH3[i]], w=[UYB[tb]])
            P.barrier()
            p3.close()

        block = st.enter_context(nc.Block())
        P.emit(block)
    return nc


def _core_layout(c):
    b = c // 4
    qtr = c % 4
    sts = []
    for i in range(2):
        sts.append(("p", b, qtr * 1024 + i * T, 4096))
    for i in range(8):
        sts.append(("s", b, qtr * 4096 + i * T, 16384))
    return sts


def _rope_table(pos):
    half = 16
    inv = (np.float32(500000.0) ** (-np.arange(half, dtype=np.float32) / np.float32(half))).astype(np.float32)
    ang = (pos.astype(np.float32)[:, None] * inv[None, :]).astype(np.float32)
    cos = np.cos(ang).astype(np.float32)
    sin = np.sin(ang).astype(np.float32)
    return np.concatenate([cos, cos, -sin, sin], axis=1)


_NC_CACHE = {}


def kernel(x_prompt, x_sample, norm1_g, w_in, pool_w, pool_scale, pool_proj, q_norm_g, k_norm_g, sink,
           attn_proj, w_out, norm2_g, router_group_w, router_group_b, router_expert_w, router_expert_b,
           w_gate, w_up, w_down):
    f32 = np.float32
    xp = np.asarray(x_prompt, f32)
    xsm = np.asarray(x_sample, f32)
    bc = lambda v, n: np.ascontiguousarray(np.broadcast_to(np.asarray(v, f32).reshape(1, n), (128, n)))
    shared = {
        "w_in": np.ascontiguousarray(np.asarray(w_in, f32)[0]),
        "pool_w": np.ascontiguousarray(np.asarray(pool_w, f32)[0]),
        "pool_proj": np.ascontiguousarray(np.asarray(pool_proj, f32)[0]),
        "attn_proj": np.ascontiguousarray(np.asarray(attn_proj, f32)[0]),
        "w_out": np.ascontiguousarray(np.asarray(w_out, f32)[0]),
        "w_gate": np.ascontiguousarray(np.asarray(w_gate, f32)[0]),
        "w_up": np.ascontiguousarray(np.asarray(w_up, f32)[0]),
        "w_down": np.ascontiguousarray(np.asarray(w_down, f32)[0]),
        "g1T": np.ascontiguousarray(np.asarray(norm1_g, f32)[0].reshape(16, 128).T),
        "g2bc": bc(np.asarray(norm2_g)[0], D),
        "pscT": np.ascontiguousarray(np.asarray(pool_scale, f32)[0].reshape(8, 128).T),
        "qgbc": bc(np.asarray(q_norm_g)[0], 128),
        "kgbc": bc(np.asarray(k_norm_g)[0], 128),
        "sinkbc": bc(np.asarray(sink)[0], 16),
        "wr": np.ascontiguousarray(np.concatenate([np.asarray(router_group_w, f32)[0], np.asarray(router_expert_w, f32)[0]], axis=1)),
        "brbc": bc(np.concatenate([np.asarray(router_group_b, f32)[0], np.asarray(router_expert_b, f32)[0]]), 36),
        "ident": np.eye(128, dtype=f32),
        "lstrict": np.triu(np.ones((128, 128), f32), 1),
        "ones": np.ones((128, 128), f32),
        "ecap": bc(np.arange(NE, dtype=f32) * CAP, NE),
        "pidx": (DUMP + np.arange(128, dtype=f32)).reshape(128, 1),
    }
    jj = np.arange(128)[:, None]
    qq = np.arange(128)[None, :]
    mL = np.where(qq <= jj, 0.0, NEG).astype(f32)
    mR = np.where(jj <= qq, 0.0, NEG).astype(f32)
    shared["masks"] = np.ascontiguousarray(np.concatenate([np.tile(mL, (1, 4)), np.tile(mR, (1, 4))], axis=1))

    in_maps = []
    for c in range(NCORE):
        sts = _core_layout(c)
        x_in = np.zeros((NST, TH, D), f32)
        rope = np.zeros((NST, TH, 64), f32)
        kb = np.zeros((128, NST * NBH), f32)
        invc = np.zeros((NST, 128, 4 * T), f32)
        for si, (which, b, s0, S) in enumerate(sts):
            src = xp[b] if which == "p" else xsm[b]
            lo = s0 - 128
            hi = s0 + T + 128
            a = max(lo, 0)
            z = min(hi, S)
            x_in[si, a - lo:z - lo] = src[a:z]
            pos = np.arange(lo, hi)
            rope[si] = _rope_table(np.clip(pos, 0, S - 1))
            for blk in range(NBH):
                p0 = lo + blk * 128
                if p0 < 0 or p0 >= S:
                    kb[:, si * NBH + blk] = NEG
            t = np.arange(s0, s0 + T)
            for g, w in enumerate((2, 4, 8, 16)):
                h = w // 2
                cnt = np.clip(t + h, 0, S) - np.clip(t - h, 0, S)
                invc[si, :, g * T:(g + 1) * T] = (1.0 / cnt.astype(f32))[None, :]
        m = dict(shared)
        m.update({"x_in": x_in, "rope": rope, "kbias": kb, "invcnt": invc})
        in_maps.append(m)

    if "nc" not in _NC_CACHE:
        _NC_CACHE["nc"] = build_program()
    nc = _NC_CACHE["nc"]
    res = run_bass_kernel_spmd(nc, in_maps, core_ids=list(range(NCORE)))
    y_prompt = np.zeros((2, 4096, D), f32)
    y_sample = np.zeros((2, 16384, D), f32)
    for c in range(NCORE):
        y = np.asarray(res.results[c]["y"], f32)
        b = c // 4
        qtr = c % 4
        y_prompt[b, qtr * 1024:(qtr + 1) * 1024] = y[0:1024]
        y_sample[b, qtr * 4096:(qtr + 1) * 4096] = y[1024:5120]
    return (y_prompt, y_sample)
```

```python
import contextlib
import numpy as np
import concourse.bass as bass
import concourse.mybir as mybir
from concourse.bass_utils import run_bass_kernel_spmd

F32 = mybir.dt.float32
BF16 = mybir.dt.bfloat16
I32 = mybir.dt.int32
AF = mybir.ActivationFunctionType
ALU = mybir.AluOpType
AX = mybir.AxisListType

ENGS = ("sync", "tensor", "scalar", "vector", "gpsimd")

D = 2048
NCORE = 8
NB = 4
NBH = NB + 2
T = NB * 128
TH = NBH * 128
NST = 10
NTOK = NST * T
NE = 32
CAP = 512
NSLOT = NE * CAP
DUMP = NSLOT
DFF = 1024
EPS = 1e-6
NEG = -30000.0
import os
PHASES = int(os.environ.get('MK_PHASES', '3'))


class Sem:
    def __init__(self, handle, name):
        self.h = handle
        self.name = name
        self.count = 0


class U:
    def __init__(self, name):
        self.name = name
        self.lw = []
        self.rd = []
        self.sem = None


class Prog:
    def __init__(self, nc, stack):
        self.nc = nc
        self.stack = stack
        self.q = {e: [] for e in ENGS}
        self.sems = []
        self.esem = {}
        for e in ("tensor", "scalar", "vector", "gpsimd"):
            self.esem[e] = self.new_sem("e_" + e)
        self.waited = {e: {} for e in ENGS}

    def new_sem(self, name):
        h = self.stack.enter_context(self.nc.semaphore(name))
        s = Sem(h, name)
        self.sems.append(s)
        return s

    def _waits(self, eng, r, w, extra=()):
        need = {}

        def add(t):
            s, v = t
            if need.get(s, 0) < v:
                need[s] = v
        for u in r:
            for t in u.lw:
                add(t)
        for u in w:
            for t in u.lw:
                add(t)
            for t in u.rd:
                add(t)
        for t in extra:
            add(t)
        out = []
        wd = self.waited[eng]
        for s, v in need.items():
            if wd.get(s, 0) >= v:
                continue
            wd[s] = v
            out.append((s, v))
        return out

    def _record(self, ticket, r, w, wacc):
        for u in r:
            u.rd.append(ticket)
            if len(u.rd) > 64:
                u.rd = _compress(u.rd)
        for u in w:
            u.lw = [ticket]
            u.rd = []
        for u in wacc:
            u.lw.append(ticket)
            if len(u.lw) > 64:
                u.lw = _compress(u.lw)

    def op(self, eng, fn, r=(), w=(), wacc=(), extra=()):
        waits = self._waits(eng, list(r) + list(wacc), w, extra)
        s = self.esem[eng]
        s.count += 1
        ticket = (s, s.count)
        self._record(ticket, r, w, wacc)

        def run(e, waits=waits, fn=fn, s=s):
            for (ws, v) in waits:
                e.wait_ge(ws.h, v)
            fn(e).then_inc(s.h, 1)
        self.q[eng].append(run)
        return ticket

    def group(self, eng, fns, r=(), w=(), extra=()):
        waits = self._waits(eng, r, w, extra)
        s = self.esem[eng]
        s.count += 1
        ticket = (s, s.count)
        self._record(ticket, r, w, ())

        def run(e, waits=waits, fns=fns, s=s):
            for (ws, v) in waits:
                e.wait_ge(ws.h, v)
            for f in fns[:-1]:
                f(e)
            fns[-1](e).then_inc(s.h, 1)
        self.q[eng].append(run)
        return ticket

    def dma(self, eng, fn, su, r=(), w=(), wacc=(), extra=()):
        if su.sem is None:
            su.sem = self.new_sem("d_" + su.name)
        waits = self._waits(eng, list(r) + list(wacc), w, extra)
        s = su.sem
        s.count += 16
        ticket = (s, s.count)
        self._record(ticket, r, w, wacc)

        def run(e, waits=waits, fn=fn, s=s):
            for (ws, v) in waits:
                e.wait_ge(ws.h, v)
            fn(e).then_inc(s.h, 16)
        self.q[eng].append(run)
        return ticket

    def barrier(self, engs=ENGS):
        for eng in engs:
            lst = []
            wd = self.waited[eng]
            for s in self.sems:
                if s.count > 0 and wd.get(s, 0) < s.count:
                    wd[s] = s.count
                    lst.append((s, s.count))

            def run(e, lst=lst):
                for (ws, v) in lst:
                    e.wait_ge(ws.h, v)
            self.q[eng].append(run)

    def emit(self, block):
        q = self.q

        @block.sync
        def _(e):
            for f in q["sync"]:
                f(e)

        @block.tensor
        def _(e):
            for f in q["tensor"]:
                f(e)

        @block.scalar
        def _(e):
            for f in q["scalar"]:
                f(e)

        @block.vector
        def _(e):
            for f in q["vector"]:
                f(e)

        @block.gpsimd
        def _(e):
            for f in q["gpsimd"]:
                f(e)


def _compress(tickets):
    need = {}
    for s, v in tickets:
        if need.get(s, 0) < v:
            need[s] = v
    return list(need.items())


def handoff(olds, news):
    ts = []
    for ou in olds:
        ts.extend(ou.lw)
        ts.extend(ou.rd)
    ts = _compress(ts)
    for nu in news:
        nu.rd.extend(ts)


def build_program():
    nc = bass.Bass("TRN2", target_bir_lowering=False)
    dt = nc.dram_tensor

    def din(name, shape, d=F32):
        return dt(name, list(shape), d, kind="ExternalInput").ap()

    x_in = din("x_in", [NST, TH, D])
    rope = din("rope", [NST, TH, 64])
    kbias_d = din("kbias", [128, NST * NBH])
    invcnt_d = din("invcnt", [NST, 128, 4 * T])
    w_in = din("w_in", [D, 8192])
    pool_w = din("pool_w", [4, 256, 256])
    pool_proj = din("pool_proj", [1024, D])
    attn_proj = din("attn_proj", [D, D])
    w_out = din("w_out", [D, D])
    w_gate = din("w_gate", [NE, D, DFF])
    w_up = din("w_up", [NE, D, DFF])
    w_down = din("w_down", [NE, DFF, D])
    g1T_d = din("g1T", [128, 16])
    g2_d = din("g2bc", [128, D])
    pscT_d = din("pscT", [128, 8])
    qg_d = din("qgbc", [128, 128])
    kg_d = din("kgbc", [128, 128])
    sink_d = din("sinkbc", [128, 16])
    wr_d = din("wr", [D, 36])
    br_d = din("brbc", [128, 36])
    ident_d = din("ident", [128, 128])
    masks_d = din("masks", [128, 2 * 512])
    lstrict_d = din("lstrict", [128, 128])
    ones_d = din("ones", [128, 128])
    ecap_d = din("ecap", [128, NE])
    pidx_d = din("pidx", [128, 1])

    y_out = dt("y", [NTOK, D], F32, kind="ExternalOutput").ap()
    xg = dt("xg", [NSLOT + 128, D], BF16, kind="Internal").ap()
    ye = dt("ye", [NSLOT + 128, D], F32, kind="Internal").ap()

    with contextlib.ExitStack() as st:
        P = Prog(nc, st)
        block = None

        def sb(name, shape, d, stack=st):
            return stack.enter_context(nc.sbuf_tensor("s_" + name, list(shape), d))

        banks = [st.enter_context(nc.psum_tensor("bank%d" % i, [128, 512], F32)) for i in range(8)]
        banks_bf = [b.bitcast(BF16) for b in banks]
        BK = [U("bk%d" % i) for i in range(8)]
        bstate = {"i": 0}

        def nb():
            i = bstate["i"]
            bstate["i"] = (i + 1) % 8
            return i

        UC = U("const")
        g1T = sb("g1T", [128, 16], F32)
        g2bc = sb("g2bc", [128, D], F32)
        pscT = sb("pscT", [128, 8], F32)
        qgbc = sb("qgbc", [128, 128], F32)
        kgbc = sb("kgbc", [128, 128], F32)
        esink = sb("esink", [128, 16], F32)
        wr = sb("wr", [128, 16, 36], F32)
        brbc = sb("brbc", [128, 36], F32)
        ident_f = sb("ident_f", [128, 128], F32)
        ident_b = sb("ident_b", [128, 128], BF16)
        masks = sb("masks", [128, 2, 512], BF16)
        lstrict = sb("lstrict", [128, 128], BF16)
        ones_b = sb("ones_b", [128, 128], BF16)
        ecap = sb("ecap", [128, NE], F32)
        pidx = sb("pidx", [128, 1], F32)
        kbias = sb("kbias", [128, NST * NBH], F32)
        poolw = sb("poolw", [128, 8, 256], BF16)
        tslot = sb("tslot", [128, NST * NB, 2], I32)
        twt = sb("twt", [128, NST * NB, 2], F32)
        basebc = sb("basebc", [128, NE], F32)
        UTS = [U("ts%d" % i) for i in range(NST * NB)]
        UBASE = U("base")

        def cload(eng, out, in_):
            P.dma(eng, lambda e: e.dma_start(out=out, in_=in_), UC, wacc=[UC])

        cload("sync", g1T[:], g1T_d)
        cload("sync", g2bc[:], g2_d)
        cload("sync", pscT[:], pscT_d)
        cload("sync", qgbc[:], qg_d)
        cload("sync", kgbc[:], kg_d)
        cload("sync", esink[:], sink_d)
        cload("sync", wr[:], wr_d.rearrange("(k p) n -> p k n", p=128))
        cload("sync", brbc[:], br_d)
        cload("sync", ident_f[:], ident_d)
        cload("sync", ecap[:], ecap_d)
        cload("sync", pidx[:], pidx_d)
        cload("sync", kbias[:], kbias_d)
        cload("gpsimd", ident_b[:], ident_d)
        cload("gpsimd", masks[:], masks_d.rearrange("p (a n) -> p a n", a=2))
        cload("gpsimd", lstrict[:], lstrict_d)
        cload("gpsimd", ones_b[:], ones_d)
        cload("gpsimd", poolw[:], pool_w.rearrange("g (cc p) d -> p (g cc) d", p=128))
        P.op("scalar", lambda e: e.activation(out=esink[:], in_=esink[:], func=AF.Exp), r=[UC], w=[UC])
        P.op("vector", lambda e: e.memset(basebc[:], 0.0), w=[UBASE])

        UXGZ = U("xgz")

        p1 = contextlib.ExitStack()
        sb1 = lambda name, shape, d: sb(name, shape, d, p1)
        xnT = sb1("xnT", [128, 16, TH], BF16)
        NWS = 5
        wbuf = [sb1("wbuf%d" % i, [128, 16, 256], BF16) for i in range(NWS)]
        hbuf = [sb1("hbuf%d" % i, [128, D], F32) for i in range(4)]
        xs = sb1("xs", [128, D], BF16)
        yT = sb1("yT", [128, 8, T], BF16)
        qT = sb1("qT", [128, 16, T], BF16)
        mT = qT
        kT = sb1("kT", [128, 4, TH], BF16)
        v_sb = sb1("v_sb", [128, NBH, 512], BF16)
        oT = sb1("oT", [128, 16, T], BF16)
        mixT = oT[:, 0:8, :]
        R1 = sb1("R1", [128, 11264], BF16)
        R2x = sb1("R2x", [128, 1024], BF16)
        stat = sb1("stat", [128, 64], F32)
        ropet = sb1("ropet", [128, NBH, 64], F32)
        rtmp = sb1("rtmp", [128, NB, 264], F32)
        rtb = sb1("rtb", [128, NB, 32], BF16)
        rti = sb1("rti", [128, 8], I32)

        def view(ap2d, d, pattern=None, **kw):
            v = ap2d.bitcast(d) if d != BF16 else ap2d
            if pattern:
                v = v.rearrange(pattern, **kw)
            return v

        invc = view(R1[:, 0:4096], F32, "p (g t) -> p g t", g=4)
        xpc = [view(R1[:, 4096 + i * 1536: 4096 + (i + 1) * 1536], F32) for i in range(2)]
        sA = view(R1[:, 7168:8704], F32)
        sB = view(R1[:, 8704:10240], F32)
        tmpw = view(R1[:, 10240:11264], F32)
        sg = view(R1[:, 0:2048], F32, "p (c t) -> p c t", c=2)
        tacc = view(R1[:, 2048:4096], F32, "p (c t) -> p c t", c=2)
        hnT = view(R1[:, 0:4096], F32, "p (k t) -> p k t", k=16)
        h2b = hbuf[2][:].bitcast(BF16)
        h3b = hbuf[3][:].bitcast(BF16)
        pT = [view(h2b[:, i * 1536:(i + 1) * 1536], BF16, "p (j n) -> p j n", j=3) for i in range(2)]
        sqj = h3b[:, 0:2048]
        ddt = view(R2x[:, 0:1024], F32)
        qr2 = [view(R1[:, i * 512:(i + 1) * 512], F32) for i in range(6)]
        qbf2 = [R1[:, 3072 + i * 256: 3072 + (i + 1) * 256] for i in range(6)]
        rotA2 = [view(R1[:, 4608 + i * 128: 4608 + (i + 1) * 128], F32, "p (h d) -> p h d", h=2) for i in range(6)]
        rotB2 = [view(R1[:, 5376 + i * 128: 5376 + (i + 1) * 128], F32, "p (h d) -> p h d", h=2) for i in range(6)]

        UXB = [U("xb%d" % i) for i in range(4)]
        UXS = U("xs")
        USTAT = U("stat")
        UXN = [U("xn%d" % b) for b in range(NBH)]
        UW = [U("w%d" % i) for i in range(5)]
        UXPC = [U("xpc%d" % i) for i in range(2)]
        USA, USB, UTMPW, UINVC = U("sA"), U("sB"), U("tmpw"), U("invc")
        UMIX = [U("mix%d" % c) for c in range(8)]
        UYT = [U("yT%d" % c) for c in range(8)]
        UQR = [U("qr%d" % i) for i in range(6)]
        UQBF = [U("qbf%d" % i) for i in range(6)]
        UROT = [U("rot%d" % i) for i in range(6)]
        UB2 = UQR + UQBF + UROT
        UQT = [[U("qT%d_%d" % (b, g)) for g in range(4)] for b in range(NB)]
        UKT = [U("kT%d" % b) for b in range(NBH)]
        UV = [U("v%d" % b) for b in range(NBH)]
        UPT = [[U("pT%d_%d" % (i, j)) for j in range(3)] for i in range(2)]
        UDD = U("dd")
        USQJ = U("sqj")
        UOT = [[U("oT%d_%d" % (b, k)) for k in range(4)] for b in range(NB)]
        USG = [U("sg%d" % c) for c in range(2)]
        UTACC = [U("tacc%d" % c) for c in range(2)]
        UMT = [U("mT%d" % c) for c in range(16)]
        UHNT = U("hnT")
        UROPE = U("rope")
        URT = [U("rt%d" % b) for b in range(NB)]
        URTB = [U("rtb%d" % b) for b in range(NB)]
        URTI = U("rti")
        UXG = U("xg")
        UYB = [U("y%d" % i) for i in range(NST * NB)]
        wslot = {"i": 0}
        if PHASES >= 2:
            allOT0 = [UOT[qb][kh] for qb in range(NB) for kh in (2, 3)]
            UZ0 = U("zt0")
            P.op("vector", lambda e: e.memset(oT[:, 8:16, :], 0.0), w=allOT0)
            zsrc = oT[:, 8:16, :].rearrange("p k t -> p (k t)").rearrange("p (j d) -> p j d", d=D)
            nfull = (NSLOT + 128) // 256
            for kz in range(nfull):
                P.dma("sync", lambda e, kz=kz: e.dma_start(
                    out=xg[kz * 256:(kz + 1) * 256, :].rearrange("(j p) d -> p j d", p=128), in_=zsrc),
                    UZ0, r=allOT0, wacc=[UXGZ])
            rem0 = nfull * 256
            if rem0 < NSLOT + 128:
                nrem = (NSLOT + 128 - rem0) // 128
                P.dma("sync", lambda e: e.dma_start(
                    out=xg[rem0:rem0 + nrem * 128, :].rearrange("(j p) d -> p j d", p=128), in_=zsrc[:, 0:nrem, :]),
                    UZ0, r=allOT0, wacc=[UXGZ])

        def load_w(src_ap, kc):
            i = wslot["i"]
            wslot["i"] = (i + 1) % NWS
            P.dma("gpsimd", lambda e, i=i: e.dma_start(out=wbuf[i][:, 0:kc, :],
                                                       in_=src_ap.rearrange("(k p) n -> p k n", p=128)),
                  UW[i], w=[UW[i]])
            return i

        xslot = {"i": 0}

        for s in range(NST):
            handoff(UXB[2:4], [u for row in UPT for u in row] + [USQJ])
            P.dma("sync", lambda e, s=s: e.dma_start(out=ropet[:], in_=rope[s].rearrange("(b p) c -> p b c", p=128)),
                  UROPE, w=[UROPE])
            for b in range(NBH):
                xi = xslot["i"]
                xslot["i"] = 1 - xi
                xb = hbuf[xi]
                P.dma("sync", lambda e, s=s, b=b, xb=xb: e.dma_start(out=xb[:], in_=x_in[s, b * 128:(b + 1) * 128, :]),
                      UXB[xi], w=[UXB[xi]])
                P.op("scalar", lambda e, xb=xb: e.activation(out=sqj, in_=xb[:], func=AF.Square, accum_out=stat[:, 0:1]),
                     r=[UXB[xi]], w=[USQJ, USTAT])
                P.op("vector", lambda e: e.tensor_scalar(out=stat[:, 1:2], in0=stat[:, 0:1], scalar1=1.0 / D, scalar2=EPS,
                                                         op0=ALU.mult, op1=ALU.add), r=[USTAT], w=[USTAT])
                P.op("scalar", lambda e: e.activation(out=stat[:, 2:3], in_=stat[:, 1:2], func=AF.Sqrt), r=[USTAT], w=[USTAT])
                P.op("vector", lambda e: e.reciprocal(out=stat[:, 3:4], in_=stat[:, 2:3]), r=[USTAT], w=[USTAT])
                P.op("scalar", lambda e, xb=xb: e.activation(out=xs[:], in_=xb[:], func=AF.Copy, scale=stat[:, 3:4]),
                     r=[UXB[xi], USTAT], w=[UXS])
                for half in range(2):
                    bi = nb()
                    fns = [(lambda e, c=c, bi=bi, half=half: e.transpose(
                        out=banks_bf[bi][:, c * 128:(c + 1) * 128],
                        in_=xs[:, (half * 8 + c) * 128:(half * 8 + c + 1) * 128], identity=ident_b[:])) for c in range(8)]
                    P.group("tensor", fns, r=[UXS, UC], w=[BK[bi]])
                    P.op("vector", lambda e, bi=bi, half=half, b=b: e.tensor_tensor(
                        out=xnT[:, half * 8:(half + 1) * 8, b * 128:(b + 1) * 128],
                        in0=banks_bf[bi][:].rearrange("p (c t) -> p c t", c=8),
                        in1=g1T[:, half * 8:(half + 1) * 8].unsqueeze(2).to_broadcast([128, 8, 128]), op=ALU.mult),
                        r=[BK[bi], UC], w=[UXN[b]])

            handoff(USG + UTACC + [UHNT] + UB2, [UINVC] + UXPC + [USA, USB, UTMPW])
            handoff([UOT[qb][kh] for qb in range(NB) for kh in (0, 1)], UMIX)
            P.dma("sync", lambda e, s=s: e.dma_start(out=invc, in_=invcnt_d[s].rearrange("p (g t) -> p g t", g=4)),
                  UINVC, w=[UINVC])
            for pc in range(4):
                wi = load_w(w_in[:, pc * 256:(pc + 1) * 256], 16)
                for cc in range(2):
                    ch = pc * 2 + cc
                    g = ch // 2
                    xi2 = ch % 2
                    xp = xpc[xi2]
                    for half in range(2):
                        bi = nb()
                        t0 = half * 384
                        fns = [(lambda e, k=k, bi=bi, wi=wi, cc=cc, t0=t0: e.matmul(
                            banks[bi][:, 0:384], lhsT=wbuf[wi][:, k, cc * 128:(cc + 1) * 128],
                            rhs=xnT[:, k, t0:t0 + 384], start=(k == 0), stop=(k == 15))) for k in range(16)]
                        P.group("tensor", fns, r=[UW[wi]] + UXN, w=[BK[bi]])
                        P.op("scalar", lambda e, bi=bi, xp=xp, t0=t0: e.activation(out=xp[:, t0:t0 + 384], in_=banks[bi][:, 0:384],
                                                                                 func=AF.Copy),
                             r=[BK[bi]], w=[UXPC[xi2]])
                    P.op("vector", lambda e, xp=xp: e.tensor_tensor(out=sA[:, 1:TH], in0=xp[:, 0:TH - 1], in1=xp[:, 1:TH], op=ALU.add),
                         r=[UXPC[xi2]], w=[USA])
                    cur, ucur, oth, uoth = sA, USA, sB, USB
                    lo = 1
                    for lvl in range(g):
                        sh = 1 << lvl
                        nlo = lo + sh
                        nhi = TH - lo - sh + 1
                        P.op("vector", lambda e, cur=cur, oth=oth, sh=sh, nlo=nlo, nhi=nhi: e.tensor_tensor(
                            out=oth[:, nlo:nhi], in0=cur[:, nlo - sh:nhi - sh], in1=cur[:, nlo + sh:nhi + sh], op=ALU.add),
                            r=[ucur], w=[uoth])
                        cur, ucur, oth, uoth = oth, uoth, cur, ucur
                        lo = nlo
                    P.op("vector", lambda e, cur=cur, g=g: e.tensor_tensor(out=tmpw[:], in0=cur[:, 128:128 + T], in1=invc[:, g, :],
                                                                          op=ALU.mult), r=[ucur, UINVC], w=[UTMPW])
                    P.op("vector", lambda e, xp=xp, ch=ch: e.tensor_tensor(out=mixT[:, ch, :], in0=tmpw[:], in1=xp[:, 128:128 + T],
                                                                          op=ALU.subtract), r=[UTMPW, UXPC[xi2]], w=[UMIX[ch]])
            for g in range(4):
                for dc in range(2):
                    bi = nb()
                    fns = [(lambda e, cc=cc, g=g, dc=dc, bi=bi: e.matmul(
                        banks[bi][:, :], lhsT=poolw[:, g * 2 + cc, dc * 128:(dc + 1) * 128],
                        rhs=mixT[:, 2 * g + cc, :], start=(cc == 0), stop=(cc == 1))) for cc in range(2)]
                    P.group("tensor", fns, r=[UC, UMIX[2 * g], UMIX[2 * g + 1]], w=[BK[bi]])
                    ch = 2 * g + dc
                    P.op("scalar", lambda e, bi=bi, ch=ch: e.activation(out=yT[:, ch, :], in_=banks[bi][:, :], func=AF.Copy,
                                                                       scale=pscT[:, ch:ch + 1]), r=[BK[bi], UC], w=[UYT[ch]])

            handoff([UINVC] + UXPC + [USA, USB, UTMPW], UB2)
            handoff(UMT, [u for row in UQT for u in row])
            pieces = [("q", i) for i in range(8)] + [("k", i) for i in range(2)] + [("v", i) for i in range(2)]
            b2a = {"i": 0}
            b2b = {"i": 0}

            def nbA():
                i = b2a["i"]
                b2a["i"] = (i + 1) % 6
                return i

            def nbB():
                i = b2b["i"]
                b2b["i"] = (i + 1) % 2
                return 6 + i

            def b2_mm(kind, pi_):
                col0 = {"q": 1024, "k": 3072, "v": 3584}[kind] + pi_ * 256
                wi = load_w(w_in[:, col0:col0 + 256], 16)
                blocks = list(range(1, 1 + NB)) if kind == "q" else list(range(NBH))
                pairs = []
                for p0 in range(0, len(blocks), 2):
                    bi = nbA()
                    fns = []
                    for j in range(2):
                        b = blocks[p0 + j]
                        fns += [(lambda e, k=k, bi=bi, wi=wi, b=b, j=j: e.matmul(
                            banks[bi][:, j * 256:(j + 1) * 256], lhsT=xnT[:, k, b * 128:(b + 1) * 128], rhs=wbuf[wi][:, k, :],
                            start=(k == 0), stop=(k == 15))) for k in range(16)]
                    P.group("tensor", fns, r=[UW[wi], UXN[blocks[p0]], UXN[blocks[p0 + 1]]], w=[BK[bi]])
                    pairs.append((bi, blocks[p0], blocks[p0 + 1]))
                return pairs

            def b2_post(kind, pi_, pairs):
                if kind == "v":
                    for (bi, b0, b1) in pairs:
                        P.op("scalar", lambda e, bi=bi, b0=b0, pi_=pi_: e.activation(
                            out=v_sb[:, b0:b0 + 2, pi_ * 256:(pi_ + 1) * 256],
                            in_=banks[bi][:, :].rearrange("p (j c) -> p j c", j=2), func=AF.Copy),
                            r=[BK[bi]], w=[UV[b0], UV[b1]])
                    return
                gbc = qgbc if kind == "q" else kgbc
                nblk = 2 * len(pairs)
                for pj, (bi, b0, b1) in enumerate(pairs):
                    for j in range(2):
                        for hh in range(2):
                            c = 8 + (pj * 2 + j) * 2 + hh
                            P.op("scalar", lambda e, bi=bi, j=j, hh=hh, c=c: e.activation(
                                out=sqj[:, 0:128], in_=banks[bi][:, j * 256 + hh * 128: j * 256 + (hh + 1) * 128], func=AF.Square,
                                accum_out=stat[:, c:c + 1]), r=[BK[bi]], w=[USQJ, USTAT])
                n2 = nblk * 2
                P.op("vector", lambda e, n2=n2: e.tensor_scalar(out=stat[:, 20:20 + n2], in0=stat[:, 8:8 + n2], scalar1=1.0 / 128,
                                                                scalar2=EPS, op0=ALU.mult, op1=ALU.add), r=[USTAT], w=[USTAT])
                P.op("scalar", lambda e, n2=n2: e.activation(out=stat[:, 32:32 + n2], in_=stat[:, 20:20 + n2], func=AF.Sqrt),
                     r=[USTAT], w=[USTAT])
                P.op("vector", lambda e, n2=n2: e.reciprocal(out=stat[:, 44:44 + n2], in_=stat[:, 32:32 + n2]), r=[USTAT], w=[USTAT])
                items = []
                for pj, (bi, b0, b1) in enumerate(pairs):
                    for j, b in enumerate((b0, b1)):
                        items.append((pj * 2 + j, bi, j, b))
                for (ti, bi, j, b) in items:
                    qr3 = qr2[ti].rearrange("p (h d) -> p h d", h=2)
                    P.op("vector", lambda e, ti=ti, bi=bi, j=j, qr3=qr3: e.tensor_tensor(
                        out=qr3, in0=banks[bi][:, j * 256:(j + 1) * 256].rearrange("p (h d) -> p h d", h=2),
                        in1=stat[:, 44 + ti * 2:44 + ti * 2 + 2].unsqueeze(2).to_broadcast([128, 2, 128]), op=ALU.mult),
                        r=[BK[bi], USTAT], w=[UQR[ti]])
                for (ti, bi, j, b) in items:
                    qr3 = qr2[ti].rearrange("p (h d) -> p h d", h=2)
                    qb3 = qbf2[ti].rearrange("p (h d) -> p h d", h=2)
                    P.op("vector", lambda e, qr3=qr3, qb3=qb3, gbc=gbc: e.tensor_tensor(
                        out=qb3[:, :, 32:128], in0=qr3[:, :, 32:128],
                        in1=gbc[:, 32:128].unsqueeze(1).to_broadcast([128, 2, 96]), op=ALU.mult),
                        r=[UQR[ti], UC], w=[UQBF[ti]])
                    P.op("vector", lambda e, qr3=qr3, gbc=gbc: e.tensor_tensor(
                        out=qr3[:, :, 0:32], in0=qr3[:, :, 0:32],
                        in1=gbc[:, 0:32].unsqueeze(1).to_broadcast([128, 2, 32]), op=ALU.mult),
                        r=[UC], w=[UQR[ti]])
                for (ti, bi, j, b) in items:
                    qr3 = qr2[ti].rearrange("p (h d) -> p h d", h=2)
                    P.op("vector", lambda e, ti=ti, qr3=qr3, b=b: e.tensor_tensor(
                        out=rotA2[ti], in0=qr3[:, :, 0:32], in1=ropet[:, b, 0:32].unsqueeze(1).to_broadcast([128, 2, 32]), op=ALU.mult),
                        r=[UQR[ti], UROPE], w=[UROT[ti]])
                    P.op("vector", lambda e, ti=ti, qr3=qr3, b=b: e.tensor_tensor(
                        out=rotB2[ti][:, :, 0:16], in0=qr3[:, :, 16:32],
                        in1=ropet[:, b, 32:48].unsqueeze(1).to_broadcast([128, 2, 16]), op=ALU.mult),
                        r=[UQR[ti], UROPE], wacc=[UROT[ti]])
                    P.op("vector", lambda e, ti=ti, qr3=qr3, b=b: e.tensor_tensor(
                        out=rotB2[ti][:, :, 16:32], in0=qr3[:, :, 0:16],
                        in1=ropet[:, b, 48:64].unsqueeze(1).to_broadcast([128, 2, 16]), op=ALU.mult),
                        r=[UQR[ti], UROPE], wacc=[UROT[ti]])
                for (ti, bi, j, b) in items:
                    qb3 = qbf2[ti].rearrange("p (h d) -> p h d", h=2)
                    P.op("vector", lambda e, ti=ti, qb3=qb3: e.tensor_tensor(out=qb3[:, :, 0:32], in0=rotA2[ti], in1=rotB2[ti], op=ALU.add),
                         r=[UROT[ti]], wacc=[UQBF[ti]])
                for pj, (bi, b0, b1) in enumerate(pairs):
                    bj = nbB()
                    fns = []
                    for j in range(2):
                        ti = pj * 2 + j
                        for hh in range(2):
                            fns.append(lambda e, ti=ti, hh=hh, j=j, bj=bj: e.transpose(
                                out=banks_bf[bj][:, hh * 256 + j * 128: hh * 256 + (j + 1) * 128],
                                in_=qbf2[ti][:, hh * 128:(hh + 1) * 128], identity=ident_b[:]))
                    P.group("tensor", fns, r=[UQBF[pj * 2], UQBF[pj * 2 + 1], UC], w=[BK[bj]])
                    src = banks_bf[bj][:, 0:512].rearrange("p (h t) -> p h t", h=2)
                    if kind == "q":
                        qb = b0 - 1
                        P.op("scalar", lambda e, src=src, pi_=pi_, qb=qb: e.activation(
                            out=qT[:, pi_ * 2:(pi_ + 1) * 2, qb * 128:(qb + 2) * 128], in_=src, func=AF.Copy),
                            r=[BK[bj]], wacc=[UQT[qb][pi_ // 2], UQT[qb + 1][pi_ // 2]])
                    else:
                        P.op("scalar", lambda e, src=src, pi_=pi_, b0=b0: e.activation(
                            out=kT[:, pi_ * 2:(pi_ + 1) * 2, b0 * 128:(b0 + 2) * 128], in_=src, func=AF.Copy),
                            r=[BK[bj]], wacc=[UKT[b0], UKT[b1]])

            for row in UQT:
                for u in row:
                    u.lw = list(u.rd) + list(u.lw)
                    u.rd = []
            for u in UKT:
                u.lw = list(u.rd) + list(u.lw)
                u.rd = []
            prev = None
            for (kind, pi_) in pieces:
                pairs = b2_mm(kind, pi_)
                if prev is not None:
                    b2_post(*prev)
                prev = (kind, pi_, pairs)
            b2_post(*prev)

            handoff(UMIX, [u for row in UOT for u in row])
            scale = 128.0 ** -0.5
            pslot = {"i": 0}
            for qb in range(NB):
                for kh in range(4):
                    pi = pslot["i"]
                    pslot["i"] = 1 - pi
                    for jc in range(3):
                        kb = qb + jc
                        bi = nb()
                        fns = [lambda e, bi=bi, kb=kb, kh=kh, qb=qb, jc=jc: e.matmul(
                            banks[bi][:, :].rearrange("p (h q) -> p h q", h=4), lhsT=kT[:, kh, kb * 128:(kb + 1) * 128],
                            rhs=qT[:, kh * 4:(kh + 1) * 4, qb * 128:(qb + 1) * 128], start=True, stop=(jc == 1))]
                        if jc != 1:
                            mi = 0 if jc == 0 else 1
                            fns.append(lambda e, bi=bi, mi=mi: e.matmul(banks[bi][:, :], lhsT=ident_b[:], rhs=masks[:, mi, :],
                                                                      start=False, stop=True))
                        P.group("tensor", fns, r=[UKT[kb], UC] + UQT[qb], w=[BK[bi]])
                        col = s * NBH + kb
                        P.op("scalar", lambda e, bi=bi, pi=pi, jc=jc, col=col: e.activation(
                            out=pT[pi][:, jc, :], in_=banks[bi][:, :], func=AF.Exp, bias=kbias[:, col:col + 1], scale=scale),
                            r=[BK[bi], UC], w=[UPT[pi][jc]])
                    bo = nb()
                    fns = [(lambda e, jc=jc, bo=bo, pi=pi, qb=qb, kh=kh: e.matmul(
                        banks[bo][:, :], lhsT=v_sb[:, qb + jc, kh * 128:(kh + 1) * 128], rhs=pT[pi][:, jc, :],
                        start=(jc == 0), stop=(jc == 2))) for jc in range(3)]
                    P.group("tensor", fns, r=[UV[qb], UV[qb + 1], UV[qb + 2]] + UPT[pi], w=[BK[bo]])
                    bd = nb()
                    fns = [(lambda e, jc=jc, bd=bd, pi=pi: e.matmul(
                        banks[bd][:, :], lhsT=ones_b[:], rhs=pT[pi][:, jc, :], start=(jc == 0), stop=(jc == 2))) for jc in range(3)]
                    P.group("tensor", fns, r=[UC] + UPT[pi], w=[BK[bd]])
                    dd3 = ddt.rearrange("p (h q) -> p h q", h=4)
                    P.op("vector", lambda e, bd=bd, kh=kh, dd3=dd3: e.tensor_tensor(
                        out=dd3, in0=banks[bd][:, :].rearrange("p (h q) -> p h q", h=4),
                        in1=esink[:, kh * 4:(kh + 1) * 4].unsqueeze(2).to_broadcast([128, 4, 128]), op=ALU.add),
                        r=[BK[bd], UC], w=[UDD])
                    P.op("vector", lambda e: e.reciprocal(out=ddt, in_=ddt), r=[UDD], w=[UDD])
                    P.op("vector", lambda e, bo=bo, qb=qb, kh=kh, dd3=dd3: e.tensor_tensor(
                        out=oT[:, kh * 4:(kh + 1) * 4, qb * 128:(qb + 1) * 128],
                        in0=banks[bo][:, :].rearrange("p (h q) -> p h q", h=4), in1=dd3, op=ALU.mult),
                        r=[BK[bo], UDD], w=[UOT[qb][kh]])

            handoff([UINVC] + UXPC + [USA, USB, UTMPW, UHNT] + UB2, USG + UTACC)
            handoff([u for row in UQT for u in row], UMT)
            allOT = [u for row in UOT for u in row]
            xn_main = lambda k: xnT[:, k, 128:128 + T]
            for cg in range(8):
                wi = load_w(w_in[:, 4096 + cg * 256: 4096 + (cg + 1) * 256], 16)
                for cc in range(2):
                    bi = nb()
                    fns = [(lambda e, k=k, bi=bi, wi=wi, cc=cc: e.matmul(
                        banks[bi][:, :], lhsT=wbuf[wi][:, k, cc * 128:(cc + 1) * 128], rhs=xn_main(k),
                        start=(k == 0), stop=(k == 15))) for k in range(16)]
                    P.group("tensor", fns, r=[UW[wi]] + UXN, w=[BK[bi]])
                    P.op("scalar", lambda e, bi=bi, cc=cc: e.activation(out=sg[:, cc, :], in_=banks[bi][:, :], func=AF.Sigmoid),
                         r=[BK[bi]], w=[USG[cc]])
                wi = load_w(pool_proj[:, cg * 256:(cg + 1) * 256], 8)
                for cc in range(2):
                    bi = nb()
                    fns = [(lambda e, k=k, bi=bi, wi=wi, cc=cc: e.matmul(
                        banks[bi][:, :], lhsT=wbuf[wi][:, k, cc * 128:(cc + 1) * 128], rhs=yT[:, k, :],
                        start=(k == 0), stop=(k == 7))) for k in range(8)]
                    P.group("tensor", fns, r=[UW[wi]] + UYT, w=[BK[bi]])
                    P.op("vector", lambda e, bi=bi, cc=cc: e.tensor_tensor(out=tacc[:, cc, :], in0=banks[bi][:, :], in1=sg[:, cc, :],
                                                                        op=ALU.mult), r=[BK[bi], USG[cc]], w=[UTACC[cc]])
                wi = load_w(w_in[:, 6144 + cg * 256: 6144 + (cg + 1) * 256], 16)
                for cc in range(2):
                    bi = nb()
                    fns = [(lambda e, k=k, bi=bi, wi=wi, cc=cc: e.matmul(
                        banks[bi][:, :], lhsT=wbuf[wi][:, k, cc * 128:(cc + 1) * 128], rhs=xn_main(k),
                        start=(k == 0), stop=(k == 15))) for k in range(16)]
                    P.group("tensor", fns, r=[UW[wi]] + UXN, w=[BK[bi]])
                    P.op("scalar", lambda e, bi=bi, cc=cc: e.activation(out=sg[:, cc, :], in_=banks[bi][:, :], func=AF.Sigmoid),
                         r=[BK[bi]], w=[USG[cc]])
                wi = load_w(attn_proj[:, cg * 256:(cg + 1) * 256], 16)
                for cc in range(2):
                    bi = nb()
                    fns = [(lambda e, k=k, bi=bi, wi=wi, cc=cc: e.matmul(
                        banks[bi][:, :], lhsT=wbuf[wi][:, k, cc * 128:(cc + 1) * 128], rhs=oT[:, k, :],
                        start=(k == 0), stop=(k == 15))) for k in range(16)]
                    P.group("tensor", fns, r=[UW[wi]] + allOT, w=[BK[bi]])
                    P.op("vector", lambda e, bi=bi, cc=cc: e.tensor_tensor(out=sg[:, cc, :], in0=banks[bi][:, :], in1=sg[:, cc, :],
                                                                        op=ALU.mult), r=[BK[bi], USG[cc]], w=[USG[cc]])
                    c = cg * 2 + cc
                    P.op("vector", lambda e, cc=cc, c=c: e.tensor_tensor(out=mT[:, c, :], in0=sg[:, cc, :], in1=tacc[:, cc, :],
                                                                      op=ALU.add), r=[USG[cc], UTACC[cc]], w=[UMT[c]])

            handoff([u for row in UPT for u in row] + [USQJ], UXB[2:4])
            handoff(USG + UTACC, [UHNT])
            for b in range(NB):
                P.dma("sync", lambda e, s=s, b=b: e.dma_start(out=hbuf[b][:], in_=x_in[s, (b + 1) * 128:(b + 2) * 128, :]),
                      UXB[b], w=[UXB[b]])
            for cg in range(8):
                wi = load_w(w_out[:, cg * 256:(cg + 1) * 256], 16)
                for b0 in (0, 2):
                    bi = nb()
                    fns = []
                    for j in range(2):
                        b = b0 + j
                        fns += [(lambda e, k=k, bi=bi, wi=wi, b=b, j=j: e.matmul(
                            banks[bi][:, j * 256:(j + 1) * 256], lhsT=mT[:, k, b * 128:(b + 1) * 128], rhs=wbuf[wi][:, k, :],
                            start=(k == 0), stop=(k == 15))) for k in range(16)]
                    P.group("tensor", fns, r=[UW[wi]] + UMT, w=[BK[bi]])
                    for j in range(2):
                        b = b0 + j
                        P.op("vector", lambda e, bi=bi, b=b, cg=cg, j=j: e.tensor_tensor(
                            out=hbuf[b][:, cg * 256:(cg + 1) * 256], in0=banks[bi][:, j * 256:(j + 1) * 256],
                            in1=hbuf[b][:, cg * 256:(cg + 1) * 256], op=ALU.add), r=[BK[bi]], w=[UXB[b]])
            for b in range(NB):
                tb = s * NB + b
                hb = hbuf[b]
                L = rtmp[:, b, :]
                P.dma("sync", lambda e, tb=tb, hb=hb: e.dma_start(out=y_out[tb * 128:(tb + 1) * 128, :], in_=hb[:]),
                      UXB[b], r=[UXB[b]], w=[UYB[tb]])
                if PHASES < 2:
                    continue
                P.op("scalar", lambda e, hb=hb: e.activation(out=xs[:], in_=hb[:], func=AF.Square, accum_out=stat[:, 0:1]),
                     r=[UXB[b]], w=[UXS, USTAT])
                P.op("vector", lambda e: e.tensor_scalar(out=stat[:, 1:2], in0=stat[:, 0:1], scalar1=1.0 / D, scalar2=EPS,
                                                         op0=ALU.mult, op1=ALU.add), r=[USTAT], w=[USTAT])
                P.op("scalar", lambda e: e.activation(out=stat[:, 2:3], in_=stat[:, 1:2], func=AF.Sqrt), r=[USTAT], w=[USTAT])
                P.op("vector", lambda e: e.reciprocal(out=stat[:, 3:4], in_=stat[:, 2:3]), r=[USTAT], w=[USTAT])
                P.op("vector", lambda e, hb=hb: e.scalar_tensor_tensor(out=hb[:], in0=hb[:], scalar=stat[:, 3:4], in1=g2bc[:],
                                                                      op0=ALU.mult, op1=ALU.mult), r=[USTAT, UC], w=[UXB[b]])
                for q4 in range(4):
                    bi = nb()
                    fns = [(lambda e, c=c, bi=bi, q4=q4, hb=hb: e.transpose(
                        out=banks[bi][:, c * 128:(c + 1) * 128], in_=hb[:, (q4 * 4 + c) * 128:(q4 * 4 + c + 1) * 128],
                        identity=ident_f[:])) for c in range(4)]
                    P.group("tensor", fns, r=[UXB[b], UC], w=[BK[bi]])
                    P.op("scalar", lambda e, bi=bi, q4=q4: e.activation(
                        out=hnT[:, q4 * 4:(q4 + 1) * 4, :], in_=banks[bi][:, :].rearrange("p (c t) -> p c t", c=4), func=AF.Copy),
                        r=[BK[bi]], w=[UHNT])
                bi = nb()
                fns = [(lambda e, k=k, bi=bi: e.matmul(banks[bi][:, 0:36], lhsT=hnT[:, k, :], rhs=wr[:, k, :],
                                                       start=(k == 0), stop=(k == 15))) for k in range(16)]
                P.group("tensor", fns, r=[UHNT, UC], w=[BK[bi]])
                P.op("vector", lambda e, bi=bi, L=L: e.tensor_tensor(out=L[:, 0:36], in0=banks[bi][:, 0:36], in1=brbc[:], op=ALU.add),
                     r=[BK[bi], UC], w=[URT[b]])
            if PHASES >= 2:
                BL = range(NB)
                Ls = [rtmp[:, b, :] for b in BL]
                tbs = [s * NB + b for b in BL]

                def vop(fn, extra_r=(), w_of=None):
                    for b in BL:
                        L = Ls[b]
                        wl = [URT[b]] if w_of is None else w_of(b)
                        P.op("vector", lambda e, L=L, b=b, fn=fn: fn(e, L, b), r=[URT[b]] + list(extra_r), **wl) \
                            if isinstance(wl, dict) else P.op("vector", lambda e, L=L, b=b, fn=fn: fn(e, L, b),
                                                              r=[URT[b]] + list(extra_r), w=wl)

                def aop(fn):
                    for b in BL:
                        L = Ls[b]
                        P.op("scalar", lambda e, L=L, b=b, fn=fn: fn(e, L, b), r=[URT[b]], w=[URT[b]])

                vop(lambda e, L, b: e.memset(L[:, 40:48], -1e30))
                vop(lambda e, L, b: e.tensor_copy(out=L[:, 40:44], in_=L[:, 0:4]))
                vop(lambda e, L, b: e.max(out=L[:, 48:56], in_=L[:, 40:48]))
                vop(lambda e, L, b: e.tensor_scalar(out=L[:, 56:60], in0=L[:, 0:4], scalar1=L[:, 48:49], scalar2=None, op0=ALU.is_equal))
                vop(lambda e, L, b: e.tensor_scalar(out=L[:, 240:241], in0=L[:, 48:49], scalar1=-1.0, scalar2=None, op0=ALU.mult))
                aop(lambda e, L, b: e.activation(out=L[:, 244:248], in_=L[:, 0:4], func=AF.Exp, bias=L[:, 240:241], scale=1.0,
                                                 accum_out=L[:, 241:242]))
                vop(lambda e, L, b: e.tensor_scalar(out=L[:, 60:64], in0=L[:, 56:60], scalar1=-1.0, scalar2=1e30,
                                                    op0=ALU.add, op1=ALU.mult))
                vop(lambda e, L, b: e.tensor_tensor(
                    out=L[:, 64:96].rearrange("p (g e) -> p g e", g=4), in0=L[:, 4:36].rearrange("p (g e) -> p g e", g=4),
                    in1=L[:, 60:64].unsqueeze(2).to_broadcast([128, 4, 8]), op=ALU.add))
                vop(lambda e, L, b: e.max(out=L[:, 96:104], in_=L[:, 64:96]))
                vop(lambda e, L, b: e.tensor_scalar(out=L[:, 104:136], in0=L[:, 64:96], scalar1=L[:, 96:97], scalar2=None, op0=ALU.is_equal))
                vop(lambda e, L, b: e.tensor_scalar(out=L[:, 136:168], in0=L[:, 64:96], scalar1=L[:, 97:98], scalar2=None, op0=ALU.is_equal))
                vop(lambda e, L, b: e.tensor_tensor(out=L[:, 248:249], in0=L[:, 97:98], in1=L[:, 96:97], op=ALU.subtract))
                aop(lambda e, L, b: e.activation(out=L[:, 249:250], in_=L[:, 248:249], func=AF.Exp))
                vop(lambda e, L, b: e.reciprocal(out=L[:, 242:243], in_=L[:, 241:242]))
                vop(lambda e, L, b: e.tensor_scalar(out=L[:, 250:251], in0=L[:, 249:250], scalar1=1.0, scalar2=None, op0=ALU.add))
                vop(lambda e, L, b: e.reciprocal(out=L[:, 251:252], in_=L[:, 250:251]))
                vop(lambda e, L, b: e.tensor_tensor(out=L[:, 252:253], in0=L[:, 249:250], in1=L[:, 251:252], op=ALU.mult))
                for b in BL:
                    L, tb = Ls[b], tbs[b]
                    P.op("vector", lambda e, L=L, tb=tb: e.tensor_scalar(out=twt[:, tb, 0:1], in0=L[:, 251:252], scalar1=L[:, 242:243],
                                                                         scalar2=None, op0=ALU.mult), r=[URT[b]], w=[UTS[tb]])
                for b in BL:
                    L, tb = Ls[b], tbs[b]
                    P.op("vector", lambda e, L=L, tb=tb: e.tensor_scalar(out=twt[:, tb, 1:2], in0=L[:, 252:253], scalar1=L[:, 242:243],
                                                                         scalar2=None, op0=ALU.mult), r=[URT[b]], wacc=[UTS[tb]])
                for b in BL:
                    L = Ls[b]
                    P.op("vector", lambda e, L=L, b=b: e.tensor_tensor(out=rtb[:, b, :], in0=L[:, 104:136], in1=L[:, 136:168], op=ALU.add),
                         r=[URT[b]], w=[URTB[b]])
                cb = []
                for b in BL:
                    bi = nb()
                    cb.append(bi)
                    P.group("tensor", [lambda e, bi=bi, b=b: e.matmul(banks[bi][:, 0:32], lhsT=lstrict[:], rhs=rtb[:, b, :], start=True, stop=True),
                                       lambda e, bi=bi, b=b: e.matmul(banks[bi][:, 32:64], lhsT=ones_b[:], rhs=rtb[:, b, :], start=True, stop=True)],
                            r=[URTB[b], UC], w=[BK[bi]])
                for b in BL:
                    L, bi = Ls[b], cb[b]
                    P.op("vector", lambda e, bi=bi, L=L: e.tensor_tensor(out=L[:, 168:200], in0=banks[bi][:, 0:32], in1=basebc[:], op=ALU.add),
                         r=[BK[bi], UBASE, URT[b]], w=[URT[b]])
                    P.op("vector", lambda e, bi=bi: e.tensor_tensor(out=basebc[:], in0=banks[bi][:, 32:64], in1=basebc[:], op=ALU.add),
                         r=[BK[bi]], w=[UBASE])
                for kk in range(2):
                    o0 = 104 + 32 * kk
                    vop(lambda e, L, b, o0=o0: e.tensor_tensor(out=L[:, 200:232], in0=L[:, o0:o0 + 32], in1=L[:, 168:200], op=ALU.mult))
                    vop(lambda e, L, b: e.tensor_reduce(out=L[:, 253:254], in_=L[:, 200:232], axis=AX.X, op=ALU.add))
                    vop(lambda e, L, b, o0=o0: e.tensor_tensor(out=L[:, 200:232], in0=L[:, o0:o0 + 32], in1=ecap[:], op=ALU.mult), extra_r=[UC])
                    vop(lambda e, L, b: e.tensor_reduce(out=L[:, 254:255], in_=L[:, 200:232], axis=AX.X, op=ALU.add))
                    vop(lambda e, L, b: e.tensor_scalar(out=L[:, 255:256], in0=L[:, 253:254], scalar1=float(CAP), scalar2=None, op0=ALU.is_lt))
                    vop(lambda e, L, b: e.tensor_tensor(out=L[:, 256:257], in0=L[:, 253:254], in1=L[:, 254:255], op=ALU.add))
                    vop(lambda e, L, b: e.tensor_tensor(out=L[:, 256:257], in0=L[:, 256:257], in1=pidx[:], op=ALU.subtract), extra_r=[UC])
                    vop(lambda e, L, b: e.scalar_tensor_tensor(out=L[:, 257:258], in0=L[:, 256:257], scalar=L[:, 255:256],
                                                               in1=pidx[:], op0=ALU.mult, op1=ALU.add), extra_r=[UC])
                    for b in BL:
                        L, tb = Ls[b], tbs[b]
                        P.op("vector", lambda e, L=L, tb=tb, kk=kk: e.tensor_copy(out=tslot[:, tb, kk:kk + 1], in_=L[:, 257:258]),
                             r=[URT[b]], wacc=[UTS[tb]])
                for b in BL:
                    tb = tbs[b]
                    hb = hbuf[b]
                    P.op("scalar", lambda e, hb=hb: e.activation(out=xs[:], in_=hb[:], func=AF.Copy), r=[UXB[b]], w=[UXS])
                    for kk in range(2):
                        P.dma("gpsimd", lambda e, tb=tb, kk=kk: e.indirect_dma_start(
                            out=xg, out_offset=bass.IndirectOffsetOnAxis(ap=tslot[:, tb, kk:kk + 1], axis=0),
                            in_=xs[:], in_offset=None), UXS, r=[UXS, UTS[tb], UXGZ], wacc=[UXG])

        P.barrier()
        p1.close()

        if PHASES >= 2:
            p2 = contextlib.ExitStack()
            sb2 = lambda name, shape, d: sb(name, shape, d, p2)
            xgt = [sb2("xgt%d" % i, [128, D], BF16) for i in range(4)]
            xgT = sb2("xgT", [128, 16, CAP], BF16)
            wgu = [sb2("wgu%d" % i, [128, 16, 512], BF16) for i in range(4)]
            wd = [sb2("wd%d" % i, [128, 8, 1024], BF16) for i in range(2)]
            hT = sb2("hT", [128, 8, CAP], BF16)
            sil = [sb2("sil%d" % i, [128, CAP], F32) for i in range(2)]
            yet = [sb2("yet%d" % i, [128, D], F32) for i in range(4)]
            zt = sb2("zt", [128, D], F32)
            UXGT = [U("xgt%d" % i) for i in range(4)]
            UXGTT = [U("xgT%d" % i) for i in range(4)]
            UWGU = [U("wgu%d" % i) for i in range(4)]
            UWD = [U("wd%d" % i) for i in range(2)]
            UHT = [U("hT%d" % f) for f in range(8)]
            USIL = [U("sil%d" % i) for i in range(2)]
            UYET = [U("yet%d" % i) for i in range(4)]
            UYE = U("ye")
            UZT = U("zt")
            P.op("vector", lambda e: e.memset(zt[:], 0.0), w=[UZT])
            P.dma("sync", lambda e: e.dma_start(out=ye[NSLOT:NSLOT + 128, :], in_=zt[:]), UZT, r=[UZT], wacc=[UYE])
            gslot = {"i": 0}
            dslot = {"i": 0}
            sslot = {"i": 0}
            for ex in range(NE):
                for sbk in range(4):
                    r0 = ex * CAP + sbk * 128
                    P.dma("sync", lambda e, r0=r0, sbk=sbk: e.dma_start(out=xgt[sbk][:], in_=xg[r0:r0 + 128, :]),
                          UXGT[sbk], r=[UXG], w=[UXGT[sbk]])
                    for half in range(2):
                        bi = nb()
                        fns = [(lambda e, c=c, bi=bi, half=half, sbk=sbk: e.transpose(
                            out=banks_bf[bi][:, c * 128:(c + 1) * 128],
                            in_=xgt[sbk][:, (half * 8 + c) * 128:(half * 8 + c + 1) * 128], identity=ident_b[:])) for c in range(8)]
                        P.group("tensor", fns, r=[UXGT[sbk], UC], w=[BK[bi]])
                        P.op("vector", lambda e, bi=bi, half=half, sbk=sbk: e.tensor_copy(
                            out=xgT[:, half * 8:(half + 1) * 8, sbk * 128:(sbk + 1) * 128],
                            in_=banks_bf[bi][:].rearrange("p (c t) -> p c t", c=8)), r=[BK[bi]], w=[UXGTT[sbk]])
                for pc in range(2):
                    gi = gslot["i"]
                    ui = (gi + 1) % 4
                    gslot["i"] = (gi + 2) % 4
                    P.dma("gpsimd", lambda e, ex=ex, pc=pc, gi=gi: e.dma_start(
                        out=wgu[gi][:], in_=w_gate[ex].rearrange("(k p) n -> p k n", p=128)[:, :, pc * 512:(pc + 1) * 512]),
                        UWGU[gi], w=[UWGU[gi]])
                    P.dma("gpsimd", lambda e, ex=ex, pc=pc, ui=ui: e.dma_start(
                        out=wgu[ui][:], in_=w_up[ex].rearrange("(k p) n -> p k n", p=128)[:, :, pc * 512:(pc + 1) * 512]),
                        UWGU[ui], w=[UWGU[ui]])
                    for fc in range(4):
                        f = pc * 4 + fc
                        bg = nb()
                        fns = [(lambda e, k=k, bg=bg, gi=gi, fc=fc: e.matmul(
                            banks[bg][:, :], lhsT=wgu[gi][:, k, fc * 128:(fc + 1) * 128], rhs=xgT[:, k, :],
                            start=(k == 0), stop=(k == 15))) for k in range(16)]
                        P.group("tensor", fns, r=[UWGU[gi]] + UXGTT, w=[BK[bg]])
                        bu = nb()
                        fns = [(lambda e, k=k, bu=bu, ui=ui, fc=fc: e.matmul(
                            banks[bu][:, :], lhsT=wgu[ui][:, k, fc * 128:(fc + 1) * 128], rhs=xgT[:, k, :],
                            start=(k == 0), stop=(k == 15))) for k in range(16)]
                        P.group("tensor", fns, r=[UWGU[ui]] + UXGTT, w=[BK[bu]])
                        si = sslot["i"]
                        sslot["i"] = 1 - si
                        P.op("scalar", lambda e, bg=bg, si=si: e.activation(out=sil[si][:], in_=banks[bg][:, :], func=AF.Silu),
                             r=[BK[bg]], w=[USIL[si]])
                        P.op("vector", lambda e, bu=bu, si=si, f=f: e.tensor_tensor(out=hT[:, f, :], in0=banks[bu][:, :], in1=sil[si][:],
                                                                                 op=ALU.mult), r=[BK[bu], USIL[si]], w=[UHT[f]])
                for cg2 in range(2):
                    di = dslot["i"]
                    dslot["i"] = 1 - di
                    P.dma("gpsimd", lambda e, ex=ex, cg2=cg2, di=di: e.dma_start(
                        out=wd[di][:], in_=w_down[ex].rearrange("(k p) n -> p k n", p=128)[:, :, cg2 * 1024:(cg2 + 1) * 1024]),
                        UWD[di], w=[UWD[di]])
                    for cgh in range(2):
                        cg = cg2 * 2 + cgh
                        for sbk in range(4):
                            bi = nb()
                            fns = [(lambda e, f=f, bi=bi, di=di, sbk=sbk, cgh=cgh: e.matmul(
                                banks[bi][:, :], lhsT=hT[:, f, sbk * 128:(sbk + 1) * 128], rhs=wd[di][:, f, cgh * 512:(cgh + 1) * 512],
                                start=(f == 0), stop=(f == 7))) for f in range(8)]
                            P.group("tensor", fns, r=[UWD[di]] + UHT, w=[BK[bi]])
                            P.op("scalar", lambda e, bi=bi, sbk=sbk, cg=cg: e.activation(
                                out=yet[sbk][:, cg * 512:(cg + 1) * 512], in_=banks[bi][:, :], func=AF.Copy),
                                r=[BK[bi]], w=[UYET[sbk]])
                for sbk in range(4):
                    r0 = ex * CAP + sbk * 128
                    P.dma("sync", lambda e, r0=r0, sbk=sbk: e.dma_start(out=ye[r0:r0 + 128, :], in_=yet[sbk][:]),
                          UYET[sbk], r=[UYET[sbk]], wacc=[UYE])
            P.barrier()
            p2.close()

            p3 = contextlib.ExitStack()
            sb3 = lambda name, shape, d: sb(name, shape, d, p3)
            hb3 = [sb3("hb3_%d" % i, [128, D], F32) for i in range(2)]
            r1 = [sb3("r1_%d" % i, [128, D], F32) for i in range(2)]
            r2 = [sb3("r2_%d" % i, [128, D], F32) for i in range(2)]
            UH3 = [U("h3_%d" % i) for i in range(2)]
            UR1 = [U("r1_%d" % i) for i in range(2)]
            UR2 = [U("r2_%d" % i) for i in range(2)]
            for tb in range(NST * NB):
                i = tb % 2
                P.dma("sync", lambda e, tb=tb, i=i: e.dma_start(out=hb3[i][:], in_=y_out[tb * 128:(tb + 1) * 128, :]),
                      UH3[i], r=[UYB[tb]], w=[UH3[i]])
                P.dma("gpsimd", lambda e, tb=tb, i=i: e.indirect_dma_start(
                    out=r1[i][:], out_offset=None, in_=ye,
                    in_offset=bass.IndirectOffsetOnAxis(ap=tslot[:, tb, 0:1], axis=0)), UR1[i], r=[UYE, UTS[tb]], w=[UR1[i]])
                P.dma("gpsimd", lambda e, tb=tb, i=i: e.indirect_dma_start(
                    out=r2[i][:], out_offset=None, in_=ye,
                    in_offset=bass.IndirectOffsetOnAxis(ap=tslot[:, tb, 1:2], axis=0)), UR2[i], r=[UYE, UTS[tb]], w=[UR2[i]])
                P.op("vector", lambda e, tb=tb, i=i: e.scalar_tensor_tensor(
                    out=hb3[i][:], in0=r1[i][:], scalar=twt[:, tb, 0:1], in1=hb3[i][:], op0=ALU.mult, op1=ALU.add),
                    r=[UR1[i], UTS[tb]], w=[UH3[i]])
                P.op("vector", lambda e, tb=tb, i=i: e.scalar_tensor_tensor(
                    out=hb3[i][:], in0=r2[i][:], scalar=twt[:, tb, 1:2], in1=hb3[i][:], op0=ALU.mult, op1=ALU.add),
                    r=[UR2[i], UTS[tb]], w=[UH3[i]])
                P.dma("sync", lambda e, tb=tb, i=i: e.dma_start(out=y_out[tb * 128:(tb + 1) * 128, :], in_=hb3[i][:]),
                      UH3[i], r=[UH3[i]], w=[UYB[tb]])
            P.barrier()
            p3.close()

        block = st.enter_context(nc.Block())
        P.emit(block)
    return nc


def _core_layout(c):
    b = c // 4
    qtr = c % 4
    sts = []
    for i in range(2):
        sts.append(("p", b, qtr * 1024 + i * T, 4096))
    for i in range(8):
        sts.append(("s", b, qtr * 4096 + i * T, 16384))
    return sts


def _rope_table(pos):
    half = 16
    inv = (np.float32(500000.0) ** (-np.arange(half, dtype=np.float32) / np.float32(half))).astype(np.float32)
    ang = (pos.astype(np.float32)[:, None] * inv[None, :]).astype(np.float32)
    cos = np.cos(ang).astype(np.float32)
    sin = np.sin(ang).astype(np.float32)
    return np.concatenate([cos, cos, -sin, sin], axis=1)


_NC_CACHE = {}


def kernel(x_prompt, x_sample, norm1_g, w_in, pool_w, pool_scale, pool_proj, q_norm_g, k_norm_g, sink,
           attn_proj, w_out, norm2_g, router_group_w, router_group_b, router_expert_w, router_expert_b,
           w_gate, w_up, w_down):
    f32 = np.float32
    xp = np.asarray(x_prompt, f32)
    xsm = np.asarray(x_sample, f32)
    bc = lambda v, n: np.ascontiguousarray(np.broadcast_to(np.asarray(v, f32).reshape(1, n), (128, n)))
    shared = {
        "w_in": np.ascontiguousarray(np.asarray(w_in, f32)[0]),
        "pool_w": np.ascontiguousarray(np.asarray(pool_w, f32)[0]),
        "pool_proj": np.ascontiguousarray(np.asarray(pool_proj, f32)[0]),
        "attn_proj": np.ascontiguousarray(np.asarray(attn_proj, f32)[0]),
        "w_out": np.ascontiguousarray(np.asarray(w_out, f32)[0]),
        "w_gate": np.ascontiguousarray(np.asarray(w_gate, f32)[0]),
        "w_up": np.ascontiguousarray(np.asarray(w_up, f32)[0]),
        "w_down": np.ascontiguousarray(np.asarray(w_down, f32)[0]),
        "g1T": np.ascontiguousarray(np.asarray(norm1_g, f32)[0].reshape(16, 128).T),
        "g2bc": bc(np.asarray(norm2_g)[0], D),
        "pscT": np.ascontiguousarray(np.asarray(pool_scale, f32)[0].reshape(8, 128).T),
        "qgbc": bc(np.asarray(q_norm_g)[0], 128),
        "kgbc": bc(np.asarray(k_norm_g)[0], 128),
        "sinkbc": bc(np.asarray(sink)[0], 16),
        "wr": np.ascontiguousarray(np.concatenate([np.asarray(router_group_w, f32)[0], np.asarray(router_expert_w, f32)[0]], axis=1)),
        "brbc": bc(np.concatenate([np.asarray(router_group_b, f32)[0], np.asarray(router_expert_b, f32)[0]]), 36),
        "ident": np.eye(128, dtype=f32),
        "lstrict": np.triu(np.ones((128, 128), f32), 1),
        "ones": np.ones((128, 128), f32),
        "ecap": bc(np.arange(NE, dtype=f32) * CAP, NE),
        "pidx": (DUMP + np.arange(128, dtype=f32)).reshape(128, 1),
    }
    jj = np.arange(128)[:, None]
    qq = np.arange(128)[None, :]
    mL = np.where(qq <= jj, 0.0, NEG).astype(f32)
    mR = np.where(jj <= qq, 0.0, NEG).astype(f32)
    shared["masks"] = np.ascontiguousarray(np.concatenate([np.tile(mL, (1, 4)), np.tile(mR, (1, 4))], axis=1))

    in_maps = []
    for c in range(NCORE):
        sts = _core_layout(c)
        x_in = np.zeros((NST, TH, D), f32)
        rope = np.zeros((NST, TH, 64), f32)
        kb = np.zeros((128, NST * NBH), f32)
        invc = np.zeros((NST, 128, 4 * T), f32)
        for si, (which, b, s0, S) in enumerate(sts):
            src = xp[b] if which == "p" else xsm[b]
            lo = s0 - 128
            hi = s0 + T + 128
            a = max(lo, 0)
            z = min(hi, S)
            x_in[si, a - lo:z - lo] = src[a:z]
            pos = np.arange(lo, hi)
            rope[si] = _rope_table(np.clip(pos, 0, S - 1))
            for blk in range(NBH):
                p0 = lo + blk * 128
                if p0 < 0 or p0 >= S:
                    kb[:, si * NBH + blk] = NEG
            t = np.arange(s0, s0 + T)
            for g, w in enumerate((2, 4, 8, 16)):
                h = w // 2
                cnt = np.clip(t + h, 0, S) - np.clip(t - h, 0, S)
                invc[si, :, g * T:(g + 1) * T] = (1.0 / cnt.astype(f32))[None, :]
        m = dict(shared)
        m.update({"x_in": x_in, "rope": rope, "kbias": kb, "invcnt": invc})
        in_maps.append(m)

    if "nc" not in _NC_CACHE:
        _NC_CACHE["nc"] = build_program()
    nc = _NC_CACHE["nc"]
    res = run_bass_kernel_spmd(nc, in_maps, core_ids=list(range(NCORE)))
    y_prompt = np.zeros((2, 4096, D), f32)
    y_sample = np.zeros((2, 16384, D), f32)
    for c in range(NCORE):
        y = np.asarray(res.results[c]["y"], f32)
        b = c // 4
        qtr = c % 4
        y_prompt[b, qtr * 1024:(qtr + 1) * 1024] = y[0:1024]
        y_sample[b, qtr * 4096:(qtr + 1) * 4096] = y[1024:5120]
    return (y_prompt, y_sample)
```
